# Optimizing a Trainium2 kernel written in Bass

```python
import math
import jax, jax.numpy as jnp
from jax import lax
import numpy as np

D_MODEL = 1024
BATCH = 4
SEQ = 4096
DEPTH = 2

GRID_W = 64
CTX_LEN = 256
N_HEADS = 8
HEAD_DIM = 64
V_DIM = 2 * HEAD_DIM
QK_W = N_HEADS * 2 * HEAD_DIM
ATTN_W = N_HEADS * V_DIM
CONV_W = 1024
CONV_K = 31
ROT_AXIS = HEAD_DIM // 2
ROPE_THETA = 10000.0
Q_BLOCK = 128
D_FF = 2816
N_EXPERTS = 8
TOP_K = 2
D_FF_EXPERT = 3584
N_DENSE = (DEPTH + 1) // 2
N_MOE = DEPTH // 2
EPS = 1e-6
Q_OFF = 0
K_OFF = Q_OFF + QK_W
V_OFF = K_OFF + QK_W
GLU_OFF = V_OFF + ATTN_W
GATE_OFF = GLU_OFF + 2 * CONV_W
IN_W = GATE_OFF + 2 * D_MODEL

kernel_name = "hybrid_diffattn_conformer_moe_dit"


def _rms(x, g):
    xf = x.astype(jnp.float32)
    y = xf * lax.rsqrt(jnp.mean(xf * xf, axis=-1, keepdims=True) + EPS)
    return (y * g.astype(jnp.float32)).astype(x.dtype)


def _layer_norm(x, g, b):
    xf = x.astype(jnp.float32)
    mu = jnp.mean(xf, axis=-1, keepdims=True)
    xc = xf - mu
    var = jnp.mean(xc * xc, axis=-1, keepdims=True)
    y = xc * lax.rsqrt(var + EPS) * g.astype(jnp.float32) + b.astype(jnp.float32)
    return y.astype(x.dtype)


def _modulate(h, shift, scale):
    return h * (1 + scale) + shift


def _axial_rope(rows):
    row = jnp.broadcast_to(jnp.arange(rows, dtype=jnp.float32)[:, None], (rows, GRID_W)).reshape(-1)
    col = jnp.broadcast_to(jnp.arange(GRID_W, dtype=jnp.float32)[None, :], (rows, GRID_W)).reshape(-1)
    inv = ROPE_THETA ** (-jnp.arange(0, ROT_AXIS, 2, dtype=jnp.float32) / ROT_AXIS)
    ar = row[:, None] * inv
    ac = col[:, None] * inv
    ang = jnp.concatenate([ar, ar, ac, ac], axis=-1)
    return jnp.cos(ang), jnp.sin(ang)


def _apply_rope(x, cos, sin):
    xs = x.reshape(x.shape[:-1] + (2, 2, ROT_AXIS // 2))
    rot = jnp.concatenate([-xs[..., 1:2, :], xs[..., 0:1, :]], axis=-2).reshape(x.shape)
    c = cos[None, :, None, None, :]
    s = sin[None, :, None, None, :]
    return (x * c + rot * s).astype(x.dtype)


def _qk_heads(raw, g):
    b, n = raw.shape[:2]
    return _rms(raw.reshape(b, n, N_HEADS, 2, HEAD_DIM), g)


def _v_heads(raw):
    b, n = raw.shape[:2]
    return raw.reshape(b, n, N_HEADS, V_DIM)


def _diff_attend(q, k, v, lam):
    s = jnp.einsum('bqhcd,bkhcd->bhcqk', q, k, preferred_element_type=jnp.float32) * (HEAD_DIM ** -0.5)
    p = jax.nn.softmax(s, axis=-1)
    w = p[:, :, 0] - lam * p[:, :, 1]
    return jnp.einsum('bhqk,bkhd->bqhd', w.astype(v.dtype), v)


def _latent_diff_attention(q, k_all, v_all, lam):
    b, s = q.shape[:2]
    nb = s // Q_BLOCK
    qb = jnp.moveaxis(q.reshape((b, nb, Q_BLOCK) + q.shape[2:]), 1, 0)
    ob = lax.map(lambda qq: _diff_attend(qq, k_all, v_all, lam), qb)
    return jnp.moveaxis(ob, 0, 1).reshape(b, s, N_HEADS, V_DIM)


def _attn_out(o, g, lam_init, w_o):
    b, n = o.shape[:2]
    o = _rms(o, g) * (1 - lam_init)
    return o.reshape(b, n, ATTN_W) @ w_o


def _conformer_conv(u_glu, dw_w, dw_b, ln_g, ln_b, w_o):
    a, gt = u_glu[..., :CONV_W], u_glu[..., CONV_W:]
    u = a * jax.nn.sigmoid(gt)
    u = lax.conv_general_dilated(u, dw_w[:, None, :].astype(u.dtype), window_strides=(1,),
                                 padding=[(CONV_K // 2, CONV_K // 2)],
                                 dimension_numbers=('NWC', 'WIO', 'NWC'),
                                 feature_group_count=CONV_W) + dw_b
    u = jax.nn.silu(_layer_norm(u, ln_g, ln_b))
    return u @ w_o


def _merge(p, y_attn, dw_w, dw_b, ln_g, ln_b, w_conv_o, w_out):
    y_conv = _conformer_conv(p[..., GLU_OFF:GATE_OFF], dw_w, dw_b, ln_g, ln_b, w_conv_o)
    gates = jax.nn.sigmoid(p[..., GATE_OFF:])
    return (gates[..., :D_MODEL] * y_attn + gates[..., D_MODEL:] * y_conv) @ w_out


def _swiglu(h, wg, wu, wd):
    return (jax.nn.silu(h @ wg) * (h @ wu)) @ wd


def _moe_swiglu(h, w_r, wg, wu, wd):
    lead = h.shape[:-1]
    t = h.reshape(-1, D_MODEL)
    logits = (t @ w_r).astype(jnp.float32)
    top_v, top_i = lax.top_k(logits, TOP_K)
    top_w = jax.nn.softmax(top_v, axis=-1)
    gates = jnp.sum(jax.nn.one_hot(top_i, N_EXPERTS, dtype=jnp.float32) * top_w[..., None], axis=1)
    out = jnp.zeros_like(t)
    for e in range(N_EXPERTS):
        out = out + gates[:, e:e + 1].astype(t.dtype) * _swiglu(t, wg[e], wu[e], wd[e])
    return out.reshape(lead + (D_MODEL,))


def _ffn(h, l, w_ff_gate, w_ff_up, w_ff_down, w_router, w_exp_gate, w_exp_up, w_exp_down):
    i = l // 2
    if l % 2 == 0:
        return _swiglu(h, w_ff_gate[i], w_ff_up[i], w_ff_down[i])
    return _moe_swiglu(h, w_router[i], w_exp_gate[i], w_exp_up[i], w_exp_down[i])


def setup_inputs(seed: int = 0) -> dict:
    key = jax.random.key(seed)
    ks = jax.random.split(key, 32)
    f = jnp.float32
    nrm = lambda k, shp, s: jax.random.normal(k, shp, f) * s
    gain = lambda k, shp: 1.0 + 0.02 * jax.random.normal(k, shp, f)
    d = D_MODEL
    return {
        "x": nrm(ks[0], (BATCH, SEQ, d), 1.0),
        "c": nrm(ks[1], (BATCH, d), 1.0),
        "ctx": nrm(ks[2], (BATCH, CTX_LEN, d), 1.0),
        "c_ctx": nrm(ks[3], (d,), 1.0),
        "w_mod": nrm(ks[4], (DEPTH, d, 6 * d), 0.5 * d ** -0.5),
        "b_mod": nrm(ks[5], (DEPTH, 6 * d), 0.01),
        "g_mix": gain(ks[6], (DEPTH, d)),
        "w_in": nrm(ks[7], (DEPTH, d, IN_W), d ** -0.5),
        "q_norm_g": gain(ks[8], (DEPTH, HEAD_DIM)),
        "k_norm_g": gain(ks[9], (DEPTH, HEAD_DIM)),
        "lambda_q1": nrm(ks[10], (DEPTH, HEAD_DIM), 0.1),
        "lambda_k1": nrm(ks[11], (DEPTH, HEAD_DIM), 0.1),
        "lambda_q2": nrm(ks[12], (DEPTH, HEAD_DIM), 0.1),
        "lambda_k2": nrm(ks[13], (DEPTH, HEAD_DIM), 0.1),
        "subln_g": gain(ks[14], (DEPTH, V_DIM)),
        "w_attn_o": nrm(ks[15], (DEPTH, ATTN_W, d), ATTN_W ** -0.5),
        "dw_weight": nrm(ks[16], (DEPTH, CONV_K, CONV_W), CONV_K ** -0.5),
        "dw_bias": nrm(ks[17], (DEPTH, CONV_W), 0.01),
        "conv_ln_g": gain(ks[18], (DEPTH, CONV_W)),
        "conv_ln_b": nrm(ks[19], (DEPTH, CONV_W), 0.01),
        "w_conv_o": nrm(ks[20], (DEPTH, CONV_W, d), CONV_W ** -0.5),
        "w_out": nrm(ks[21], (DEPTH, d, d), d ** -0.5),
        "g_ffn": gain(ks[22], (DEPTH, d)),
        "w_ff_gate": nrm(ks[23], (N_DENSE, d, D_FF), d ** -0.5),
        "w_ff_up": nrm(ks[24], (N_DENSE, d, D_FF), d ** -0.5),
        "w_ff_down": nrm(ks[25], (N_DENSE, D_FF, d), D_FF ** -0.5),
        "w_router": nrm(ks[26], (N_MOE, d, N_EXPERTS), d ** -0.5),
        "w_exp_gate": nrm(ks[27], (N_MOE, N_EXPERTS, d, D_FF_EXPERT), d ** -0.5),
        "w_exp_up": nrm(ks[28], (N_MOE, N_EXPERTS, d, D_FF_EXPERT), d ** -0.5),
        "w_exp_down": nrm(ks[29], (N_MOE, N_EXPERTS, D_FF_EXPERT, d), D_FF_EXPERT ** -0.5),
    }


def reference(x, c, ctx, c_ctx, w_mod, b_mod, g_mix, w_in, q_norm_g, k_norm_g,
              lambda_q1, lambda_k1, lambda_q2, lambda_k2, subln_g, w_attn_o,
              dw_weight, dw_bias, conv_ln_g, conv_ln_b, w_conv_o, w_out, g_ffn,
              w_ff_gate, w_ff_up, w_ff_down, w_router, w_exp_gate, w_exp_up, w_exp_down):
    s = x.shape[1]
    rows = s // GRID_W
    cos, sin = _axial_rope(rows)
    xc = ctx
    f32 = jnp.float32
    for l in range(DEPTH):
        last = l == DEPTH - 1
        ml = jnp.split((jax.nn.silu(c) @ w_mod[l] + b_mod[l])[:, None, :], 6, axis=-1)
        mc = jnp.split(jax.nn.silu(c_ctx) @ w_mod[l] + b_mod[l], 6, axis=-1)
        lam_init = 0.8 - 0.6 * math.exp(-0.3 * l)
        lam = (jnp.exp(jnp.sum(lambda_q1[l].astype(f32) * lambda_k1[l].astype(f32)))
               - jnp.exp(jnp.sum(lambda_q2[l].astype(f32) * lambda_k2[l].astype(f32))) + lam_init)

        h_lat = _modulate(_rms(x, g_mix[l]), ml[0], ml[1])
        h_ctx = _modulate(_rms(xc, g_mix[l]), mc[0], mc[1])
        p_lat = h_lat @ w_in[l]
        if last:
            p_kv = h_ctx @ w_in[l][:, K_OFF:GLU_OFF]
            k_ctx_raw, v_ctx_raw = p_kv[..., :QK_W], p_kv[..., QK_W:]
        else:
            p_ctx = h_ctx @ w_in[l]
            k_ctx_raw, v_ctx_raw = p_ctx[..., K_OFF:V_OFF], p_ctx[..., V_OFF:GLU_OFF]
        k_ctx = _qk_heads(k_ctx_raw, k_norm_g[l])
        v_ctx = _v_heads(v_ctx_raw)
        q_lat = _apply_rope(_qk_heads(p_lat[..., Q_OFF:K_OFF], q_norm_g[l]), cos, sin)
        k_lat = _apply_rope(_qk_heads(p_lat[..., K_OFF:V_OFF], k_norm_g[l]), cos, sin)
        v_lat = _v_heads(p_lat[..., V_OFF:GLU_OFF])
        k_all = jnp.concatenate([k_ctx, k_lat], axis=1)
        v_all = jnp.concatenate([v_ctx, v_lat], axis=1)
        o_lat = _latent_diff_attention(q_lat, k_all, v_all, lam)
        y_attn_lat = _attn_out(o_lat, subln_g[l], lam_init, w_attn_o[l])
        mix_lat = _merge(p_lat, y_attn_lat, dw_weight[l], dw_bias[l], conv_ln_g[l], conv_ln_b[l], w_conv_o[l], w_out[l])
        x = x + ml[2] * mix_lat
        if not last:
            q_ctx = _qk_heads(p_ctx[..., Q_OFF:K_OFF], q_norm_g[l])
            o_ctx = _diff_attend(q_ctx, k_ctx, v_ctx, lam)
            y_attn_ctx = _attn_out(o_ctx, subln_g[l], lam_init, w_attn_o[l])
            mix_ctx = _merge(p_ctx, y_attn_ctx, dw_weight[l], dw_bias[l], conv_ln_g[l], conv_ln_b[l], w_conv_o[l], w_out[l])
            xc = xc + mc[2] * mix_ctx

        h2 = _modulate(_rms(x, g_ffn[l]), ml[3], ml[4])
        x = x + ml[5] * _ffn(h2, l, w_ff_gate, w_ff_up, w_ff_down, w_router, w_exp_gate, w_exp_up, w_exp_down)
        if not last:
            h2c = _modulate(_rms(xc, g_ffn[l]), mc[3], mc[4])
            xc = xc + mc[5] * _ffn(h2c, l, w_ff_gate, w_ff_up, w_ff_down, w_router, w_exp_gate, w_exp_up, w_exp_down)
    return x
```

```python
import math
import types
from contextlib import ExitStack
import numpy as np
import concourse.bass as bass
import concourse.mybir as mybir
from concourse.bass_utils import run_bass_kernel_spmd

F32 = mybir.dt.float32
BF16 = mybir.dt.bfloat16
AF = mybir.ActivationFunctionType
ALU = mybir.AluOpType
AX = mybir.AxisListType

D = 1024
CTX = 256
HALF = 2048
RALL = CTX + 2 * HALF
NH = 8
INW = 7168
DFF = 2816
DFE = 3584
NE = 8
EPS = 1e-6
UT_W = 4448
DEBUG = False
SKIP = set()


def ucol(r):
    if r < CTX:
        return 16 + r
    if r < CTX + HALF:
        return 288 + 16 + (r - CTX)
    return 2368 + 16 + (r - CTX - HALF)


GROUPS = [(0, 256, "ctx")] + [(CTX + 512 * i, 512, "own") for i in range(4)] + \
         [(CTX + HALF + 512 * i, 512, "oth") for i in range(4)]


def freeze(fn):
    if fn.__closure__ is None:
        return fn
    cells = []
    for c in fn.__closure__:
        try:
            cells.append(types.CellType(c.cell_contents))
        except ValueError:
            cells.append(c)
    return types.FunctionType(fn.__code__, fn.__globals__, fn.__name__, fn.__defaults__, tuple(cells))


class Sem:
    def __init__(self, h):
        self.h = h
        self.val = 0


class Slot:
    def __init__(self, name):
        self.name = name
        self.w = {}
        self.r = {}
        self.ld = None
        self.st = None


class Prog:
    CE = ("tensor", "vector", "scalar", "gpsimd")
    ENGS = ("tensor", "vector", "scalar", "gpsimd", "sync")

    def __init__(self, nc, n_hw=40, n_sw=10):
        self.nc = nc
        self.psets = [{e: Sem(nc.alloc_semaphore(name=f"pg{i}_{e}")) for e in self.CE} for i in range(2)]
        self.cur = 1
        self.pools = {"sync": [Sem(nc.alloc_semaphore(name=f"dh_{i}")) for i in range(n_hw)],
                      "gpsimd": [Sem(nc.alloc_semaphore(name=f"ds_{i}")) for i in range(n_sw)]}
        self.rr = {"sync": 0, "gpsimd": 0}
        self.reset()

    def reset(self):
        self.q = {e: [] for e in self.ENGS}
        self.waited = {e: {} for e in self.ENGS}
        self.cur = 1 - self.cur
        self.psem = self.psets[self.cur]
        for sm in self.psem.values():
            sm.val = 0
        self.slots = []

    def drain(self, eng="sync"):
        for pool in self.pools.values():
            for sem in pool:
                if sem.val > 0:
                    self._wait(eng, (sem, sem.val))

    def slot(self, name="s"):
        sl = Slot(name)
        self.slots.append(sl)
        return sl

    def slots_n(self, n, name="s"):
        return [self.slot(f"{name}{i}") for i in range(n)]

    def _wait(self, eng, tk):
        sem, val = tk
        if eng == "tensor" and sem is self.psem["tensor"]:
            return
        if self.waited[eng].get(id(sem), 0) >= val:
            return
        self.waited[eng][id(sem)] = val
        self.q[eng].append(lambda e, sem=sem, val=val: e.wait_ge(sem.h, val))

    def _deps(self, eng, reads, writes):
        for sl in reads:
            for tk in sl.w.values():
                self._wait(eng, tk)
        for sl in writes:
            for tk in sl.w.values():
                self._wait(eng, tk)
            for tk in sl.r.values():
                self._wait(eng, tk)

    def _commit(self, tk, reads, writes):
        key = id(tk[0])
        for sl in reads:
            sl.r[key] = tk
        for sl in writes:
            if sl.r:
                sl.w = {}
                sl.r = {}
            sl.w[key] = tk

    def op(self, eng, fn, reads=(), writes=()):
        self._deps(eng, reads, writes)
        fn = freeze(fn)
        sem = self.psem[eng]
        sem.val += 1
        tk = (sem, sem.val)
        self.q[eng].append(lambda e, fn=fn, sem=sem: fn(e).then_inc(sem.h, 1))
        self._commit(tk, reads, writes)
        return tk

    def dma(self, eng, out, in_, slot, load):
        if load:
            self._deps(eng, (), (slot,))
        else:
            self._deps(eng, (slot,), ())
        pool = self.pools[eng]
        sem = pool[self.rr[eng] % len(pool)]
        self.rr[eng] += 1
        if sem.val > 0:
            self._wait(eng, (sem, sem.val))
        sem.val += 16
        tk = (sem, sem.val)
        self.q[eng].append(
            lambda e, out=out, in_=in_, sem=sem: e.dma_start(out=out, in_=in_).then_inc(sem.h, 16))
        if load:
            self._commit(tk, (), (slot,))
        else:
            self._commit(tk, (slot,), ())
        return tk

    def flush(self):
        for pool in self.pools.values():
            for sem in pool:
                if sem.val > 0:
                    self._wait("sync", (sem, sem.val))
        nc = self.nc
        others = list(self.psets[1 - self.cur].values())
        self.q["vector"].insert(0, lambda e: [e.sem_clear(sm.h) for sm in others])
        with nc.Block() as block:
            for eng in self.ENGS:
                items = self.q[eng]

                def body(e, items=items):
                    for it in items:
                        it(e)
                getattr(block, eng)(body)
        self.reset()


class Builder:
    def __init__(self, nc, upto=None):
        self.nc = nc
        self.upto = upto
        self.P = Prog(nc)
        dt_in = lambda name, shape, dt=F32: nc.dram_tensor(name, list(shape), dt, kind="ExternalInput").ap()
        kind_s = "ExternalOutput"
        dt_sc = lambda name, shape, dt: nc.dram_tensor(name, list(shape), dt, kind=kind_s).ap()
        self.x0 = dt_in("x0", [RALL, D])
        self.cvec = dt_in("cvec", [128, 16])
        self.rope = dt_in("rope", [RALL, 2, 64])
        self.mask = dt_in("mask", [128, 2])
        self.identf = dt_in("identf", [128, 128])
        self.w_mod = dt_in("w_mod", [2, D, 6 * D])
        self.b_mod = dt_in("b_mod", [2, 6 * D])
        self.g_mix = dt_in("g_mix", [2, D])
        self.w_in = dt_in("w_in", [2, D, INW])
        self.q_norm_g = dt_in("q_norm_g", [2, 64])
        self.k_norm_g = dt_in("k_norm_g", [2, 64])
        self.lams = [dt_in(n, [2, 64]) for n in ("lambda_q1", "lambda_k1", "lambda_q2", "lambda_k2")]
        self.subln_g = dt_in("subln_g", [2, 128])
        self.w_attn_o = dt_in("w_attn_o", [2, D, D])
        self.dwT = dt_in("dwT", [2, 128, 8, 31])
        self.cvp = dt_in("cvp", [2, 128, 3, 8])
        self.w_conv_o = dt_in("w_conv_o", [2, D, D])
        self.w_out = dt_in("w_out", [2, D, D])
        self.g_ffn = dt_in("g_ffn", [2, D])
        self.w_ff_gate = dt_in("w_ff_gate", [1, D, DFF])
        self.w_ff_up = dt_in("w_ff_up", [1, D, DFF])
        self.w_ff_down = dt_in("w_ff_down", [1, DFF, D])
        self.w_router = dt_in("w_router", [1, D, NE])
        self.w_exp_gate = dt_in("w_exp_gate", [1, NE, D, DFE])
        self.w_exp_up = dt_in("w_exp_up", [1, NE, D, DFE])
        self.w_exp_down = dt_in("w_exp_down", [1, NE, DFE, D])
        self.out = nc.dram_tensor("out", [HALF, D], F32, kind="ExternalOutput").ap()
        self.XS = dt_sc("XS", [RALL, D], F32)
        self.MODV = dt_sc("MODV", [2, 2, 6 * D], F32)
        self.KT = dt_sc("KT", [NH, 128, RALL], BF16)
        self.QT = dt_sc("QT", [NH, 128, RALL], BF16)
        self.V = dt_sc("V", [RALL, D], BF16)
        self.UT = dt_sc("UT", [8, 128, UT_W], BF16)
        self.GT = dt_sc("GT", [16, 128, RALL], BF16)
        self.MC = dt_sc("MC", [8, 128, RALL], BF16)
        self.OT = dt_sc("OT", [NH, 128, RALL], BF16)
        self.ident = nc.alloc_sbuf_tensor("ident", [128, 128], BF16)
        self.onesf = nc.alloc_sbuf_tensor("onesf", [128, 128], F32)
        self.nhalf = nc.alloc_sbuf_tensor("nhalf", [128, 64], F32)
        self.maskt = nc.alloc_sbuf_tensor("maskt", [128, 2], F32)

    def sb(self, es, name, shape, dt):
        self.uid = getattr(self, "uid", 0) + 1
        return es.enter_context(self.nc.sbuf_tensor(f"{name}_{self.uid}", list(shape), dt))

    def ps(self, es, name, shape, dt=F32):
        self.uid = getattr(self, "uid", 0) + 1
        return es.enter_context(self.nc.psum_tensor(f"{name}_{self.uid}", list(shape), dt))

    def rsqrt(self, src_ap, dst_ap, tmp_ap, n_inv, slots_r, slots_w, tmp_slot, width, mode="act"):
        P = self.P
        P.op("vector", lambda e: e.tensor_scalar(out=tmp_ap, in0=src_ap, scalar1=n_inv, scalar2=EPS,
                                                 op0=ALU.mult, op1=ALU.add),
             reads=slots_r, writes=(tmp_slot,))
        if mode == "pool":
            nh = self.nhalf[:, 0:width]
            P.op("gpsimd", lambda e: e.tensor_tensor(out=dst_ap, in0=tmp_ap, in1=nh, op=ALU.pow),
                 reads=(tmp_slot,), writes=slots_w)
        else:
            P.op("scalar", lambda e: e.activation(out=tmp_ap, in_=tmp_ap, func=AF.Sqrt), reads=(tmp_slot,), writes=(tmp_slot,))
            P.op("vector", lambda e: e.reciprocal(out=dst_ap, in_=tmp_ap), reads=(tmp_slot,), writes=slots_w)

    def load_mod_tile(self, es, l, v, who, name, gbc=None, gslot=None):
        P = self.P
        t = self.sb(es, name, [128, D], F32)
        sl = P.slot(name)
        src = self.MODV[l, who, v * D:(v + 1) * D].partition_broadcast(128)
        P.dma("sync", t[:], src, sl, True)
        if gbc is not None:
            P.op("vector", lambda e: e.scalar_tensor_tensor(out=t[:], in0=t[:], scalar=1.0, in1=gbc[:],
                                                            op0=ALU.add, op1=ALU.mult),
                 reads=(gslot,), writes=(sl,))
        return t, sl

    def load_w(self, dst_tile, dst_slot, src_ap, kchunks, ncols, piece=512, order=None):
        P = self.P
        src = src_ap.rearrange("(k p) n -> p k n", p=128)
        npieces = (ncols + piece - 1) // piece
        slots = [dst_slot] * npieces if dst_slot is not None else P.slots_n(npieces, "wp")
        for pi in (order if order is not None else range(npieces)):
            c0 = pi * piece
            c1 = min(ncols, c0 + piece)
            P.dma("gpsimd", dst_tile[:, 0:kchunks, c0:c1], src[:, :, c0:c1], slots[pi], True)
        return slots

    def phase_const(self):
        P = self.P
        with ExitStack() as es:
            sl = P.slot("c")
            sl2 = P.slot("c2")
            P.dma("gpsimd", self.ident[:], self.identf[:, :], sl2, True)
            P.dma("sync", self.maskt[:], self.mask[:, :], sl, True)
            P.op("vector", lambda e: e.memset(self.onesf[:], 1.0), writes=(sl,))
            P.op("vector", lambda e: e.memset(self.nhalf[:], -0.5), writes=(sl,))
            P.flush()

    def phase_mod(self, layers=(0, 1)):
        P = self.P
        with ExitStack() as es:
            cv = self.sb(es, "cv", [128, 16], F32)
            sg = self.sb(es, "sgm", [128, 16], F32)
            sv = self.sb(es, "sv", [128, 16], BF16)
            bm = [self.sb(es, f"bm{l}", [2, 6 * D], F32) for l in layers]
            mv = [self.sb(es, f"mv{l}", [2, 6 * D], F32) for l in layers]
            wm = [self.sb(es, f"wm{i}", [128, 8, 512], BF16) for i in range(3)]
            pm = [self.ps(es, f"pm{i}", [128, 512]) for i in range(2)]
            s_cv, s_sv = P.slot(), P.slot()
            s_bm, s_mv = P.slots_n(len(layers)), P.slots_n(len(layers))
            s_wm = P.slots_n(3)
            s_pm = P.slots_n(2)
            P.dma("sync", cv[:], self.cvec[:, :], s_cv, True)
            P.op("scalar", lambda e: e.activation(out=sg[:], in_=cv[:], func=AF.Sigmoid), reads=(s_cv,), writes=(s_sv,))
            P.op("vector", lambda e: e.tensor_tensor(out=sv[:], in0=cv[:], in1=sg[:], op=ALU.mult),
                 reads=(s_cv, s_sv), writes=(s_sv,))
            cnt = 0
            for li, l in enumerate(layers):
                P.dma("sync", bm[li][:], self.b_mod[l].partition_broadcast(2), s_bm[li], True)
                wsrc = self.w_mod[l].rearrange("(k p) n -> p k n", p=128)
                for cg in range(12):
                    i = cnt % 3
                    ip = cnt % 2
                    cnt += 1
                    P.dma("gpsimd", wm[i][:], wsrc[:, :, cg * 512:(cg + 1) * 512], s_wm[i], True)

                    def mm(e, i=i, ip=ip):
                        for k in range(8):
                            r = e.matmul(pm[ip][0:2, :], sv[:, 2 * k:2 * k + 2], wm[i][:, k, :], start=(k == 0), stop=(k == 7))
                        return r
                    P.op("tensor", mm, reads=(s_sv, s_wm[i]), writes=(s_pm[ip],))
                    P.op("vector", lambda e, ip=ip, cg=cg, li=li: e.tensor_tensor(
                        out=mv[li][0:2, cg * 512:(cg + 1) * 512], in0=pm[ip][0:2, :], in1=bm[li][0:2, cg * 512:(cg + 1) * 512],
                        op=ALU.add), reads=(s_pm[ip], s_bm[li]), writes=(s_mv[li],))
                P.dma("sync", self.MODV[l], mv[li][:], s_mv[li], False)
            P.flush()

    def setup_norm(self, es, l, which, whos=("lat", "ctx"), ntp=2, es_tp=None):
        P = self.P
        gsrc = (self.g_mix if which == 0 else self.g_ffn)[l]
        gbc = self.sb(es, "gbc", [128, D], F32)
        s_g = P.slot("gbc")
        P.dma("sync", gbc[:], gsrc.partition_broadcast(128), s_g, True)
        vb = 0 if which == 0 else 3
        res = {}
        for who, nm in ((0, "lat"), (1, "ctx")):
            if nm not in whos:
                continue
            B, sB = self.load_mod_tile(es, l, vb, who, f"B_{nm}")
            A, sA = self.load_mod_tile(es, l, vb + 1, who, f"A_{nm}", gbc, s_g)
            res[nm] = (A, sA, B, sB)
        st = dict(mod=res)
        st["x"] = [self.sb(es, f"xr{i}", [128, D], F32) for i in range(2)]
        st["s_x"] = P.slots_n(2, "x")
        st["junk"] = self.sb(es, "junk", [128, D], BF16)
        st["s_junk"] = P.slot("junk")
        st["ss"] = [self.sb(es, f"ss{i}", [128, 4], F32) for i in range(2)]
        st["s_ss"] = P.slots_n(2, "ss")
        st["hf"] = self.sb(es, "hf", [128, D], F32)
        st["s_hf"] = P.slot("hf")
        st["h"] = [self.sb(es, f"h{i}", [128, D], BF16) for i in range(2)]
        st["s_h"] = P.slots_n(2, "h")
        st["ntp"] = ntp
        st["tp"] = [self.ps(es_tp if es_tp is not None else es, f"tp{i}", [128, 8, 128], BF16) for i in range(ntp)]
        st["s_tp"] = P.slots_n(ntp, "tp")
        st["cnt"] = 0
        st["tpc"] = 0
        return st

    def emit_hA(self, st, xsrc, r0, t, kind):
        P = self.P
        A, sA, B, sB = st["mod"]["ctx" if kind == "ctx" else "lat"]
        i = st["cnt"] % 2
        st["cnt"] += 1
        x, sx = st["x"][i], st["s_x"][i]
        ss, sss = st["ss"][i], st["s_ss"][i]
        h, sh = st["h"][i], st["s_h"][i]
        P.dma("sync", x[:], xsrc[r0 + t * 128:r0 + (t + 1) * 128, :], sx, True)
        P.op("scalar", lambda e: e.activation(out=st["junk"][:], in_=x[:], func=AF.Square, accum_out=ss[:, 0:1]),
             reads=(sx,), writes=(st["s_junk"], sss))
        self.rsqrt(ss[:, 0:1], ss[:, 2:3], ss[:, 1:2], 1.0 / D, (sss,), (sss,), sss, 1)
        P.op("vector", lambda e: e.scalar_tensor_tensor(
            out=st["hf"][:], in0=x[:], scalar=ss[:, 2:3], in1=A[:], op0=ALU.mult, op1=ALU.mult),
            reads=(sx, sss, sA), writes=(st["s_hf"],))
        P.op("gpsimd", lambda e: e.tensor_tensor(out=h[:], in0=st["hf"][:], in1=B[:], op=ALU.add),
             reads=(st["s_hf"], sB), writes=(sh,))
        return h, sh

    def emit_hT(self, st, xsrc, r0, n, kind, hT, s_hT, col0=0, tiles=None):
        for t in (range(n // 128) if tiles is None else tiles):
            h, sh = self.emit_hA(st, xsrc, r0, t, kind)
            self.transpose8(st, h, sh, hT, s_hT, col0 + t * 128)

    def h_prefetcher(self, st, xsrc, grp, hT, s_hT):
        state = {"t": 0, "pend": None}
        nT = grp[1] // 128

        def step():
            if state["pend"] is not None:
                h, sh, t = state["pend"]
                self.transpose8(st, h, sh, hT, s_hT, t * 128)
                state["pend"] = None
            if state["t"] < nT:
                t = state["t"]
                state["t"] += 1
                h, sh = self.emit_hA(st, xsrc, grp[0], t, grp[2])
                state["pend"] = (h, sh, t)
            return state["pend"] is not None or state["t"] < nT

        def finish():
            while step():
                pass
        return step, finish

    def transpose8(self, st, src, s_src, dst, s_dst, c0, eng="scalar"):
        P = self.P
        j = st["tpc"] % st["ntp"]
        st["tpc"] += 1
        tp, stp = st["tp"][j], st["s_tp"][j]

        def tr(e):
            for k in range(8):
                r = e.transpose(tp[:, k, :], src[:, k * 128:(k + 1) * 128], self.ident[:])
            return r
        P.op("tensor", tr, reads=(s_src,), writes=(stp,))
        if eng == "scalar":
            P.op("scalar", lambda e: e.copy(out=dst[:, 0:8, c0:c0 + 128], in_=tp[:]), reads=(stp,), writes=(s_dst,))
        else:
            P.op("vector", lambda e: e.tensor_copy(out=dst[:, 0:8, c0:c0 + 128], in_=tp[:]), reads=(stp,), writes=(s_dst,))

    def phase_kvq(self, l, xsrc, q_kinds):
        P = self.P
        with ExitStack() as es:
            st = self.setup_norm(es, l, 0)
            w = self.sb(es, "w", [128, 8, 3072], BF16)
            s_wp = self.load_w(w, None, self.w_in[l][:, 0:3072], 8, 3072, order=[2, 3, 4, 5, 0, 1])
            hT = [self.sb(es, f"hT{i}", [128, 8, 512], BF16) for i in range(2)]
            s_hT = P.slots_n(2, "hT")
            NTOK = 3
            tok = [self.ps(es, f"tok{i}", [128, 1024]) for i in range(NTOK)]
            s_tok = P.slots_n(NTOK, "tok")
            gq = self.sb(es, "gq", [128, 2, 64], F32)
            gsw = self.sb(es, "gsw", [128, 2, 64], F32)
            s_g = P.slot("g")
            P.dma("sync", gq[:, 0, :], self.q_norm_g[l].partition_broadcast(128), s_g, True)
            P.dma("sync", gq[:, 1, :], self.k_norm_g[l].partition_broadcast(128), s_g, True)
            g4 = gq[:].rearrange("p a (b h e) -> p (a b) h e", b=2, h=2)
            gs4 = gsw[:].rearrange("p a (b h e) -> p (a b) h e", b=2, h=2)
            P.op("vector", lambda e: e.tensor_copy(out=gs4[:, :, 0, :], in_=g4[:, :, 1, :]), reads=(s_g,), writes=(s_g,))
            P.op("vector", lambda e: e.tensor_copy(out=gs4[:, :, 1, :], in_=g4[:, :, 0, :]), reads=(s_g,), writes=(s_g,))
            rp = [self.sb(es, f"rp{i}", [128, 2, 64], F32) for i in range(2)]
            s_rp = P.slots_n(2, "rp")
            cg = [self.sb(es, f"cg{i}", [128, 2, 2, 64], F32) for i in range(2)]
            s_cg = P.slots_n(2, "cg")
            sq = [self.sb(es, f"sq{i}", [128, D], F32) for i in range(2)]
            kn = [self.sb(es, f"kn{i}", [128, D], F32) for i in range(2)]
            raw = [self.sb(es, f"raw{i}", [128, D], F32) for i in range(2)]
            s_raw = P.slots_n(2, "raw")
            t2 = [self.sb(es, f"t2{i}", [128, D], F32) for i in range(2)]
            kr = [self.sb(es, f"kr{i}", [128, D], BF16) for i in range(2)]
            s16 = [self.sb(es, f"s16{i}", [128, 3, 16], F32) for i in range(2)]
            s_sq, s_kn, s_t2, s_kr, s_s16 = (P.slots_n(2, "sq"), P.slots_n(2, "kn"), P.slots_n(2, "t2"),
                                             P.slots_n(2, "kr"), P.slots_n(2, "s16"))
            stage = [self.sb(es, f"stg{i}", [128, 8, 512], BF16) for i in range(4)]
            s_stage = P.slots_n(4, "stg")
            vst = [self.sb(es, f"vst{i}", [128, D], BF16) for i in range(2)]
            s_vst = P.slots_n(2, "vst")
            cnt = dict(tok=0, v=0, rp=0)

            def proj_tok(hTt, shT, t, col0):
                i = cnt["tok"] % NTOK
                cnt["tok"] += 1

                def mm(e):
                    for half in range(2):
                        for k in range(8):
                            r = e.matmul(tok[i][:, half * 512:(half + 1) * 512], hTt[:, k, t * 128:(t + 1) * 128],
                                         w[:, k, col0 + half * 512:col0 + (half + 1) * 512], start=(k == 0), stop=(k == 7))
                    return r
                P.op("tensor", mm, reads=(shT, s_wp[col0 // 512], s_wp[col0 // 512 + 1]), writes=(s_tok[i],))
                return tok[i], s_tok[i]

            def chain(pt, spt, b, cgt, scg, stg, sstg, t):
                s3, ss3 = s16[b], s_s16[b]
                g3 = lambda ap: ap.rearrange("p (g e) -> p g e", e=64)
                stages = []
                def st0():
                    P.op("scalar", lambda e: e.activation(out=sq[b][:], in_=pt[:], func=AF.Square), reads=(spt,), writes=(s_sq[b],))
                    P.op("scalar", lambda e: e.copy(out=raw[b][:], in_=pt[:]), reads=(spt,), writes=(s_raw[b],))
                stages.append(st0)
                stages.append(lambda: P.op("vector", lambda e: e.tensor_reduce(out=s3[:, 0, :], in_=g3(sq[b][:]), axis=AX.X, op=ALU.add),
                                           reads=(s_sq[b],), writes=(ss3,)))
                stages.append(lambda: self.rsqrt(s3[:, 0, :], s3[:, 2, :], s3[:, 1, :], 1.0 / 64, (ss3,), (ss3,), ss3, 16))
                stages.append(lambda: P.op("vector", lambda e: e.tensor_tensor(
                    out=g3(kn[b][:]), in0=g3(raw[b][:]), in1=s3[:, 2, :].unsqueeze(2).to_broadcast([128, 16, 64]), op=ALU.mult),
                    reads=(s_raw[b], ss3), writes=(s_kn[b],)))
                stages.append(lambda: P.op("gpsimd", lambda e: e.tensor_tensor(
                    out=g3(sq[b][:]), in0=g3(kn[b][:]), in1=cgt[:, b, 0, :].unsqueeze(1).to_broadcast([128, 16, 64]), op=ALU.mult),
                    reads=(s_kn[b], scg), writes=(s_sq[b],)))
                kn5 = kn[b][:].rearrange("p (g h e) -> p g h e", h=2, e=16)
                t25 = t2[b][:].rearrange("p (g h e) -> p g h e", h=2, e=16)
                sg5 = cgt[:, b, 1, :].rearrange("p (b h e) -> p b h e", h=2, e=16)

                def st_t2():
                    for hh in range(2):
                        P.op("vector", lambda e, hh=hh: e.tensor_tensor(
                            out=t25[:, :, hh, :].rearrange("p (g b) e -> p g b e", b=2),
                            in0=kn5[:, :, 1 - hh, :].rearrange("p (g b) e -> p g b e", b=2),
                            in1=sg5[:, :, hh, :].unsqueeze(1).to_broadcast([128, 16, 2, 16]), op=ALU.mult),
                            reads=(s_kn[b], scg), writes=(s_t2[b],))
                stages.append(st_t2)
                stages.append(lambda: P.op("gpsimd", lambda e: e.tensor_tensor(out=kr[b][:], in0=sq[b][:], in1=t2[b][:], op=ALU.add),
                                           reads=(s_sq[b], s_t2[b]), writes=(s_kr[b],)))
                stages.append(lambda: self.transpose8(st, kr[b], s_kr[b], stg, sstg, t * 128, eng="scalar"))
                return stages

            pending = []
            self.emit_hT(st, xsrc, GROUPS[0][0], GROUPS[0][1], GROUPS[0][2], hT[0], s_hT[0])
            for gi, (r0, n, kind) in enumerate(GROUPS):
                hTt, shT = hT[gi % 2], s_hT[gi % 2]
                nxt = GROUPS[gi + 1] if gi + 1 < len(GROUPS) else None
                need_q = kind in q_kinds
                T = n // 128
                pf_step, pf_finish = (None, None)
                if nxt is not None:
                    pf_step, pf_finish = self.h_prefetcher(st, xsrc, nxt, hT[(gi + 1) % 2], s_hT[(gi + 1) % 2])
                for t in range(T):
                    if pf_step is not None:
                        pf_step()
                    i = cnt["rp"] % 2
                    cnt["rp"] += 1
                    P.dma("sync", rp[i][:], self.rope[r0 + t * 128:r0 + (t + 1) * 128, :, :], s_rp[i], True)
                    for which in range(2):
                        if which == 0 and not need_q:
                            continue
                        P.op("gpsimd", lambda e, i=i, which=which: e.tensor_tensor(
                            out=cg[i][:, which, 0, :], in0=rp[i][:, 0, :], in1=gq[:, which, :], op=ALU.mult),
                            reads=(s_rp[i], s_g), writes=(s_cg[i],))
                        P.op("gpsimd", lambda e, i=i, which=which: e.tensor_tensor(
                            out=cg[i][:, which, 1, :], in0=rp[i][:, 1, :], in1=gsw[:, which, :], op=ALU.mult),
                            reads=(s_rp[i], s_g), writes=(s_cg[i],))
                    pk, spk = proj_tok(hTt, shT, t, 1024)
                    pv, spv = proj_tok(hTt, shT, t, 2048)
                    if need_q:
                        pq, spq = proj_tok(hTt, shT, t, 0)
                    for f in pending:
                        f()
                    pending = []
                    vi = cnt["v"] % 2
                    cnt["v"] += 1
                    P.op("scalar", lambda e, pv=pv, vi=vi: e.copy(out=vst[vi][:], in_=pv[:]), reads=(spv,), writes=(s_vst[vi],))
                    P.dma("sync", self.V[r0 + t * 128:r0 + (t + 1) * 128, :], vst[vi][:], s_vst[vi], False)
                    sk = (gi % 2) * 2 + 1
                    sq_ = (gi % 2) * 2
                    chains = [chain(pk, spk, 1, cg[i], s_cg[i], stage[sk], s_stage[sk], t)]
                    if need_q:
                        chains.append(chain(pq, spq, 0, cg[i], s_cg[i], stage[sq_], s_stage[sq_], t))
                    nst = len(chains[0])
                    for si in range(nst - 1):
                        for ch in chains:
                            ch[si]()
                    for ch in chains:
                        pending.append(ch[nst - 1])
                    if t == T - 1 and pf_finish is not None:
                        pf_finish()
                    if t == T - 1:
                        def stores(r0=r0, n=n, sk=sk, sq_=sq_, need_q=need_q):
                            P.dma("sync", self.KT[:, :, r0:r0 + n].rearrange("h p r -> p h r"), stage[sk][:, :, 0:n], s_stage[sk], False)
                            if need_q:
                                P.dma("sync", self.QT[:, :, r0:r0 + n].rearrange("h p r -> p h r"), stage[sq_][:, :, 0:n], s_stage[sq_], False)
                        pending.append(stores)
            for f in pending:
                f()
            P.flush()

    def phase_glu(self, l, xsrc, glu_groups, gate_kinds, do_flush=True):
        P = self.P
        with ExitStack() as es:
            st = self.setup_norm(es, l, 0)
            w = self.sb(es, "w", [128, 8, 4096], BF16)
            s_wp = self.load_w(w, None, self.w_in[l][:, 3072:INW], 8, 4096, order=[0, 2, 1, 3, 4, 5, 6, 7])
            hT = [self.sb(es, f"hT{i}", [128, 8, 512], BF16) for i in range(2)]
            s_hT = P.slots_n(2, "hT")
            fm = [self.ps(es, f"fm{i}", [128, 512]) for i in range(4)]
            s_fm = P.slots_n(4, "fm")
            sgt = [self.sb(es, f"sgt{i}", [128, 512], F32) for i in range(2)]
            s_sgt = P.slots_n(2, "sgt")
            stage = [self.sb(es, f"stg{i}", [128, 8, 512], BF16) for i in range(3)]
            s_stage = P.slots_n(3, "stg")
            cnt = dict(fm=0, sg=0, stage=0, g=0)

            def proj_fm(hTt, shT, col0, n):
                i = cnt["fm"] % 4
                cnt["fm"] += 1

                def mm(e):
                    for k in range(8):
                        r = e.matmul(fm[i][:, 0:n], w[:, k, col0:col0 + 128], hTt[:, k, 0:n], start=(k == 0), stop=(k == 7))
                    return r
                P.op("tensor", mm, reads=(shT, s_wp[col0 // 512]), writes=(s_fm[i],))
                return fm[i], s_fm[i]

            work = [(gi, g) for gi, g in enumerate(GROUPS) if (gi in glu_groups or g[2] in gate_kinds)]
            pre = {"step": None, "finish": None}

            def prefetch():
                if pre["step"] is not None:
                    pre["step"]()
            g0 = work[0][1]
            self.emit_hT(st, xsrc, g0[0], g0[1], g0[2], hT[0], s_hT[0])
            for wi, (gi, (r0, n, kind)) in enumerate(work):
                need_glu = gi in glu_groups
                need_gate = kind in gate_kinds
                hTt, shT = hT[wi % 2], s_hT[wi % 2]
                if pre["finish"] is not None:
                    pre["finish"]()
                pre["step"] = pre["finish"] = None
                if wi + 1 < len(work):
                    b = (wi + 1) % 2
                    pre["step"], pre["finish"] = self.h_prefetcher(st, xsrc, work[wi + 1][1], hT[b], s_hT[b])
                if need_glu:
                    si = cnt["stage"] % 3
                    cnt["stage"] += 1
                    for j in range(8):
                        if j % 2 == 0:
                            prefetch()
                        pa, spa = proj_fm(hTt, shT, j * 128, n)
                        pg, spg = proj_fm(hTt, shT, 1024 + j * 128, n)
                        k = cnt["sg"] % 2
                        cnt["sg"] += 1
                        P.op("scalar", lambda e, pg=pg, k=k: e.activation(out=sgt[k][:, 0:n], in_=pg[:, 0:n], func=AF.Sigmoid),
                             reads=(spg,), writes=(s_sgt[k],))
                        P.op("vector", lambda e, pa=pa, k=k, j=j, si=si: e.tensor_tensor(
                            out=stage[si][:, j, 0:n], in0=pa[:, 0:n], in1=sgt[k][:, 0:n], op=ALU.mult),
                            reads=(spa, s_sgt[k]), writes=(s_stage[si],))
                    c0 = ucol(r0)
                    P.dma("sync", self.UT[:, :, c0:c0 + n].rearrange("j p r -> p j r"), stage[si][:, :, 0:n], s_stage[si], False)
                if need_gate:
                    for half in range(2):
                        si = cnt["stage"] % 3
                        cnt["stage"] += 1
                        for j in range(8):
                            if j % 2 == 1:
                                prefetch()
                            pg, spg = proj_fm(hTt, shT, 2048 + (half * 8 + j) * 128, n)
                            P.op("scalar", lambda e, pg=pg, j=j, si=si: e.activation(
                                out=stage[si][:, j, 0:n], in_=pg[:, 0:n], func=AF.Sigmoid),
                                reads=(spg,), writes=(s_stage[si],))
                        P.dma("sync", self.GT[half * 8:(half + 1) * 8, :, r0:r0 + n].rearrange("j p r -> p j r"),
                              stage[si][:, :, 0:n], s_stage[si], False)
            if do_flush:
                P.flush()

    def phase_fix(self):
        P = self.P
        if "fix" in SKIP:
            return
        with ExitStack() as es:
            e_in = self.sb(es, "ein", [128, 8, 4, 16], BF16)
            e_out = self.sb(es, "eout", [128, 8, 6, 16], BF16)
            s_in, s_out = P.slot(), P.slot()
            own0, oth0 = 288, 2368
            srcs = [oth0 + 16 + HALF - 16, oth0 + 16, own0 + 16 + HALF - 16, own0 + 16]
            for i, c in enumerate(srcs):
                P.dma("sync", e_in[:, :, i, :], self.UT[:, :, c:c + 16].rearrange("j p r -> p j r"), s_in, True)
            mL, mR = self.maskt[:, 0:1], self.maskt[:, 1:2]
            plan = [(0, mL), (1, mR), (2, mR), (3, mL)]
            dsts = [own0, own0 + 16 + HALF, oth0, oth0 + 16 + HALF, 0, 16 + CTX]
            for i, (srci, m) in enumerate(plan):
                P.op("vector", lambda e, i=i, srci=srci, m=m: e.tensor_scalar(
                    out=e_out[:, :, i, :], in0=e_in[:, :, srci, :], scalar1=m, scalar2=None, op0=ALU.mult),
                    reads=(s_in,), writes=(s_out,))
            P.op("vector", lambda e: e.memset(e_out[:, :, 4:6, :], 0.0), writes=(s_out,))
            for i, c in enumerate(dsts):
                P.dma("sync", self.UT[:, :, c:c + 16].rearrange("j p r -> p j r"), e_out[:, :, i, :], s_out, False)
            P.flush()

    def phase_conv(self, l, kinds):
        P = self.P
        with ExitStack() as es:
            dw = self.sb(es, "dw", [128, 8, 31], F32)
            cvp = self.sb(es, "cvp", [128, 3, 8], F32)
            idf = self.sb(es, "idf", [128, 128], F32)
            s_c = P.slot("c")
            P.dma("sync", dw[:], self.dwT[l], s_c, True)
            P.dma("sync", cvp[:], self.cvp[l], s_c, True)
            P.dma("sync", idf[:], self.identf[:, :], s_c, True)
            diag = self.sb(es, "diag", [128, 8 * 31, 128], BF16)
            s_diag = P.slots_n(8, "diag")
            for j in range(8):
                eng = "vector" if (j % 2 == 0) else "gpsimd"
                P.op(eng, lambda e, j=j: e.tensor_tensor(
                    out=diag[:, j * 31:(j + 1) * 31, :], in0=idf[:].unsqueeze(1).to_broadcast([128, 31, 128]),
                    in1=dw[:, j, :].unsqueeze(2).to_broadcast([128, 31, 128]), op=ALU.mult),
                    reads=(s_c,), writes=(s_diag[j],))
            wco = self.sb(es, "wco", [128, 8, D], BF16)
            s_wco = P.slot("wco")
            self.load_w(wco, s_wco, self.w_conv_o[l], 8, D)
            uw = [self.sb(es, f"uw{i}", [128, 8, 544], BF16) for i in range(2)]
            s_uw = P.slots_n(2, "uw")
            g2 = [self.sb(es, f"g2{i}", [128, 8, 512], BF16) for i in range(2)]
            s_g2 = P.slots_n(2, "g2")
            cT = self.sb(es, "cT", [128, 8, 512], F32)
            s_cT = P.slot("cT")
            sqT = self.sb(es, "sqT", [128, 8, 512], F32)
            s_sqT = P.slot("sqT")
            aT = self.sb(es, "aT", [128, 8, 512], BF16)
            s_aT = P.slot("aT")
            stt = self.sb(es, "stt", [128, 4, 512], F32)
            s_stt = P.slot("stt")
            tmp = [self.sb(es, f"tmpc{i}", [128, 512], F32) for i in range(2)]
            s_tmp = P.slots_n(2, "tmp")
            mcs = [self.sb(es, f"mcs{i}", [128, 8, 512], BF16) for i in range(2)]
            s_mcs = P.slots_n(2, "mcs")
            pc = [self.ps(es, f"pc{i}", [128, 512]) for i in range(4)]
            s_pc = P.slots_n(4, "pc")
            pst = [self.ps(es, f"pst{i}", [128, 512]) for i in range(2)]
            s_pst = P.slots_n(2, "pst")
            po = [self.ps(es, f"po{i}", [128, 512]) for i in range(2)]
            s_po = P.slots_n(2, "po")
            c_pc = 0
            gcount = 0
            for gi, (r0, n, kind) in enumerate(GROUPS):
                if kind not in kinds:
                    continue
                b = gcount % 2
                gcount += 1
                c0 = ucol(r0)
                P.dma("sync", uw[b][:, :, 0:n + 30], self.UT[:, :, c0 - 15:c0 + n + 15].rearrange("j p r -> p j r"), s_uw[b], True)
                P.dma("sync", g2[b][:, :, 0:n], self.GT[8:16, :, r0:r0 + n].rearrange("j p r -> p j r"), s_g2[b], True)
                for j in range(8):
                    i = c_pc % 4
                    c_pc += 1

                    def mm(e, i=i, j=j, b=b):
                        for k in range(31):
                            r = e.matmul(pc[i][:, 0:n], diag[:, j * 31 + k, :], uw[b][:, j, k:k + n], start=(k == 0), stop=(k == 30))
                        return r
                    P.op("tensor", mm, reads=(s_diag[j], s_uw[b]), writes=(s_pc[i],))
                    P.op("scalar", lambda e, i=i, j=j: e.activation(out=cT[:, j, 0:n], in_=pc[i][:, 0:n], func=AF.Identity,
                                                                    bias=cvp[:, 0, j:j + 1], scale=1.0),
                         reads=(s_pc[i], s_c), writes=(s_cT,))
                    P.op("gpsimd", lambda e, j=j: e.tensor_tensor(out=sqT[:, j, 0:n], in0=cT[:, j, 0:n], in1=cT[:, j, 0:n], op=ALU.mult),
                         reads=(s_cT,), writes=(s_sqT,))

                def mm1(e):
                    for j in range(8):
                        r = e.matmul(pst[0][:, 0:n], self.onesf[:], cT[:, j, 0:n], start=(j == 0), stop=(j == 7))
                    return r

                def mm2(e):
                    for j in range(8):
                        r = e.matmul(pst[1][:, 0:n], self.onesf[:], sqT[:, j, 0:n], start=(j == 0), stop=(j == 7))
                    return r
                P.op("tensor", mm1, reads=(s_cT,), writes=(s_pst[0],))
                P.op("tensor", mm2, reads=(s_sqT,), writes=(s_pst[1],))
                P.op("vector", lambda e: e.tensor_scalar(out=stt[:, 0, 0:n], in0=pst[0][:, 0:n], scalar1=1.0 / D, scalar2=None, op0=ALU.mult),
                     reads=(s_pst[0],), writes=(s_stt,))
                P.op("vector", lambda e: e.tensor_tensor(out=stt[:, 2, 0:n], in0=stt[:, 0, 0:n], in1=stt[:, 0, 0:n], op=ALU.mult),
                     reads=(s_stt,), writes=(s_stt,))
                P.op("vector", lambda e: e.scalar_tensor_tensor(out=stt[:, 1, 0:n], in0=pst[1][:, 0:n], scalar=1.0 / D, in1=stt[:, 2, 0:n],
                                                                op0=ALU.mult, op1=ALU.subtract),
                     reads=(s_pst[1], s_stt), writes=(s_stt,))
                P.op("vector", lambda e: e.tensor_scalar(out=stt[:, 2, 0:n], in0=stt[:, 1, 0:n], scalar1=1.0, scalar2=EPS, op0=ALU.mult, op1=ALU.add),
                     reads=(s_stt,), writes=(s_stt,))
                P.op("scalar", lambda e: e.activation(out=stt[:, 3, 0:n], in_=stt[:, 2, 0:n], func=AF.Sqrt), reads=(s_stt,), writes=(s_stt,))
                P.op("vector", lambda e: e.reciprocal(out=stt[:, 1, 0:n], in_=stt[:, 3, 0:n]), reads=(s_stt,), writes=(s_stt,))
                for j in range(8):
                    k = j % 2
                    P.op("vector", lambda e, j=j, k=k: e.tensor_tensor(out=tmp[k][:, 0:n], in0=cT[:, j, 0:n], in1=stt[:, 0, 0:n], op=ALU.subtract),
                         reads=(s_cT, s_stt), writes=(s_tmp[k],))
                    P.op("gpsimd", lambda e, j=j, k=k: e.tensor_tensor(out=tmp[k][:, 0:n], in0=tmp[k][:, 0:n], in1=stt[:, 1, 0:n], op=ALU.mult),
                         reads=(s_tmp[k], s_stt), writes=(s_tmp[k],))
                    P.op("scalar", lambda e, j=j, k=k: e.activation(out=aT[:, j, 0:n], in_=tmp[k][:, 0:n], func=AF.Silu,
                                                                    bias=cvp[:, 2, j:j + 1], scale=cvp[:, 1, j:j + 1]),
                         reads=(s_tmp[k], s_c), writes=(s_aT,))
                for m in range(8):
                    i = m % 2

                    def mmo(e, i=i, m=m):
                        for k in range(8):
                            r = e.matmul(po[i][:, 0:n], wco[:, k, m * 128:(m + 1) * 128], aT[:, k, 0:n], start=(k == 0), stop=(k == 7))
                        return r
                    P.op("tensor", mmo, reads=(s_wco, s_aT), writes=(s_po[i],))
                    P.op("vector", lambda e, i=i, m=m, b=b: e.tensor_tensor(out=mcs[b][:, m, 0:n], in0=po[i][:, 0:n], in1=g2[b][:, m, 0:n], op=ALU.mult),
                         reads=(s_po[i], s_g2[b]), writes=(s_mcs[b],))
                P.dma("sync", self.MC[:, :, r0:r0 + n].rearrange("j p r -> p j r"), mcs[b][:, :, 0:n], s_mcs[b], False)
            P.flush()

    def phase_attn(self, l, kinds):
        P = self.P
        lam_init = 0.8 - 0.6 * math.exp(-0.3 * l)
        with ExitStack() as es:
            lv = self.sb(es, "lv", [128, 4, 64], F32)
            lsm = self.sb(es, "lsm", [128, 8], F32)
            s_l = P.slot("lam")
            for i in range(4):
                P.dma("sync", lv[:, i, :], self.lams[i][l].partition_broadcast(128), s_l, True)
            P.op("vector", lambda e: e.tensor_tensor(out=lv[:, 0, :], in0=lv[:, 0, :], in1=lv[:, 1, :], op=ALU.mult), reads=(s_l,), writes=(s_l,))
            P.op("vector", lambda e: e.tensor_tensor(out=lv[:, 2, :], in0=lv[:, 2, :], in1=lv[:, 3, :], op=ALU.mult), reads=(s_l,), writes=(s_l,))
            P.op("vector", lambda e: e.tensor_reduce(out=lsm[:, 0:1], in_=lv[:, 0, :], axis=AX.X, op=ALU.add), reads=(s_l,), writes=(s_l,))
            P.op("vector", lambda e: e.tensor_reduce(out=lsm[:, 1:2], in_=lv[:, 2, :], axis=AX.X, op=ALU.add), reads=(s_l,), writes=(s_l,))
            P.op("scalar", lambda e: e.activation(out=lsm[:, 2:4], in_=lsm[:, 0:2], func=AF.Exp), reads=(s_l,), writes=(s_l,))
            P.op("vector", lambda e: e.tensor_tensor(out=lsm[:, 4:5], in0=lsm[:, 3:4], in1=lsm[:, 2:3], op=ALU.subtract), reads=(s_l,), writes=(s_l,))
            P.op("vector", lambda e: e.tensor_scalar(out=lsm[:, 5:6], in0=lsm[:, 4:5], scalar1=-lam_init, scalar2=None, op0=ALU.add), reads=(s_l,), writes=(s_l,))
            nlam = lsm[:, 5:6]
            sgb = self.sb(es, "sgb", [128, 128], F32)
            P.dma("sync", sgb[:], self.subln_g[l].partition_broadcast(128), s_l, True)
            P.op("vector", lambda e: e.tensor_scalar(out=sgb[:], in0=sgb[:], scalar1=(1.0 - lam_init), scalar2=None, op0=ALU.mult), reads=(s_l,), writes=(s_l,))
            kt = [self.sb(es, f"kt{i}", [128, RALL], BF16) for i in range(2)]
            s_kt = P.slots_n(2, "kt")
            vx = [self.sb(es, f"vx{i}", [128, 34, 132], BF16) for i in range(2)]
            s_vx = P.slots_n(2, "vx")
            for i in range(2):
                P.op("gpsimd", lambda e, i=i: e.memset(vx[i][:, :, 128:132], 1.0), writes=(s_vx[i],))
            qz = [[self.sb(es, f"qz{c}_{i}", [128, 512], BF16) for i in range(2)] for c in range(2)]
            s_qt = P.slots_n(2, "qt")
            for c in range(2):
                for i in range(2):
                    P.op("gpsimd", lambda e, c=c, i=i: e.memset(qz[c][i][:], 0.0), writes=(s_qt[i],))
            NPT = 6
            pT = [self.sb(es, f"pT{i}", [128, 512], BF16) for i in range(NPT)]
            s_pT = P.slots_n(NPT, "pT")
            sps = [self.ps(es, f"sps{i}", [128, 512]) for i in range(3)]
            s_sps = P.slots_n(3, "sps")
            acc = self.ps(es, "acc", [128, 8, 256])
            s_acc = P.slots_n(2, "acc")
            tpo = self.ps(es, "tpo", [128, 4, 128], BF16)
            s_tpo = P.slot("tpo")
            rr = self.sb(es, "rr", [128, 8], F32)
            o = self.sb(es, "o", [128, 4, 128], F32)
            o2 = self.sb(es, "o2", [128, 4, 128], F32)
            sso = self.sb(es, "sso", [128, 3, 4], F32)
            on = self.sb(es, "on", [128, 4, 128], BF16)
            s_ep = P.slot("ep")
            s_on = P.slot("on")
            ost = [self.sb(es, f"ost{i}", [128, 512], BF16) for i in range(2)]
            s_ost = P.slots_n(2, "ost")
            c_sps = 0
            c_pt = 0
            c_q = 0
            pend_tail = []
            scale = 1.0 / 8.0
            def load_kv(h):
                hb = h % 2
                P.dma("sync", kt[hb][:], self.KT[h], s_kt[hb], True)
                P.dma("sync", vx[hb][:, :, 0:128], self.V[:, h * 128:(h + 1) * 128].rearrange("(c p) d -> p c d", p=128), s_vx[hb], True)

            def load_q(item, qb):
                h, (r0, n, kind) = item
                P.dma("sync", qz[0][qb][0:64, 0:n], self.QT[h, 0:64, r0:r0 + n], s_qt[qb], True)
                P.dma("sync", qz[1][qb][64:128, 0:n], self.QT[h, 64:128, r0:r0 + n], s_qt[qb], True)
            items = [(h, g) for h in range(NH) for g in GROUPS if g[2] in kinds]
            load_kv(0)
            load_q(items[0], 0)
            for ii, (h, (r0, n, kind)) in enumerate(items):
                hb = h % 2
                if True:
                    T = n // 128
                    nkc = 2 if kind == "ctx" else 34
                    qb = ii % 2
                    if ii + 1 < len(items):
                        load_q(items[ii + 1], (ii + 1) % 2)
                    if (ii == 0 or items[ii - 1][0] != h) and h + 1 < NH:
                        load_kv(h + 1)
                    for c in range(2):
                        LAG = 2
                        pend = []
                        for step in range(nkc + LAG):
                            if c == 0 and pend_tail and step == min(10, nkc + LAG - 1):
                                pend_tail.pop(0)()
                            if step < nkc:
                                kc = step
                                si = c_sps % 3
                                c_sps += 1
                                pi = c_pt % NPT
                                c_pt += 1
                                P.op("tensor", lambda e, si=si, kc=kc, c=c, hb=hb, qb=qb: e.matmul(
                                    sps[si][:, 0:n], kt[hb][:, kc * 128:(kc + 1) * 128],
                                    qz[c][qb][:, 0:n], start=True, stop=True),
                                    reads=(s_kt[hb], s_qt[qb]), writes=(s_sps[si],))
                                P.op("scalar", lambda e, si=si, pi=pi: e.activation(
                                    out=pT[pi][:, 0:n], in_=sps[si][:, 0:n], func=AF.Exp, scale=scale),
                                    reads=(s_sps[si],), writes=(s_pT[pi],))
                                pend.append((kc, pi))
                            if step >= LAG:
                                kc, pi = pend.pop(0)

                                def pv(e, kc=kc, pi=pi, c=c, hb=hb):
                                    for t in range(T):
                                        r = e.matmul(acc[:, c * 4 + t, 0:129], pT[pi][:, t * 128:(t + 1) * 128], vx[hb][:, kc, 0:129],
                                                     start=(kc == 0 and t % 2 == 0), stop=(kc == nkc - 1), skip_group_check=True)
                                    return r
                                P.op("tensor", pv, reads=(s_pT[pi], s_vx[hb]), writes=(s_acc[c],))
                    acc4 = acc[:].rearrange("p (c t) w -> p c t w", c=2)
                    P.op("vector", lambda e: e.reciprocal(out=rr[:].rearrange("p (c t) -> p c t", c=2)[:, :, 0:T],
                                                          in_=acc4[:, :, 0:T, 128]), reads=(s_acc[0], s_acc[1]), writes=(s_ep,))
                    P.op("vector", lambda e: e.tensor_scalar(out=rr[:, 4:4 + T], in0=rr[:, 4:4 + T], scalar1=nlam, scalar2=None, op0=ALU.mult),
                         reads=(s_ep, s_l), writes=(s_ep,))
                    P.op("vector", lambda e: e.tensor_tensor(out=o[:, 0:T, :], in0=acc4[:, 0, 0:T, 0:128],
                                                             in1=rr[:, 0:T].unsqueeze(2).to_broadcast([128, T, 128]), op=ALU.mult),
                         reads=(s_acc[0], s_ep), writes=(s_ep,))
                    P.op("vector", lambda e: e.tensor_tensor(out=o2[:, 0:T, :], in0=acc4[:, 1, 0:T, 0:128],
                                                             in1=rr[:, 4:4 + T].unsqueeze(2).to_broadcast([128, T, 128]), op=ALU.mult),
                         reads=(s_acc[1], s_ep), writes=(s_ep,))
                    P.op("gpsimd", lambda e: e.tensor_tensor(out=o[:, 0:T, :], in0=o[:, 0:T, :], in1=o2[:, 0:T, :], op=ALU.add),
                         reads=(s_ep,), writes=(s_ep,))
                    P.op("gpsimd", lambda e: e.tensor_tensor(out=o2[:, 0:T, :], in0=o[:, 0:T, :], in1=o[:, 0:T, :], op=ALU.mult),
                         reads=(s_ep,), writes=(s_ep,))
                    P.op("vector", lambda e: e.tensor_reduce(out=sso[:, 0, 0:T], in_=o2[:, 0:T, :], axis=AX.X, op=ALU.add),
                         reads=(s_ep,), writes=(s_ep,))
                    self.rsqrt(sso[:, 0, 0:T], sso[:, 2, 0:T], sso[:, 1, 0:T], 1.0 / 128, (s_ep,), (s_ep,), s_ep, T, mode="pool")
                    P.op("vector", lambda e: e.tensor_tensor(out=o[:, 0:T, :], in0=o[:, 0:T, :],
                                                             in1=sso[:, 2, 0:T].unsqueeze(2).to_broadcast([128, T, 128]), op=ALU.mult),
                         reads=(s_ep,), writes=(s_ep,))
                    P.op("vector", lambda e: e.tensor_tensor(out=on[:, 0:T, :], in0=o[:, 0:T, :],
                                                             in1=sgb[:].unsqueeze(1).to_broadcast([128, T, 128]), op=ALU.mult),
                         reads=(s_ep, s_l), writes=(s_on,))

                    def tail(T=T, n=n, ob=qb, h=h, r0=r0):
                        def tr(e):
                            for t in range(T):
                                r = e.transpose(tpo[:, t, :], on[:, t, :], self.ident[:])
                            return r
                        P.op("tensor", tr, reads=(s_on,), writes=(s_tpo,))
                        P.op("vector", lambda e: e.tensor_copy(out=ost[ob][:, 0:n], in_=tpo[:].rearrange("p t q -> p (t q)")[:, 0:n]),
                             reads=(s_tpo,), writes=(s_ost[ob],))
                        P.dma("sync", self.OT[h, :, r0:r0 + n], ost[ob][:, 0:n], s_ost[ob], False)
                    pend_tail.append(tail)
            while pend_tail:
                pend_tail.pop(0)()
            P.flush()

    def phase_merge(self, l, xsrc, xdst, kinds):
        P = self.P
        with ExitStack() as es:
            wao = self.sb(es, "wao", [128, 8, D], BF16)
            wout = self.sb(es, "wout", [128, 8, D], BF16)
            s_waop = self.load_w(wao, None, self.w_attn_o[l], 8, D)
            s_woutp = self.load_w(wout, None, self.w_out[l], 8, D)
            ml2 = {}
            for who, nm in ((0, "lat"), (1, "ctx")):
                ml2[nm] = self.load_mod_tile(es, l, 2, who, f"G_{nm}")
            oT = [self.sb(es, f"oT{i}", [128, 8, 512], BF16) for i in range(2)]
            g1 = [self.sb(es, f"g1{i}", [128, 8, 512], BF16) for i in range(2)]
            mc = [self.sb(es, f"mc{i}", [128, 8, 512], BF16) for i in range(2)]
            s_oT, s_g1, s_mc = P.slots_n(2), P.slots_n(2), P.slots_n(2)
            mg = [self.sb(es, f"mg{i}", [128, 8, 512], BF16) for i in range(2)]
            s_mg = P.slots_n(2)
            tmp = [self.sb(es, f"tm{i}", [128, 512], F32) for i in range(2)]
            s_tmp = P.slots_n(2)
            pa = [self.ps(es, f"pa{i}", [128, 512]) for i in range(2)]
            s_pa = P.slots_n(2)
            po = [self.ps(es, f"pox{i}", [128, 1024]) for i in range(2)]
            s_po = P.slots_n(2)
            xt = [self.sb(es, f"xt{i}", [128, D], F32) for i in range(2)]
            s_xt = P.slots_n(2)
            xo = [self.sb(es, f"xo{i}", [128, D], F32) for i in range(2)]
            s_xo = P.slots_n(2)
            gc = 0
            tcd = {"tc": 0}
            pend_w = []
            for gi, (r0, n, kind) in enumerate(GROUPS):
                if kind not in kinds:
                    continue
                b = gc % 2
                gc += 1
                G, sG = ml2["ctx" if kind == "ctx" else "lat"]
                P.dma("sync", oT[b][:, :, 0:n], self.OT[:, :, r0:r0 + n].rearrange("j p r -> p j r"), s_oT[b], True)
                P.dma("sync", g1[b][:, :, 0:n], self.GT[0:8, :, r0:r0 + n].rearrange("j p r -> p j r"), s_g1[b], True)
                P.dma("sync", mc[b][:, :, 0:n], self.MC[:, :, r0:r0 + n].rearrange("j p r -> p j r"), s_mc[b], True)
                for m in range(8):
                    i = m % 2

                    def mm(e, i=i, m=m, b=b):
                        for k in range(8):
                            r = e.matmul(pa[i][:, 0:n], wao[:, k, m * 128:(m + 1) * 128], oT[b][:, k, 0:n], start=(k == 0), stop=(k == 7))
                        return r
                    P.op("tensor", mm, reads=(s_waop[m // 4], s_oT[b]), writes=(s_pa[i],))
                    P.op("vector", lambda e, i=i, m=m, b=b: e.tensor_tensor(out=tmp[i][:, 0:n], in0=pa[i][:, 0:n], in1=g1[b][:, m, 0:n], op=ALU.mult),
                         reads=(s_pa[i], s_g1[b]), writes=(s_tmp[i],))
                    P.op("gpsimd", lambda e, i=i, m=m, b=b: e.tensor_tensor(out=mg[b][:, m, 0:n], in0=tmp[i][:, 0:n], in1=mc[b][:, m, 0:n], op=ALU.add),
                         reads=(s_tmp[i], s_mc[b]), writes=(s_mg[b],))
                def wout_part(r0=r0, n=n, b=b, G=G, sG=sG):
                    for t in range(n // 128):
                        i = tcd["tc"] % 2
                        tcd["tc"] += 1
                        P.dma("sync", xt[i][:], xsrc[r0 + t * 128:r0 + (t + 1) * 128, :], s_xt[i], True)

                        def mm(e, i=i, t=t, b=b):
                            for half in range(2):
                                for k in range(8):
                                    r = e.matmul(po[i][:, half * 512:(half + 1) * 512], mg[b][:, k, t * 128:(t + 1) * 128],
                                                 wout[:, k, half * 512:(half + 1) * 512], start=(k == 0), stop=(k == 7))
                            return r
                        P.op("tensor", mm, reads=(s_woutp[0], s_woutp[1], s_mg[b]), writes=(s_po[i],))
                        P.op("vector", lambda e, i=i: e.tensor_tensor(out=xo[i][:], in0=po[i][:], in1=G[:], op=ALU.mult),
                             reads=(s_po[i], sG), writes=(s_xo[i],))
                        P.op("gpsimd", lambda e, i=i: e.tensor_tensor(out=xo[i][:], in0=xo[i][:], in1=xt[i][:], op=ALU.add),
                             reads=(s_xt[i], s_xo[i]), writes=(s_xo[i],))
                        P.dma("sync", xdst[r0 + t * 128:r0 + (t + 1) * 128, :], xo[i][:], s_xo[i], False)
                if pend_w:
                    pend_w.pop(0)()
                pend_w.append(wout_part)
            while pend_w:
                pend_w.pop(0)()
            P.flush()

    def phase_ffn(self, l, groups, xsrc, xdst_fn, experts, router):
        P = self.P
        R = sum(n for _, n, _ in groups)
        NT = R // 128
        with ExitStack() as es:
            pl = self.ps(es, "pl", [128, NT, NE]) if router is not None else None
            es_tp = ExitStack()
            whos = tuple(nm for nm in ("lat", "ctx") if any((k == "ctx") == (nm == "ctx") for _, _, k in groups))
            early = router is None
            st = self.setup_norm(es, l, 1, whos=whos, ntp=(1 if early else 2), es_tp=(None if early else es_tp))
            ml5 = {}
            for who, nm in ((0, "lat"), (1, "ctx")):
                if nm in whos:
                    ml5[nm] = self.load_mod_tile(es, l, 5, who, f"G_{nm}")
            hT = [self.sb(es, f"h2T{c}", [128, 8, 512], BF16) for c in range((R + 511) // 512)]
            s_hTt = P.slots_n(NT, "h2T")
            accs = self.sb(es, "accs", [128, NT, D], F32)
            s_acc = P.slots_n(NT, "acc")
            col = 0
            tile_rows = []
            for (r0, n, kind) in groups:
                for tt in range(n // 128):
                    gt = col // 128 + tt
                    self.emit_hT(st, xsrc, r0, n, kind, hT[gt // 4], s_hTt[gt], col0=(gt % 4) * 128 - tt * 128, tiles=[tt])
                    tile_rows.append((r0 + tt * 128, kind))
                col += n
            gates = None
            if router is not None:
                wr = self.sb(es, "wr", [128, 8, NE], BF16)
                s_wr = P.slot("wr")
                P.dma("gpsimd", wr[:], router.rearrange("(k p) n -> p k n", p=128), s_wr, True)
                gates = self.sb(es, "gates", [128, NT, NE], F32)
                s_gates = P.slot("gates")
                lg = self.sb(es, "lg", [128, NT, NE], F32)
                mx = self.sb(es, "mx", [128, NT, 8], F32)
                sm = self.sb(es, "smx", [128, NT, 2], F32)
                s_pl = P.slot("pl")
                s_lg = P.slot("lg")
                for t in range(NT):
                    def mm(e, t=t):
                        for k in range(8):
                            r = e.matmul(pl[:, t, :], hT[t // 4][:, k, (t % 4) * 128:(t % 4 + 1) * 128], wr[:, k, :], start=(k == 0), stop=(k == 7))
                        return r
                    P.op("tensor", mm, reads=(s_hTt[t], s_wr), writes=(s_pl,))
                P.op("vector", lambda e: e.tensor_copy(out=lg[:], in_=pl[:]), reads=(s_pl,), writes=(s_lg,))
                for t in range(NT):
                    P.op("vector", lambda e, t=t: e.max(out=mx[:, t, :], in_=lg[:, t, :]), reads=(s_lg,), writes=(s_lg,))
                P.op("vector", lambda e: e.tensor_tensor(out=gates[:], in0=lg[:], in1=mx[:, :, 1:2].to_broadcast([128, NT, NE]), op=ALU.is_ge),
                     reads=(s_lg,), writes=(s_gates,))
                P.op("vector", lambda e: e.tensor_tensor(out=lg[:], in0=lg[:], in1=mx[:, :, 0:1].to_broadcast([128, NT, NE]), op=ALU.subtract),
                     reads=(s_lg,), writes=(s_lg,))
                P.op("scalar", lambda e: e.activation(out=lg[:], in_=lg[:], func=AF.Exp), reads=(s_lg,), writes=(s_lg,))
                P.op("vector", lambda e: e.tensor_tensor(out=gates[:], in0=gates[:], in1=lg[:], op=ALU.mult), reads=(s_lg, s_gates), writes=(s_gates,))
                P.op("vector", lambda e: e.tensor_reduce(out=sm[:, :, 0], in_=gates[:], axis=AX.X, op=ALU.add), reads=(s_gates,), writes=(s_lg,))
                P.op("vector", lambda e: e.reciprocal(out=sm[:, :, 1], in_=sm[:, :, 0]), reads=(s_lg,), writes=(s_lg,))
                P.op("vector", lambda e: e.tensor_tensor(out=gates[:], in0=gates[:], in1=sm[:, :, 1:2].to_broadcast([128, NT, NE]), op=ALU.mult),
                     reads=(s_lg, s_gates), writes=(s_gates,))
            es_tp.close()
            NPG = 3
            wg = [self.sb(es, f"wg{i}", [128, 8, 512], BF16) for i in range(2)]
            wu = [self.sb(es, f"wu{i}", [128, 8, 512], BF16) for i in range(2)]
            wd = [self.sb(es, f"wd{i}", [128, 4, D], BF16) for i in range(2)]
            s_wg, s_wu, s_wd = P.slots_n(2), P.slots_n(2), P.slots_n(2)
            pgu = [self.ps(es, f"pgu{i}", [128, 512]) for i in range(NPG)]
            s_pgu = P.slots_n(NPG)
            pd = [self.ps(es, f"pd{i}", [128, 1024]) for i in range(2)]
            s_pd = P.slots_n(2)
            sgl = [self.sb(es, f"sgl{i}", [128, 512], F32) for i in range(2)]
            s_sgl = P.slots_n(2)
            act = [self.sb(es, f"act{i}", [128, 4, 512], BF16) for i in range(2)]
            s_act = P.slots_n(2)
            sets = []
            for ei, (wga, wua, wda, F) in enumerate(experts):
                nch = F // 128
                for c0 in range(0, nch, 4):
                    sets.append((ei, c0, min(4, nch - c0)))
            c_gu = 0
            c_act = 0
            c_pd = 0
            first = [True] * NT
            rch = [(c, min(512, R - c)) for c in range(0, R, 512)]

            def load_set(si):
                ei, c0, nc_ = sets[si]
                wga, wua, wda, F = experts[ei]
                b = si % 2
                P.dma("gpsimd", wg[b][:, :, 0:nc_ * 128], wga.rearrange("(k p) n -> p k n", p=128)[:, :, c0 * 128:(c0 + nc_) * 128], s_wg[b], True)
                P.dma("gpsimd", wu[b][:, :, 0:nc_ * 128], wua.rearrange("(k p) n -> p k n", p=128)[:, :, c0 * 128:(c0 + nc_) * 128], s_wu[b], True)
                P.dma("gpsimd", wd[b][:, 0:nc_, :], wda[c0 * 128:(c0 + nc_) * 128, :].rearrange("(k p) n -> p k n", p=128), s_wd[b], True)
            load_set(0)
            fcnt = {"c": 0}

            def final_tile(t):
                row, kind = tile_rows[t]
                G, sG = ml5["ctx" if kind == "ctx" else "lat"]
                i = fcnt["c"] % 2
                fcnt["c"] += 1
                xt, s_xt = st["x"][i], st["s_x"][i]
                P.dma("sync", xt[:], xsrc[row:row + 128, :], s_xt, True)
                P.op("gpsimd", lambda e: e.tensor_tensor(out=accs[:, t, :], in0=accs[:, t, :], in1=G[:], op=ALU.mult),
                     reads=(s_acc[t], sG), writes=(s_acc[t],))
                P.op("vector", lambda e: e.tensor_tensor(out=accs[:, t, :], in0=accs[:, t, :], in1=xt[:], op=ALU.add),
                     reads=(s_acc[t], s_xt), writes=(s_acc[t],))
                P.dma("sync", xdst_fn(row), accs[:, t, :], s_acc[t], False)
            cpd = {"c": 0}
            pend_d = []
            for si, (ei, c0, nc_) in enumerate(sets):
                b = si % 2
                while pend_d:
                    pend_d.pop(0)()
                if si + 1 < len(sets):
                    load_set(si + 1)
                for (rc0, rn) in rch:
                    ab = c_act % 2
                    c_act += 1
                    for j in range(nc_):
                        ig = c_gu % NPG
                        iu = (c_gu + 1) % NPG
                        c_gu += 2

                        def mmg(e, j=j, ig=ig, b=b):
                            for k in range(8):
                                r = e.matmul(pgu[ig][:, 0:rn], wg[b][:, k, j * 128:(j + 1) * 128], hT[rc0 // 512][:, k, 0:rn], start=(k == 0), stop=(k == 7))
                            return r

                        def mmu(e, j=j, iu=iu, b=b):
                            for k in range(8):
                                r = e.matmul(pgu[iu][:, 0:rn], wu[b][:, k, j * 128:(j + 1) * 128], hT[rc0 // 512][:, k, 0:rn], start=(k == 0), stop=(k == 7))
                            return r
                        hsl = tuple(s_hTt[rc0 // 128:(rc0 + rn) // 128]) if early else tuple(s_hTt)
                        P.op("tensor", mmg, reads=(s_wg[b],) + hsl, writes=(s_pgu[ig],))
                        P.op("tensor", mmu, reads=(s_wu[b],) + hsl, writes=(s_pgu[iu],))
                        k2 = j % 2
                        P.op("scalar", lambda e, ig=ig, k2=k2: e.activation(out=sgl[k2][:, 0:rn], in_=pgu[ig][:, 0:rn], func=AF.Silu),
                             reads=(s_pgu[ig],), writes=(s_sgl[k2],))
                        P.op("vector", lambda e, iu=iu, k2=k2, j=j, ab=ab: e.tensor_tensor(
                            out=act[ab][:, j, 0:rn], in0=pgu[iu][:, 0:rn], in1=sgl[k2][:, 0:rn], op=ALU.mult),
                            reads=(s_pgu[iu], s_sgl[k2]), writes=(s_act[ab],))
                    def down_part(rc0=rc0, rn=rn, ab=ab, b=b, ei=ei, nc_=nc_, last_set=(si == len(sets) - 1)):
                        for tt in range(rn // 128):
                            t = rc0 // 128 + tt
                            ip = cpd["c"] % 2
                            cpd["c"] += 1

                            def mmd(e, tt=tt, ip=ip, ab=ab, b=b):
                                for half in range(2):
                                    for j in range(nc_):
                                        r = e.matmul(pd[ip][:, half * 512:(half + 1) * 512], act[ab][:, j, tt * 128:(tt + 1) * 128],
                                                     wd[b][:, j, half * 512:(half + 1) * 512], start=(j == 0), stop=(j == nc_ - 1))
                                return r
                            P.op("tensor", mmd, reads=(s_wd[b], s_act[ab]), writes=(s_pd[ip],))
                            if gates is not None:
                                gsc = gates[:, t, ei:ei + 1]
                                rd = (s_pd[ip], s_gates)
                                if first[t]:
                                    P.op("vector", lambda e, t=t, ip=ip, gsc=gsc: e.tensor_scalar(
                                        out=accs[:, t, :], in0=pd[ip][:], scalar1=gsc, scalar2=None, op0=ALU.mult), reads=rd, writes=(s_acc[t],))
                                else:
                                    P.op("vector", lambda e, t=t, ip=ip, gsc=gsc: e.scalar_tensor_tensor(
                                        out=accs[:, t, :], in0=pd[ip][:], scalar=gsc, in1=accs[:, t, :], op0=ALU.mult, op1=ALU.add),
                                        reads=rd + (s_acc[t],), writes=(s_acc[t],))
                            else:
                                if first[t]:
                                    P.op("vector", lambda e, t=t, ip=ip: e.tensor_copy(out=accs[:, t, :], in_=pd[ip][:]), reads=(s_pd[ip],), writes=(s_acc[t],))
                                else:
                                    P.op("vector", lambda e, t=t, ip=ip: e.tensor_tensor(out=accs[:, t, :], in0=pd[ip][:], in1=accs[:, t, :], op=ALU.add),
                                         reads=(s_pd[ip], s_acc[t]), writes=(s_acc[t],))
                            first[t] = False
                            if last_set:
                                final_tile(t)
                    if pend_d:
                        pend_d.pop(0)()
                    pend_d.append(down_part)
            while pend_d:
                pend_d.pop(0)()
            P.flush()

    def build(self):
        upto = self.upto
        stage = [0]

        def done():
            stage[0] += 1
            return upto is not None and stage[0] >= upto
        self.phase_const()
        ALLK = ("ctx", "own", "oth")
        self.phase_mod((0, 1))
        if done(): return
        self.phase_kvq(0, self.x0, ALLK)
        if done(): return
        self.phase_glu(0, self.x0, set(range(9)), ALLK, do_flush=False)
        self.P.drain("sync")
        self.P.drain("vector")
        self.phase_fix()
        if done(): return
        self.phase_conv(0, ALLK)
        if done(): return
        self.phase_attn(0, ALLK)
        if done(): return
        self.phase_merge(0, self.x0, self.XS, ALLK)
        if done(): return
        dense = [(self.w_ff_gate[0], self.w_ff_up[0], self.w_ff_down[0], DFF)]
        xs_fn = lambda r: self.XS[r:r + 128, :]
        self.phase_ffn(0, GROUPS[0:3], self.XS, xs_fn, dense, None)
        self.phase_ffn(0, GROUPS[3:6], self.XS, xs_fn, dense, None)
        self.phase_ffn(0, GROUPS[6:9], self.XS, xs_fn, dense, None)
        if done(): return
        self.phase_kvq(1, self.XS, ("own",))
        self.phase_glu(1, self.XS, {1, 2, 3, 4, 5, 8}, ("own",), do_flush=False)
        self.P.drain("sync")
        self.P.drain("vector")
        self.phase_fix()
        self.phase_conv(1, ("own",))
        if done(): return
        self.phase_attn(1, ("own",))
        self.phase_merge(1, self.XS, self.XS, ("own",))
        if done(): return
        experts = [(self.w_exp_gate[0, e], self.w_exp_up[0, e], self.w_exp_down[0, e], DFE) for e in range(NE)]
        out_fn = lambda r: self.out[r - CTX:r - CTX + 128, :]
        self.phase_ffn(1, GROUPS[1:5], self.XS, out_fn, experts, self.w_router[0])


def build_nc(upto=None):
    nc = bass.Bass("TRN2", target_bir_lowering=False)
    b = Builder(nc, upto)
    for nm in ("kvq", "glu", "conv", "attn", "merge", "ffn", "mod"):
        if nm in SKIP:
            setattr(b, "phase_" + nm, lambda *a, **k: None)
    b.build()
    return nc


def rope_tables():
    S, GW, RA = 4096, 64, 32
    t = np.arange(S)
    row = (t // GW).astype(np.float32)
    col = (t % GW).astype(np.float32)
    inv = (10000.0 ** (-np.arange(0, RA, 2, dtype=np.float32) / RA)).astype(np.float32)
    ar = row[:, None] * inv
    ac = col[:, None] * inv
    ang = np.concatenate([ar, ar, ac, ac], axis=-1).astype(np.float32)
    cos = np.cos(ang).astype(np.float32)
    sin = np.sin(ang).astype(np.float32)
    ss = sin.copy()
    ss4 = ss.reshape(S, 2, 2, 16)
    ss4[:, :, 0, :] *= -1.0
    return cos, ss4.reshape(S, 64)


def make_in_maps(inputs):
    f = lambda a: np.ascontiguousarray(np.asarray(a, dtype=np.float32))
    x = f(inputs["x"]); c = f(inputs["c"]); ctx = f(inputs["ctx"]); c_ctx = f(inputs["c_ctx"])
    cos, ss = rope_tables()
    shared = {}
    for k in ("w_mod", "b_mod", "g_mix", "w_in", "q_norm_g", "k_norm_g", "lambda_q1", "lambda_k1", "lambda_q2",
              "lambda_k2", "subln_g", "w_attn_o", "w_conv_o", "w_out", "g_ffn", "w_ff_gate", "w_ff_up", "w_ff_down",
              "w_router", "w_exp_gate", "w_exp_up", "w_exp_down"):
        shared[k] = f(inputs[k])
    dw = f(inputs["dw_weight"])
    shared["dwT"] = np.ascontiguousarray(dw.reshape(2, 31, 8, 128).transpose(0, 3, 2, 1))
    cv = np.stack([f(inputs["dw_bias"]), f(inputs["conv_ln_g"]), f(inputs["conv_ln_b"])], axis=1)
    shared["cvp"] = np.ascontiguousarray(cv.reshape(2, 3, 8, 128).transpose(0, 3, 1, 2))
    shared["identf"] = np.eye(128, dtype=np.float32)
    maps = []
    for core in range(8):
        b, h = core // 2, core % 2
        own = slice(h * HALF, (h + 1) * HALF)
        oth = slice((1 - h) * HALF, (2 - h) * HALF)
        m = dict(shared)
        m["x0"] = np.ascontiguousarray(np.concatenate([ctx[b], x[b, own], x[b, oth]], axis=0))
        cvec = np.stack([c[b].reshape(8, 128).T, c_ctx.reshape(8, 128).T], axis=2).reshape(128, 16)
        m["cvec"] = np.ascontiguousarray(cvec)
        rp = np.zeros((RALL, 2, 64), np.float32)
        rp[:CTX, 0, :] = 1.0
        rp[CTX:CTX + HALF, 0, :] = cos[own]; rp[CTX:CTX + HALF, 1, :] = ss[own]
        rp[CTX + HALF:, 0, :] = cos[oth]; rp[CTX + HALF:, 1, :] = ss[oth]
        m["rope"] = rp
        mk = np.zeros((128, 2), np.float32)
        mk[:, 0] = float(h); mk[:, 1] = float(1 - h)
        m["mask"] = mk
        maps.append(m)
    return maps


def kernel(**inputs):
    maps = make_in_maps(inputs)
    nc = build_nc()
    res = run_bass_kernel_spmd(nc, maps, core_ids=list(range(8)))
    out = np.zeros((4, 4096, D), np.float32)
    for core in range(8):
        b, h = core // 2, core % 2
        out[b, h * HALF:(h + 1) * HALF] = res.results[core]["out"]
    return out
```

```python
import math
import types
from contextlib import ExitStack
import numpy as np
import concourse.bass as bass
import concourse.mybir as mybir
from concourse.bass_utils import run_bass_kernel_spmd

F32 = mybir.dt.float32
BF16 = mybir.dt.bfloat16
AF = mybir.ActivationFunctionType
ALU = mybir.AluOpType
AX = mybir.AxisListType

D = 1024
CTX = 256
HALF = 2048
RALL = CTX + 2 * HALF
NH = 8
INW = 7168
DFF = 2816
DFE = 3584
NE = 8
EPS = 1e-6
UT_W = 4448
DEBUG = False
SKIP = set()


def ucol(r):
    if r < CTX:
        return 16 + r
    if r < CTX + HALF:
        return 288 + 16 + (r - CTX)
    return 2368 + 16 + (r - CTX - HALF)


GROUPS = [(0, 256, "ctx")] + [(CTX + 512 * i, 512, "own") for i in range(4)] + \
         [(CTX + HALF + 512 * i, 512, "oth") for i in range(4)]


def freeze(fn):
    if fn.__closure__ is None:
        return fn
    cells = []
    for c in fn.__closure__:
        try:
            cells.append(types.CellType(c.cell_contents))
        except ValueError:
            cells.append(c)
    return types.FunctionType(fn.__code__, fn.__globals__, fn.__name__, fn.__defaults__, tuple(cells))


class Sem:
    def __init__(self, h):
        self.h = h
        self.val = 0


class Slot:
    def __init__(self, name):
        self.name = name
        self.w = {}
        self.r = {}
        self.ld = None
        self.st = None


class Prog:
    CE = ("tensor", "vector", "scalar", "gpsimd")
    ENGS = ("tensor", "vector", "scalar", "gpsimd", "sync")

    def __init__(self, nc, n_hw=40, n_sw=10):
        self.nc = nc
        self.psets = [{e: Sem(nc.alloc_semaphore(name=f"pg{i}_{e}")) for e in self.CE} for i in range(2)]
        self.cur = 1
        self.pools = {"sync": [Sem(nc.alloc_semaphore(name=f"dh_{i}")) for i in range(n_hw)],
                      "gpsimd": [Sem(nc.alloc_semaphore(name=f"ds_{i}")) for i in range(n_sw)]}
        self.rr = {"sync": 0, "gpsimd": 0}
        self.reset()

    def reset(self):
        self.q = {e: [] for e in self.ENGS}
        self.waited = {e: {} for e in self.ENGS}
        self.cur = 1 - self.cur
        self.psem = self.psets[self.cur]
        for sm in self.psem.values():
            sm.val = 0
        self.slots = []

    def drain(self, eng="sync"):
        for pool in self.pools.values():
            for sem in pool:
                if sem.val > 0:
                    self._wait(eng, (sem, sem.val))

    def slot(self, name="s"):
        sl = Slot(name)
        self.slots.append(sl)
        return sl

    def slots_n(self, n, name="s"):
        return [self.slot(f"{name}{i}") for i in range(n)]

    def _wait(self, eng, tk):
        sem, val = tk
        if eng == "tensor" and sem is self.psem["tensor"]:
            return
        if self.waited[eng].get(id(sem), 0) >= val:
            return
        self.waited[eng][id(sem)] = val
        self.q[eng].append(lambda e, sem=sem, val=val: e.wait_ge(sem.h, val))

    def _deps(self, eng, reads, writes):
        for sl in reads:
            for tk in sl.w.values():
                self._wait(eng, tk)
        for sl in writes:
            for tk in sl.w.values():
                self._wait(eng, tk)
            for tk in sl.r.values():
                self._wait(eng, tk)

    def _commit(self, tk, reads, writes):
        key = id(tk[0])
        for sl in reads:
            sl.r[key] = tk
        for sl in writes:
            if sl.r:
                sl.w = {}
                sl.r = {}
            sl.w[key] = tk

    def op(self, eng, fn, reads=(), writes=()):
        self._deps(eng, reads, writes)
        fn = freeze(fn)
        sem = self.psem[eng]
        sem.val += 1
        tk = (sem, sem.val)
        self.q[eng].append(lambda e, fn=fn, sem=sem: fn(e).then_inc(sem.h, 1))
        self._commit(tk, reads, writes)
        return tk

    def dma(self, eng, out, in_, slot, load):
        if load:
            self._deps(eng, (), (slot,))
        else:
            self._deps(eng, (slot,), ())
        pool = self.pools[eng]
        sem = pool[self.rr[eng] % len(pool)]
        self.rr[eng] += 1
        if sem.val > 0:
            self._wait(eng, (sem, sem.val))
        sem.val += 16
        tk = (sem, sem.val)
        self.q[eng].append(
            lambda e, out=out, in_=in_, sem=sem: e.dma_start(out=out, in_=in_).then_inc(sem.h, 16))
        if load:
            self._commit(tk, (), (slot,))
        else:
            self._commit(tk, (slot,), ())
        return tk

    def flush(self):
        for pool in self.pools.values():
            for sem in pool:
                if sem.val > 0:
                    self._wait("sync", (sem, sem.val))
        nc = self.nc
        others = list(self.psets[1 - self.cur].values())
        self.q["vector"].insert(0, lambda e: [e.sem_clear(sm.h) for sm in others])
        with nc.Block() as block:
            for eng in self.ENGS:
                items = self.q[eng]

                def body(e, items=items):
                    for it in items:
                        it(e)
                getattr(block, eng)(body)
        self.reset()


class Builder:
    def __init__(self, nc, upto=None):
        self.nc = nc
        self.upto = upto
        self.P = Prog(nc)
        dt_in = lambda name, shape, dt=F32: nc.dram_tensor(name, list(shape), dt, kind="ExternalInput").ap()
        kind_s = "ExternalOutput"
        dt_sc = lambda name, shape, dt: nc.dram_tensor(name, list(shape), dt, kind=kind_s).ap()
        self.x0 = dt_in("x0", [RALL, D])
        self.cvec = dt_in("cvec", [128, 16])
        self.rope = dt_in("rope", [RALL, 2, 64])
        self.mask = dt_in("mask", [128, 2])
        self.identf = dt_in("identf", [128, 128])
        self.w_mod = dt_in("w_mod", [2, D, 6 * D])
        self.b_mod = dt_in("b_mod", [2, 6 * D])
        self.g_mix = dt_in("g_mix", [2, D])
        self.w_in = dt_in("w_in", [2, D, INW])
        self.q_norm_g = dt_in("q_norm_g", [2, 64])
        self.k_norm_g = dt_in("k_norm_g", [2, 64])
        self.lams = [dt_in(n, [2, 64]) for n in ("lambda_q1", "lambda_k1", "lambda_q2", "lambda_k2")]
        self.subln_g = dt_in("subln_g", [2, 128])
        self.w_attn_o = dt_in("w_attn_o", [2, D, D])
        self.dwT = dt_in("dwT", [2, 128, 8, 31])
        self.cvp = dt_in("cvp", [2, 128, 3, 8])
        self.w_conv_o = dt_in("w_conv_o", [2, D, D])
        self.w_out = dt_in("w_out", [2, D, D])
        self.g_ffn = dt_in("g_ffn", [2, D])
        self.w_ff_gate = dt_in("w_ff_gate", [1, D, DFF])
        self.w_ff_up = dt_in("w_ff_up", [1, D, DFF])
        self.w_ff_down = dt_in("w_ff_down", [1, DFF, D])
        self.w_router = dt_in("w_router", [1, D, NE])
        self.w_exp_gate = dt_in("w_exp_gate", [1, NE, D, DFE])
        self.w_exp_up = dt_in("w_exp_up", [1, NE, D, DFE])
        self.w_exp_down = dt_in("w_exp_down", [1, NE, DFE, D])
        self.out = nc.dram_tensor("out", [HALF, D], F32, kind="ExternalOutput").ap()
        self.XS = dt_sc("XS", [RALL, D], F32)
        self.MODV = dt_sc("MODV", [2, 2, 6 * D], F32)
        self.KT = dt_sc("KT", [NH, 128, RALL], BF16)
        self.QT = dt_sc("QT", [NH, 128, RALL], BF16)
        self.V = dt_sc("V", [RALL, D], BF16)
        self.UT = dt_sc("UT", [8, 128, UT_W], BF16)
        self.GT = dt_sc("GT", [16, 128, RALL], BF16)
        self.MC = dt_sc("MC", [8, 128, RALL], BF16)
        self.OT = dt_sc("OT", [NH, 128, RALL], BF16)
        self.ident = nc.alloc_sbuf_tensor("ident", [128, 128], BF16)
        self.onesf = nc.alloc_sbuf_tensor("onesf", [128, 128], F32)
        self.nhalf = nc.alloc_sbuf_tensor("nhalf", [128, 64], F32)
        self.maskt = nc.alloc_sbuf_tensor("maskt", [128, 2], F32)

    def sb(self, es, name, shape, dt):
        self.uid = getattr(self, "uid", 0) + 1
        return es.enter_context(self.nc.sbuf_tensor(f"{name}_{self.uid}", list(shape), dt))

    def ps(self, es, name, shape, dt=F32):
        self.uid = getattr(self, "uid", 0) + 1
        return es.enter_context(self.nc.psum_tensor(f"{name}_{self.uid}", list(shape), dt))

    def rsqrt(self, src_ap, dst_ap, tmp_ap, n_inv, slots_r, slots_w, tmp_slot, width, mode="act"):
        P = self.P
        P.op("vector", lambda e: e.tensor_scalar(out=tmp_ap, in0=src_ap, scalar1=n_inv, scalar2=EPS,
                                                 op0=ALU.mult, op1=ALU.add),
             reads=slots_r, writes=(tmp_slot,))
        if mode == "pool":
            nh = self.nhalf[:, 0:width]
            P.op("gpsimd", lambda e: e.tensor_tensor(out=dst_ap, in0=tmp_ap, in1=nh, op=ALU.pow),
                 reads=(tmp_slot,), writes=slots_w)
        else:
            P.op("scalar", lambda e: e.activation(out=tmp_ap, in_=tmp_ap, func=AF.Sqrt), reads=(tmp_slot,), writes=(tmp_slot,))
            P.op("vector", lambda e: e.reciprocal(out=dst_ap, in_=tmp_ap), reads=(tmp_slot,), writes=slots_w)

    def load_mod_tile(self, es, l, v, who, name, gbc=None, gslot=None):
        P = self.P
        t = self.sb(es, name, [128, D], F32)
        sl = P.slot(name)
        src = self.MODV[l, who, v * D:(v + 1) * D].partition_broadcast(128)
        P.dma("sync", t[:], src, sl, True)
        if gbc is not None:
            P.op("vector", lambda e: e.scalar_tensor_tensor(out=t[:], in0=t[:], scalar=1.0, in1=gbc[:],
                                                            op0=ALU.add, op1=ALU.mult),
                 reads=(gslot,), writes=(sl,))
        return t, sl

    def load_w(self, dst_tile, dst_slot, src_ap, kchunks, ncols, piece=512, order=None):
        P = self.P
        src = src_ap.rearrange("(k p) n -> p k n", p=128)
        npieces = (ncols + piece - 1) // piece
        slots = [dst_slot] * npieces if dst_slot is not None else P.slots_n(npieces, "wp")
        for pi in (order if order is not None else range(npieces)):
            c0 = pi * piece
            c1 = min(ncols, c0 + piece)
            P.dma("gpsimd", dst_tile[:, 0:kchunks, c0:c1], src[:, :, c0:c1], slots[pi], True)
        return slots

    def phase_const(self):
        P = self.P
        with ExitStack() as es:
            sl = P.slot("c")
            sl2 = P.slot("c2")
            P.dma("gpsimd", self.ident[:], self.identf[:, :], sl2, True)
            P.dma("sync", self.maskt[:], self.mask[:, :], sl, True)
            P.op("vector", lambda e: e.memset(self.onesf[:], 1.0), writes=(sl,))
            P.op("vector", lambda e: e.memset(self.nhalf[:], -0.5), writes=(sl,))
            P.flush()

    def phase_mod(self, layers=(0, 1)):
        P = self.P
        with ExitStack() as es:
            cv = self.sb(es, "cv", [128, 16], F32)
            sg = self.sb(es, "sgm", [128, 16], F32)
            sv = self.sb(es, "sv", [128, 16], BF16)
            bm = [self.sb(es, f"bm{l}", [2, 6 * D], F32) for l in layers]
            mv = [self.sb(es, f"mv{l}", [2, 6 * D], F32) for l in layers]
            wm = [self.sb(es, f"wm{i}", [128, 8, 512], BF16) for i in range(3)]
            pm = [self.ps(es, f"pm{i}", [128, 512]) for i in range(2)]
            s_cv, s_sv = P.slot(), P.slot()
            s_bm, s_mv = P.slots_n(len(layers)), P.slots_n(len(layers))
            s_wm = P.slots_n(3)
            s_pm = P.slots_n(2)
            P.dma("sync", cv[:], self.cvec[:, :], s_cv, True)
            P.op("scalar", lambda e: e.activation(out=sg[:], in_=cv[:], func=AF.Sigmoid), reads=(s_cv,), writes=(s_sv,))
            P.op("vector", lambda e: e.tensor_tensor(out=sv[:], in0=cv[:], in1=sg[:], op=ALU.mult),
                 reads=(s_cv, s_sv), writes=(s_sv,))
            cnt = 0
            for li, l in enumerate(layers):
                P.dma("sync", bm[li][:], self.b_mod[l].partition_broadcast(2), s_bm[li], True)
                wsrc = self.w_mod[l].rearrange("(k p) n -> p k n", p=128)
                for cg in range(12):
                    i = cnt % 3
                    ip = cnt % 2
                    cnt += 1
                    P.dma("gpsimd", wm[i][:], wsrc[:, :, cg * 512:(cg + 1) * 512], s_wm[i], True)

                    def mm(e, i=i, ip=ip):
                        for k in range(8):
                            r = e.matmul(pm[ip][0:2, :], sv[:, 2 * k:2 * k + 2], wm[i][:, k, :], start=(k == 0), stop=(k == 7))
                        return r
                    P.op("tensor", mm, reads=(s_sv, s_wm[i]), writes=(s_pm[ip],))
                    P.op("vector", lambda e, ip=ip, cg=cg, li=li: e.tensor_tensor(
                        out=mv[li][0:2, cg * 512:(cg + 1) * 512], in0=pm[ip][0:2, :], in1=bm[li][0:2, cg * 512:(cg + 1) * 512],
                        op=ALU.add), reads=(s_pm[ip], s_bm[li]), writes=(s_mv[li],))
                P.dma("sync", self.MODV[l], mv[li][:], s_mv[li], False)
            P.flush()

    def setup_norm(self, es, l, which, whos=("lat", "ctx"), ntp=2, es_tp=None):
        P = self.P
        gsrc = (self.g_mix if which == 0 else self.g_ffn)[l]
        gbc = self.sb(es, "gbc", [128, D], F32)
        s_g = P.slot("gbc")
        P.dma("sync", gbc[:], gsrc.partition_broadcast(128), s_g, True)
        vb = 0 if which == 0 else 3
        res = {}
        for who, nm in ((0, "lat"), (1, "ctx")):
            if nm not in whos:
                continue
            B, sB = self.load_mod_tile(es, l, vb, who, f"B_{nm}")
            A, sA = self.load_mod_tile(es, l, vb + 1, who, f"A_{nm}", gbc, s_g)
            res[nm] = (A, sA, B, sB)
        st = dict(mod=res)
        st["x"] = [self.sb(es, f"xr{i}", [128, D], F32) for i in range(2)]
        st["s_x"] = P.slots_n(2, "x")
        st["junk"] = self.sb(es, "junk", [128, D], BF16)
        st["s_junk"] = P.slot("junk")
        st["ss"] = [self.sb(es, f"ss{i}", [128, 4], F32) for i in range(2)]
        st["s_ss"] = P.slots_n(2, "ss")
        st["hf"] = self.sb(es, "hf", [128, D], F32)
        st["s_hf"] = P.slot("hf")
        st["h"] = [self.sb(es, f"h{i}", [128, D], BF16) for i in range(2)]
        st["s_h"] = P.slots_n(2, "h")
        st["ntp"] = ntp
        st["tp"] = [self.ps(es_tp if es_tp is not None else es, f"tp{i}", [128, 8, 128], BF16) for i in range(ntp)]
        st["s_tp"] = P.slots_n(ntp, "tp")
        st["cnt"] = 0
        st["tpc"] = 0
        return st

    def emit_hA(self, st, xsrc, r0, t, kind):
        P = self.P
        A, sA, B, sB = st["mod"]["ctx" if kind == "ctx" else "lat"]
        i = st["cnt"] % 2
        st["cnt"] += 1
        x, sx = st["x"][i], st["s_x"][i]
        ss, sss = st["ss"][i], st["s_ss"][i]
        h, sh = st["h"][i], st["s_h"][i]
        P.dma("sync", x[:], xsrc[r0 + t * 128:r0 + (t + 1) * 128, :], sx, True)
        P.op("scalar", lambda e: e.activation(out=st["junk"][:], in_=x[:], func=AF.Square, accum_out=ss[:, 0:1]),
             reads=(sx,), writes=(st["s_junk"], sss))
        self.rsqrt(ss[:, 0:1], ss[:, 2:3], ss[:, 1:2], 1.0 / D, (sss,), (sss,), sss, 1)
        P.op("vector", lambda e: e.scalar_tensor_tensor(
            out=st["hf"][:], in0=x[:], scalar=ss[:, 2:3], in1=A[:], op0=ALU.mult, op1=ALU.mult),
            reads=(sx, sss, sA), writes=(st["s_hf"],))
        P.op("gpsimd", lambda e: e.tensor_tensor(out=h[:], in0=st["hf"][:], in1=B[:], op=ALU.add),
             reads=(st["s_hf"], sB), writes=(sh,))
        return h, sh

    def emit_hT(self, st, xsrc, r0, n, kind, hT, s_hT, col0=0, tiles=None):
        for t in (range(n // 128) if tiles is None else tiles):
            h, sh = self.emit_hA(st, xsrc, r0, t, kind)
            self.transpose8(st, h, sh, hT, s_hT, col0 + t * 128)

    def h_prefetcher(self, st, xsrc, grp, hT, s_hT):
        state = {"t": 0, "pend": None}
        nT = grp[1] // 128

        def step():
            if state["pend"] is not None:
                h, sh, t = state["pend"]
                self.transpose8(st, h, sh, hT, s_hT, t * 128)
                state["pend"] = None
            if state["t"] < nT:
                t = state["t"]
                state["t"] += 1
                h, sh = self.emit_hA(st, xsrc, grp[0], t, grp[2])
                state["pend"] = (h, sh, t)
            return state["pend"] is not None or state["t"] < nT

        def finish():
            while step():
                pass
        return step, finish

    def transpose8(self, st, src, s_src, dst, s_dst, c0, eng="scalar"):
        P = self.P
        j = st["tpc"] % st["ntp"]
        st["tpc"] += 1
        tp, stp = st["tp"][j], st["s_tp"][j]

        def tr(e):
            for k in range(8):
                r = e.transpose(tp[:, k, :], src[:, k * 128:(k + 1) * 128], self.ident[:])
            return r
        P.op("tensor", tr, reads=(s_src,), writes=(stp,))
        if eng == "scalar":
            P.op("scalar", lambda e: e.copy(out=dst[:, 0:8, c0:c0 + 128], in_=tp[:]), reads=(stp,), writes=(s_dst,))
        else:
            P.op("vector", lambda e: e.tensor_copy(out=dst[:, 0:8, c0:c0 + 128], in_=tp[:]), reads=(stp,), writes=(s_dst,))

    def phase_kvq(self, l, xsrc, q_kinds):
        P = self.P
        with ExitStack() as es:
            st = self.setup_norm(es, l, 0)
            w = self.sb(es, "w", [128, 8, 3072], BF16)
            s_wp = self.load_w(w, None, self.w_in[l][:, 0:3072], 8, 3072, order=[2, 3, 4, 5, 0, 1])
            hT = [self.sb(es, f"hT{i}", [128, 8, 512], BF16) for i in range(2)]
            s_hT = P.slots_n(2, "hT")
            NTOK = 3
            tok = [self.ps(es, f"tok{i}", [128, 1024]) for i in range(NTOK)]
            s_tok = P.slots_n(NTOK, "tok")
            gq = self.sb(es, "gq", [128, 2, 64], F32)
            gsw = self.sb(es, "gsw", [128, 2, 64], F32)
            s_g = P.slot("g")
            P.dma("sync", gq[:, 0, :], self.q_norm_g[l].partition_broadcast(128), s_g, True)
            P.dma("sync", gq[:, 1, :], self.k_norm_g[l].partition_broadcast(128), s_g, True)
            g4 = gq[:].rearrange("p a (b h e) -> p (a b) h e", b=2, h=2)
            gs4 = gsw[:].rearrange("p a (b h e) -> p (a b) h e", b=2, h=2)
            P.op("vector", lambda e: e.tensor_copy(out=gs4[:, :, 0, :], in_=g4[:, :, 1, :]), reads=(s_g,), writes=(s_g,))
            P.op("vector", lambda e: e.tensor_copy(out=gs4[:, :, 1, :], in_=g4[:, :, 0, :]), reads=(s_g,), writes=(s_g,))
            rp = [self.sb(es, f"rp{i}", [128, 2, 64], F32) for i in range(2)]
            s_rp = P.slots_n(2, "rp")
            cg = [self.sb(es, f"cg{i}", [128, 2, 2, 64], F32) for i in range(2)]
            s_cg = P.slots_n(2, "cg")
            sq = [self.sb(es, f"sq{i}", [128, D], F32) for i in range(2)]
            kn = [self.sb(es, f"kn{i}", [128, D], F32) for i in range(2)]
            raw = [self.sb(es, f"raw{i}", [128, D], F32) for i in range(2)]
            s_raw = P.slots_n(2, "raw")
            t2 = [self.sb(es, f"t2{i}", [128, D], F32) for i in range(2)]
            kr = [self.sb(es, f"kr{i}", [128, D], BF16) for i in range(2)]
            s16 = [self.sb(es, f"s16{i}", [128, 3, 16], F32) for i in range(2)]
            s_sq, s_kn, s_t2, s_kr, s_s16 = (P.slots_n(2, "sq"), P.slots_n(2, "kn"), P.slots_n(2, "t2"),
                                             P.slots_n(2, "kr"), P.slots_n(2, "s16"))
            stage = [self.sb(es, f"stg{i}", [128, 8, 512], BF16) for i in range(4)]
            s_stage = P.slots_n(4, "stg")
            vst = [self.sb(es, f"vst{i}", [128, D], BF16) for i in range(2)]
            s_vst = P.slots_n(2, "vst")
            cnt = dict(tok=0, v=0, rp=0)

            def proj_tok(hTt, shT, t, col0):
                i = cnt["tok"] % NTOK
                cnt["tok"] += 1

                def mm(e):
                    for half in range(2):
                        for k in range(8):
                            r = e.matmul(tok[i][:, half * 512:(half + 1) * 512], hTt[:, k, t * 128:(t + 1) * 128],
                                         w[:, k, col0 + half * 512:col0 + (half + 1) * 512], start=(k == 0), stop=(k == 7))
                    return r
                P.op("tensor", mm, reads=(shT, s_wp[col0 // 512], s_wp[col0 // 512 + 1]), writes=(s_tok[i],))
                return tok[i], s_tok[i]

            def chain(pt, spt, b, cgt, scg, stg, sstg, t):
                s3, ss3 = s16[b], s_s16[b]
                g3 = lambda ap: ap.rearrange("p (g e) -> p g e", e=64)
                stages = []
                def st0():
                    P.op("scalar", lambda e: e.activation(out=sq[b][:], in_=pt[:], func=AF.Square), reads=(spt,), writes=(s_sq[b],))
                    P.op("scalar", lambda e: e.copy(out=raw[b][:], in_=pt[:]), reads=(spt,), writes=(s_raw[b],))
                stages.append(st0)
                stages.append(lambda: P.op("vector", lambda e: e.tensor_reduce(out=s3[:, 0, :], in_=g3(sq[b][:]), axis=AX.X, op=ALU.add),
                                           reads=(s_sq[b],), writes=(ss3,)))
                stages.append(lambda: self.rsqrt(s3[:, 0, :], s3[:, 2, :], s3[:, 1, :], 1.0 / 64, (ss3,), (ss3,), ss3, 16))
                stages.append(lambda: P.op("vector", lambda e: e.tensor_tensor(
                    out=g3(kn[b][:]), in0=g3(raw[b][:]), in1=s3[:, 2, :].unsqueeze(2).to_broadcast([128, 16, 64]), op=ALU.mult),
                    reads=(s_raw[b], ss3), writes=(s_kn[b],)))
                stages.append(lambda: P.op("gpsimd", lambda e: e.tensor_tensor(
                    out=g3(sq[b][:]), in0=g3(kn[b][:]), in1=cgt[:, b, 0, :].unsqueeze(1).to_broadcast([128, 16, 64]), op=ALU.mult),
                    reads=(s_kn[b], scg), writes=(s_sq[b],)))
                kn5 = kn[b][:].rearrange("p (g h e) -> p g h e", h=2, e=16)
                t25 = t2[b][:].rearrange("p (g h e) -> p g h e", h=2, e=16)
                sg5 = cgt[:, b, 1, :].rearrange("p (b h e) -> p b h e", h=2, e=16)

                def st_t2():
                    for hh in range(2):
                        P.op("vector", lambda e, hh=hh: e.tensor_tensor(
                            out=t25[:, :, hh, :].rearrange("p (g b) e -> p g b e", b=2),
                            in0=kn5[:, :, 1 - hh, :].rearrange("p (g b) e -> p g b e", b=2),
                            in1=sg5[:, :, hh, :].unsqueeze(1).to_broadcast([128, 16, 2, 16]), op=ALU.mult),
                            reads=(s_kn[b], scg), writes=(s_t2[b],))
                stages.append(st_t2)
                stages.append(lambda: P.op("gpsimd", lambda e: e.tensor_tensor(out=kr[b][:], in0=sq[b][:], in1=t2[b][:], op=ALU.add),
                                           reads=(s_sq[b], s_t2[b]), writes=(s_kr[b],)))
                stages.append(lambda: self.transpose8(st, kr[b], s_kr[b], stg, sstg, t * 128, eng="scalar"))
                return stages

            pending = []
            self.emit_hT(st, xsrc, GROUPS[0][0], GROUPS[0][1], GROUPS[0][2], hT[0], s_hT[0])
            for gi, (r0, n, kind) in enumerate(GROUPS):
                hTt, shT = hT[gi % 2], s_hT[gi % 2]
                nxt = GROUPS[gi + 1] if gi + 1 < len(GROUPS) else None
                need_q = kind in q_kinds
                T = n // 128
                pf_step, pf_finish = (None, None)
                if nxt is not None:
                    pf_step, pf_finish = self.h_prefetcher(st, xsrc, nxt, hT[(gi + 1) % 2], s_hT[(gi + 1) % 2])
                for t in range(T):
                    if pf_step is not None:
                        pf_step()
                    i = cnt["rp"] % 2
                    cnt["rp"] += 1
                    P.dma("sync", rp[i][:], self.rope[r0 + t * 128:r0 + (t + 1) * 128, :, :], s_rp[i], True)
                    for which in range(2):
                        if which == 0 and not need_q:
                            continue
                        P.op("gpsimd", lambda e, i=i, which=which: e.tensor_tensor(
                            out=cg[i][:, which, 0, :], in0=rp[i][:, 0, :], in1=gq[:, which, :], op=ALU.mult),
                            reads=(s_rp[i], s_g), writes=(s_cg[i],))
                        P.op("gpsimd", lambda e, i=i, which=which: e.tensor_tensor(
                            out=cg[i][:, which, 1, :], in0=rp[i][:, 1, :], in1=gsw[:, which, :], op=ALU.mult),
                            reads=(s_rp[i], s_g), writes=(s_cg[i],))
                    pk, spk = proj_tok(hTt, shT, t, 1024)
                    pv, spv = proj_tok(hTt, shT, t, 2048)
                    if need_q:
                        pq, spq = proj_tok(hTt, shT, t, 0)
                    for f in pending:
                        f()
                    pending = []
                    vi = cnt["v"] % 2
                    cnt["v"] += 1
                    P.op("scalar", lambda e, pv=pv, vi=vi: e.copy(out=vst[vi][:], in_=pv[:]), reads=(spv,), writes=(s_vst[vi],))
                    P.dma("sync", self.V[r0 + t * 128:r0 + (t + 1) * 128, :], vst[vi][:], s_vst[vi], False)
                    sk = (gi % 2) * 2 + 1
                    sq_ = (gi % 2) * 2
                    chains = [chain(pk, spk, 1, cg[i], s_cg[i], stage[sk], s_stage[sk], t)]
                    if need_q:
                        chains.append(chain(pq, spq, 0, cg[i], s_cg[i], stage[sq_], s_stage[sq_], t))
                    nst = len(chains[0])
                    for si in range(nst - 1):
                        for ch in chains:
                            ch[si]()
                    for ch in chains:
                        pending.append(ch[nst - 1])
                    if t == T - 1 and pf_finish is not None:
                        pf_finish()
                    if t == T - 1:
                        def stores(r0=r0, n=n, sk=sk, sq_=sq_, need_q=need_q):
                            P.dma("sync", self.KT[:, :, r0:r0 + n].rearrange("h p r -> p h r"), stage[sk][:, :, 0:n], s_stage[sk], False)
                            if need_q:
                                P.dma("sync", self.QT[:, :, r0:r0 + n].rearrange("h p r -> p h r"), stage[sq_][:, :, 0:n], s_stage[sq_], False)
                        pending.append(stores)
            for f in pending:
                f()
            P.flush()

    def phase_glu(self, l, xsrc, glu_groups, gate_kinds, do_flush=True):
        P = self.P
        with ExitStack() as es:
            st = self.setup_norm(es, l, 0)
            w = self.sb(es, "w", [128, 8, 4096], BF16)
            s_wp = self.load_w(w, None, self.w_in[l][:, 3072:INW], 8, 4096, order=[0, 2, 1, 3, 4, 5, 6, 7])
            hT = [self.sb(es, f"hT{i}", [128, 8, 512], BF16) for i in range(2)]
            s_hT = P.slots_n(2, "hT")
            fm = [self.ps(es, f"fm{i}", [128, 512]) for i in range(4)]
            s_fm = P.slots_n(4, "fm")
            sgt = [self.sb(es, f"sgt{i}", [128, 512], F32) for i in range(2)]
            s_sgt = P.slots_n(2, "sgt")
            stage = [self.sb(es, f"stg{i}", [128, 8, 512], BF16) for i in range(3)]
            s_stage = P.slots_n(3, "stg")
            cnt = dict(fm=0, sg=0, stage=0, g=0)

            def proj_fm(hTt, shT, col0, n):
                i = cnt["fm"] % 4
                cnt["fm"] += 1

                def mm(e):
                    for k in range(8):
                        r = e.matmul(fm[i][:, 0:n], w[:, k, col0:col0 + 128], hTt[:, k, 0:n], start=(k == 0), stop=(k == 7))
                    return r
                P.op("tensor", mm, reads=(shT, s_wp[col0 // 512]), writes=(s_fm[i],))
                return fm[i], s_fm[i]

            work = [(gi, g) for gi, g in enumerate(GROUPS) if (gi in glu_groups or g[2] in gate_kinds)]
            pre = {"step": None, "finish": None}

            def prefetch():
                if pre["step"] is not None:
                    pre["step"]()
            g0 = work[0][1]
            self.emit_hT(st, xsrc, g0[0], g0[1], g0[2], hT[0], s_hT[0])
            for wi, (gi, (r0, n, kind)) in enumerate(work):
                need_glu = gi in glu_groups
                need_gate = kind in gate_kinds
                hTt, shT = hT[wi % 2], s_hT[wi % 2]
                if pre["finish"] is not None:
                    pre["finish"]()
                pre["step"] = pre["finish"] = None
                if wi + 1 < len(work):
                    b = (wi + 1) % 2
                    pre["step"], pre["finish"] = self.h_prefetcher(st, xsrc, work[wi + 1][1], hT[b], s_hT[b])
                if need_glu:
                    si = cnt["stage"] % 3
                    cnt["stage"] += 1
                    for j in range(8):
                        if j % 2 == 0:
                            prefetch()
                        pa, spa = proj_fm(hTt, shT, j * 128, n)
                        pg, spg = proj_fm(hTt, shT, 1024 + j * 128, n)
                        k = cnt["sg"] % 2
                        cnt["sg"] += 1
                        P.op("scalar", lambda e, pg=pg, k=k: e.activation(out=sgt[k][:, 0:n], in_=pg[:, 0:n], func=AF.Sigmoid),
                             reads=(spg,), writes=(s_sgt[k],))
                        P.op("vector", lambda e, pa=pa, k=k, j=j, si=si: e.tensor_tensor(
                            out=stage[si][:, j, 0:n], in0=pa[:, 0:n], in1=sgt[k][:, 0:n], op=ALU.mult),
                            reads=(spa, s_sgt[k]), writes=(s_stage[si],))
                    c0 = ucol(r0)
                    P.dma("sync", self.UT[:, :, c0:c0 + n].rearrange("j p r -> p j r"), stage[si][:, :, 0:n], s_stage[si], False)
                if need_gate:
                    for half in range(2):
                        si = cnt["stage"] % 3
                        cnt["stage"] += 1
                        for j in range(8):
                            if j % 2 == 1:
                                prefetch()
                            pg, spg = proj_fm(hTt, shT, 2048 + (half * 8 + j) * 128, n)
                            P.op("scalar", lambda e, pg=pg, j=j, si=si: e.activation(
                                out=stage[si][:, j, 0:n], in_=pg[:, 0:n], func=AF.Sigmoid),
                                reads=(spg,), writes=(s_stage[si],))
                        P.dma("sync", self.GT[half * 8:(half + 1) * 8, :, r0:r0 + n].rearrange("j p r -> p j r"),
                              stage[si][:, :, 0:n], s_stage[si], False)
            if do_flush:
                P.flush()

    def phase_fix(self):
        P = self.P
        if "fix" in SKIP:
            return
        with ExitStack() as es:
            e_in = self.sb(es, "ein", [128, 8, 4, 16], BF16)
            e_out = self.sb(es, "eout", [128, 8, 6, 16], BF16)
            s_in, s_out = P.slot(), P.slot()
            own0, oth0 = 288, 2368
            srcs = [oth0 + 16 + HALF - 16, oth0 + 16, own0 + 16 + HALF - 16, own0 + 16]
            for i, c in enumerate(srcs):
                P.dma("sync", e_in[:, :, i, :], self.UT[:, :, c:c + 16].rearrange("j p r -> p j r"), s_in, True)
            mL, mR = self.maskt[:, 0:1], self.maskt[:, 1:2]
            plan = [(0, mL), (1, mR), (2, mR), (3, mL)]
            dsts = [own0, own0 + 16 + HALF, oth0, oth0 + 16 + HALF, 0, 16 + CTX]
            for i, (srci, m) in enumerate(plan):
                P.op("vector", lambda e, i=i, srci=srci, m=m: e.tensor_scalar(
                    out=e_out[:, :, i, :], in0=e_in[:, :, srci, :], scalar1=m, scalar2=None, op0=ALU.mult),
                    reads=(s_in,), writes=(s_out,))
            P.op("vector", lambda e: e.memset(e_out[:, :, 4:6, :], 0.0), writes=(s_out,))
            for i, c in enumerate(dsts):
                P.dma("sync", self.UT[:, :, c:c + 16].rearrange("j p r -> p j r"), e_out[:, :, i, :], s_out, False)
            P.flush()

    def phase_conv(self, l, kinds):
        P = self.P
        with ExitStack() as es:
            dw = self.sb(es, "dw", [128, 8, 31], F32)
            cvp = self.sb(es, "cvp", [128, 3, 8], F32)
            idf = self.sb(es, "idf", [128, 128], F32)
            s_c = P.slot("c")
            P.dma("sync", dw[:], self.dwT[l], s_c, True)
            P.dma("sync", cvp[:], self.cvp[l], s_c, True)
            P.dma("sync", idf[:], self.identf[:, :], s_c, True)
            diag = self.sb(es, "diag", [128, 8 * 31, 128], BF16)
            s_diag = P.slots_n(8, "diag")
            for j in range(8):
                eng = "vector" if (j % 2 == 0) else "gpsimd"
                P.op(eng, lambda e, j=j: e.tensor_tensor(
                    out=diag[:, j * 31:(j + 1) * 31, :], in0=idf[:].unsqueeze(1).to_broadcast([128, 31, 128]),
                    in1=dw[:, j, :].unsqueeze(2).to_broadcast([128, 31, 128]), op=ALU.mult),
                    reads=(s_c,), writes=(s_diag[j],))
            wco = self.sb(es, "wco", [128, 8, D], BF16)
            s_wco = P.slot("wco")
            self.load_w(wco, s_wco, self.w_conv_o[l], 8, D)
            uw = [self.sb(es, f"uw{i}", [128, 8, 544], BF16) for i in range(2)]
            s_uw = P.slots_n(2, "uw")
            g2 = [self.sb(es, f"g2{i}", [128, 8, 512], BF16) for i in range(2)]
            s_g2 = P.slots_n(2, "g2")
            cT = [self.sb(es, f"cT{i}", [128, 8, 512], F32) for i in range(2)]
            s_cT = P.slots_n(2, "cT")
            sqT = [self.sb(es, f"sqT{i}", [128, 8, 512], BF16) for i in range(2)]
            s_sqT = P.slots_n(2, "sqT")
            onesb = self.sb(es, "onesb", [128, 128], BF16)
            s_ob = P.slot("onesb")
            P.op("vector", lambda e: e.memset(onesb[:], 1.0), writes=(s_ob,))
            aT = self.sb(es, "aT", [128, 8, 512], BF16)
            s_aT = P.slot("aT")
            stt = self.sb(es, "stt", [128, 4, 512], F32)
            s_stt = P.slot("stt")
            tmp = [self.sb(es, f"tmpc{i}", [128, 512], F32) for i in range(2)]
            s_tmp = P.slots_n(2, "tmp")
            mcs = self.sb(es, "mcs", [128, 8, 512], BF16)
            s_mcs = P.slot("mcs")
            pc = [self.ps(es, f"pc{i}", [128, 512]) for i in range(4)]
            s_pc = P.slots_n(4, "pc")
            pst = [self.ps(es, f"pst{i}", [128, 512]) for i in range(2)]
            s_pst = P.slots_n(2, "pst")
            po = [self.ps(es, f"po{i}", [128, 512]) for i in range(2)]
            s_po = P.slots_n(2, "po")
            c_pc = 0
            gcount = 0
            prev = None

            def post_a(b, n, r0):
                def mm1(e):
                    for j in range(8):
                        r = e.matmul(pst[0][:, 0:n], self.onesf[:], cT[b][:, j, 0:n], start=(j == 0), stop=(j == 7))
                    return r

                def mm2(e):
                    for j in range(8):
                        r = e.matmul(pst[1][:, 0:n], onesb[:], sqT[b][:, j, 0:n], start=(j == 0), stop=(j == 7))
                    return r
                P.op("tensor", mm1, reads=(s_cT[b],), writes=(s_pst[0],))
                P.op("tensor", mm2, reads=(s_sqT[b], s_ob), writes=(s_pst[1],))
                P.op("vector", lambda e: e.tensor_scalar(out=stt[:, 0, 0:n], in0=pst[0][:, 0:n], scalar1=1.0 / D, scalar2=None, op0=ALU.mult),
                     reads=(s_pst[0],), writes=(s_stt,))
                P.op("vector", lambda e: e.tensor_tensor(out=stt[:, 2, 0:n], in0=stt[:, 0, 0:n], in1=stt[:, 0, 0:n], op=ALU.mult),
                     reads=(s_stt,), writes=(s_stt,))
                P.op("vector", lambda e: e.scalar_tensor_tensor(out=stt[:, 1, 0:n], in0=pst[1][:, 0:n], scalar=1.0 / D, in1=stt[:, 2, 0:n],
                                                                op0=ALU.mult, op1=ALU.subtract),
                     reads=(s_pst[1], s_stt), writes=(s_stt,))
                P.op("vector", lambda e: e.tensor_scalar(out=stt[:, 2, 0:n], in0=stt[:, 1, 0:n], scalar1=1.0, scalar2=EPS, op0=ALU.mult, op1=ALU.add),
                     reads=(s_stt,), writes=(s_stt,))
                P.op("scalar", lambda e: e.activation(out=stt[:, 3, 0:n], in_=stt[:, 2, 0:n], func=AF.Sqrt), reads=(s_stt,), writes=(s_stt,))
                P.op("vector", lambda e: e.reciprocal(out=stt[:, 1, 0:n], in_=stt[:, 3, 0:n]), reads=(s_stt,), writes=(s_stt,))
                for j in range(8):
                    k = j % 2
                    P.op("vector", lambda e, j=j, k=k: e.tensor_tensor(out=tmp[k][:, 0:n], in0=cT[b][:, j, 0:n], in1=stt[:, 0, 0:n], op=ALU.subtract),
                         reads=(s_cT[b], s_stt), writes=(s_tmp[k],))
                    P.op("gpsimd", lambda e, j=j, k=k: e.tensor_tensor(out=tmp[k][:, 0:n], in0=tmp[k][:, 0:n], in1=stt[:, 1, 0:n], op=ALU.mult),
                         reads=(s_tmp[k], s_stt), writes=(s_tmp[k],))
                    P.op("scalar", lambda e, j=j, k=k: e.activation(out=aT[:, j, 0:n], in_=tmp[k][:, 0:n], func=AF.Silu,
                                                                    bias=cvp[:, 2, j:j + 1], scale=cvp[:, 1, j:j + 1]),
                         reads=(s_tmp[k], s_c), writes=(s_aT,))

            def post_b(b, n, r0):
                for m in range(8):
                    i = m % 2

                    def mmo(e, i=i, m=m):
                        for k in range(8):
                            r = e.matmul(po[i][:, 0:n], wco[:, k, m * 128:(m + 1) * 128], aT[:, k, 0:n], start=(k == 0), stop=(k == 7))
                        return r
                    P.op("tensor", mmo, reads=(s_wco, s_aT), writes=(s_po[i],))
                    P.op("vector", lambda e, i=i, m=m: e.tensor_tensor(out=mcs[:, m, 0:n], in0=po[i][:, 0:n], in1=g2[b][:, m, 0:n], op=ALU.mult),
                         reads=(s_po[i], s_g2[b]), writes=(s_mcs,))
                P.dma("sync", self.MC[:, :, r0:r0 + n].rearrange("j p r -> p j r"), mcs[:, :, 0:n], s_mcs, False)

            for gi, (r0, n, kind) in enumerate(GROUPS):
                if kind not in kinds:
                    continue
                b = gcount % 2
                gcount += 1
                c0 = ucol(r0)
                P.dma("sync", uw[b][:, :, 0:n + 30], self.UT[:, :, c0 - 15:c0 + n + 15].rearrange("j p r -> p j r"), s_uw[b], True)
                P.dma("sync", g2[b][:, :, 0:n], self.GT[8:16, :, r0:r0 + n].rearrange("j p r -> p j r"), s_g2[b], True)
                if prev is not None:
                    post_a(*prev)
                for j in range(8):
                    i = c_pc % 4
                    c_pc += 1

                    def mm(e, i=i, j=j, b=b):
                        for k in range(31):
                            r = e.matmul(pc[i][:, 0:n], diag[:, j * 31 + k, :], uw[b][:, j, k:k + n], start=(k == 0), stop=(k == 30))
                        return r
                    P.op("tensor", mm, reads=(s_diag[j], s_uw[b]), writes=(s_pc[i],))
                    P.op("scalar", lambda e, i=i, j=j: e.activation(out=cT[b][:, j, 0:n], in_=pc[i][:, 0:n], func=AF.Identity,
                                                                    bias=cvp[:, 0, j:j + 1], scale=1.0),
                         reads=(s_pc[i], s_c), writes=(s_cT[b],))
                    P.op("gpsimd", lambda e, j=j: e.tensor_tensor(out=sqT[b][:, j, 0:n], in0=cT[b][:, j, 0:n], in1=cT[b][:, j, 0:n], op=ALU.mult),
                         reads=(s_cT[b],), writes=(s_sqT[b],))
                if prev is not None:
                    post_b(*prev)
                prev = (b, n, r0)
            post_a(*prev)
            post_b(*prev)
            P.flush()

    def phase_attn(self, l, kinds):
        P = self.P
        lam_init = 0.8 - 0.6 * math.exp(-0.3 * l)
        with ExitStack() as es:
            lv = self.sb(es, "lv", [128, 4, 64], F32)
            lsm = self.sb(es, "lsm", [128, 8], F32)
            s_l = P.slot("lam")
            for i in range(4):
                P.dma("sync", lv[:, i, :], self.lams[i][l].partition_broadcast(128), s_l, True)
            P.op("vector", lambda e: e.tensor_tensor(out=lv[:, 0, :], in0=lv[:, 0, :], in1=lv[:, 1, :], op=ALU.mult), reads=(s_l,), writes=(s_l,))
            P.op("vector", lambda e: e.tensor_tensor(out=lv[:, 2, :], in0=lv[:, 2, :], in1=lv[:, 3, :], op=ALU.mult), reads=(s_l,), writes=(s_l,))
            P.op("vector", lambda e: e.tensor_reduce(out=lsm[:, 0:1], in_=lv[:, 0, :], axis=AX.X, op=ALU.add), reads=(s_l,), writes=(s_l,))
            P.op("vector", lambda e: e.tensor_reduce(out=lsm[:, 1:2], in_=lv[:, 2, :], axis=AX.X, op=ALU.add), reads=(s_l,), writes=(s_l,))
            P.op("scalar", lambda e: e.activation(out=lsm[:, 2:4], in_=lsm[:, 0:2], func=AF.Exp), reads=(s_l,), writes=(s_l,))
            P.op("vector", lambda e: e.tensor_tensor(out=lsm[:, 4:5], in0=lsm[:, 3:4], in1=lsm[:, 2:3], op=ALU.subtract), reads=(s_l,), writes=(s_l,))
            P.op("vector", lambda e: e.tensor_scalar(out=lsm[:, 5:6], in0=lsm[:, 4:5], scalar1=-lam_init, scalar2=None, op0=ALU.add), reads=(s_l,), writes=(s_l,))
            nlam = lsm[:, 5:6]
            sgb = self.sb(es, "sgb", [128, 128], F32)
            P.dma("sync", sgb[:], self.subln_g[l].partition_broadcast(128), s_l, True)
            P.op("vector", lambda e: e.tensor_scalar(out=sgb[:], in0=sgb[:], scalar1=(1.0 - lam_init), scalar2=None, op0=ALU.mult), reads=(s_l,), writes=(s_l,))
            kt = [self.sb(es, f"kt{i}", [128, RALL], BF16) for i in range(2)]
            s_kt = P.slots_n(2, "kt")
            vx = [self.sb(es, f"vx{i}", [128, 34, 132], BF16) for i in range(2)]
            s_vx = P.slots_n(2, "vx")
            for i in range(2):
                P.op("gpsimd", lambda e, i=i: e.memset(vx[i][:, :, 128:132], 1.0), writes=(s_vx[i],))
            qz = [[self.sb(es, f"qz{c}_{i}", [128, 512], BF16) for i in range(2)] for c in range(2)]
            s_qt = P.slots_n(2, "qt")
            for c in range(2):
                for i in range(2):
                    P.op("gpsimd", lambda e, c=c, i=i: e.memset(qz[c][i][:], 0.0), writes=(s_qt[i],))
            NPT = 6
            pT = [self.sb(es, f"pT{i}", [128, 512], BF16) for i in range(NPT)]
            s_pT = P.slots_n(NPT, "pT")
            sps = [self.ps(es, f"sps{i}", [128, 512]) for i in range(3)]
            s_sps = P.slots_n(3, "sps")
            acc = self.ps(es, "acc", [128, 8, 256])
            s_acc = P.slots_n(2, "acc")
            tpo = self.ps(es, "tpo", [128, 4, 128], BF16)
            s_tpo = P.slot("tpo")
            rr = self.sb(es, "rr", [128, 8], F32)
            o = self.sb(es, "o", [128, 4, 128], F32)
            o2 = self.sb(es, "o2", [128, 4, 128], F32)
            sso = self.sb(es, "sso", [128, 3, 4], F32)
            on = self.sb(es, "on", [128, 4, 128], BF16)
            s_ep = P.slot("ep")
            s_on = P.slot("on")
            ost = [self.sb(es, f"ost{i}", [128, 512], BF16) for i in range(2)]
            s_ost = P.slots_n(2, "ost")
            c_sps = 0
            c_pt = 0
            c_q = 0
            pend_tail = []
            scale = 1.0 / 8.0
            def load_kv(h):
                hb = h % 2
                P.dma("sync", kt[hb][:], self.KT[h], s_kt[hb], True)
                P.dma("sync", vx[hb][:, :, 0:128], self.V[:, h * 128:(h + 1) * 128].rearrange("(c p) d -> p c d", p=128), s_vx[hb], True)

            def load_q(item, qb):
                h, (r0, n, kind) = item
                P.dma("sync", qz[0][qb][0:64, 0:n], self.QT[h, 0:64, r0:r0 + n], s_qt[qb], True)
                P.dma("sync", qz[1][qb][64:128, 0:n], self.QT[h, 64:128, r0:r0 + n], s_qt[qb], True)
            items = [(h, g) for h in range(NH) for g in GROUPS if g[2] in kinds]
            load_kv(0)
            load_q(items[0], 0)
            for ii, (h, (r0, n, kind)) in enumerate(items):
                hb = h % 2
                if True:
                    T = n // 128
                    nkc = 2 if kind == "ctx" else 34
                    qb = ii % 2
                    if ii + 1 < len(items):
                        load_q(items[ii + 1], (ii + 1) % 2)
                    if (ii == 0 or items[ii - 1][0] != h) and h + 1 < NH:
                        load_kv(h + 1)
                    for c in range(2):
                        LAG = 2
                        pend = []
                        for step in range(nkc + LAG):
                            if c == 0 and pend_tail and step == min(10, nkc + LAG - 1):
                                pend_tail.pop(0)()
                            if step < nkc:
                                kc = step
                                si = c_sps % 3
                                c_sps += 1
                                pi = c_pt % NPT
                                c_pt += 1
                                P.op("tensor", lambda e, si=si, kc=kc, c=c, hb=hb, qb=qb: e.matmul(
                                    sps[si][:, 0:n], kt[hb][:, kc * 128:(kc + 1) * 128],
                                    qz[c][qb][:, 0:n], start=True, stop=True),
                                    reads=(s_kt[hb], s_qt[qb]), writes=(s_sps[si],))
                                P.op("scalar", lambda e, si=si, pi=pi: e.activation(
                                    out=pT[pi][:, 0:n], in_=sps[si][:, 0:n], func=AF.Exp, scale=scale),
                                    reads=(s_sps[si],), writes=(s_pT[pi],))
                                pend.append((kc, pi))
                            if step >= LAG:
                                kc, pi = pend.pop(0)

                                def pv(e, kc=kc, pi=pi, c=c, hb=hb):
                                    for t in range(T):
                                        r = e.matmul(acc[:, c * 4 + t, 0:129], pT[pi][:, t * 128:(t + 1) * 128], vx[hb][:, kc, 0:129],
                                                     start=(kc == 0 and t % 2 == 0), stop=(kc == nkc - 1), skip_group_check=True)
                                    return r
                                P.op("tensor", pv, reads=(s_pT[pi], s_vx[hb]), writes=(s_acc[c],))
                    acc4 = acc[:].rearrange("p (c t) w -> p c t w", c=2)
                    P.op("vector", lambda e: e.reciprocal(out=rr[:].rearrange("p (c t) -> p c t", c=2)[:, :, 0:T],
                                                          in_=acc4[:, :, 0:T, 128]), reads=(s_acc[0], s_acc[1]), writes=(s_ep,))
                    P.op("vector", lambda e: e.tensor_scalar(out=rr[:, 4:4 + T], in0=rr[:, 4:4 + T], scalar1=nlam, scalar2=None, op0=ALU.mult),
                         reads=(s_ep, s_l), writes=(s_ep,))
                    P.op("vector", lambda e: e.tensor_tensor(out=o[:, 0:T, :], in0=acc4[:, 0, 0:T, 0:128],
                                                             in1=rr[:, 0:T].unsqueeze(2).to_broadcast([128, T, 128]), op=ALU.mult),
                         reads=(s_acc[0], s_ep), writes=(s_ep,))
                    P.op("vector", lambda e: e.tensor_tensor(out=o2[:, 0:T, :], in0=acc4[:, 1, 0:T, 0:128],
                                                             in1=rr[:, 4:4 + T].unsqueeze(2).to_broadcast([128, T, 128]), op=ALU.mult),
                         reads=(s_acc[1], s_ep), writes=(s_ep,))
                    P.op("gpsimd", lambda e: e.tensor_tensor(out=o[:, 0:T, :], in0=o[:, 0:T, :], in1=o2[:, 0:T, :], op=ALU.add),
                         reads=(s_ep,), writes=(s_ep,))
                    P.op("gpsimd", lambda e: e.tensor_tensor(out=o2[:, 0:T, :], in0=o[:, 0:T, :], in1=o[:, 0:T, :], op=ALU.mult),
                         reads=(s_ep,), writes=(s_ep,))
                    P.op("vector", lambda e: e.tensor_reduce(out=sso[:, 0, 0:T], in_=o2[:, 0:T, :], axis=AX.X, op=ALU.add),
                         reads=(s_ep,), writes=(s_ep,))
                    self.rsqrt(sso[:, 0, 0:T], sso[:, 2, 0:T], sso[:, 1, 0:T], 1.0 / 128, (s_ep,), (s_ep,), s_ep, T, mode="pool")
                    P.op("vector", lambda e: e.tensor_tensor(out=o[:, 0:T, :], in0=o[:, 0:T, :],
                                                             in1=sso[:, 2, 0:T].unsqueeze(2).to_broadcast([128, T, 128]), op=ALU.mult),
                         reads=(s_ep,), writes=(s_ep,))
                    P.op("vector", lambda e: e.tensor_tensor(out=on[:, 0:T, :], in0=o[:, 0:T, :],
                                                             in1=sgb[:].unsqueeze(1).to_broadcast([128, T, 128]), op=ALU.mult),
                         reads=(s_ep, s_l), writes=(s_on,))

                    def tail(T=T, n=n, ob=qb, h=h, r0=r0):
                        def tr(e):
                            for t in range(T):
                                r = e.transpose(tpo[:, t, :], on[:, t, :], self.ident[:])
                            return r
                        P.op("tensor", tr, reads=(s_on,), writes=(s_tpo,))
                        P.op("vector", lambda e: e.tensor_copy(out=ost[ob][:, 0:n], in_=tpo[:].rearrange("p t q -> p (t q)")[:, 0:n]),
                             reads=(s_tpo,), writes=(s_ost[ob],))
                        P.dma("sync", self.OT[h, :, r0:r0 + n], ost[ob][:, 0:n], s_ost[ob], False)
                    pend_tail.append(tail)
            while pend_tail:
                pend_tail.pop(0)()
            P.flush()

    def phase_merge(self, l, xsrc, xdst, kinds):
        P = self.P
        with ExitStack() as es:
            wao = self.sb(es, "wao", [128, 8, D], BF16)
            wout = self.sb(es, "wout", [128, 8, D], BF16)
            s_waop = self.load_w(wao, None, self.w_attn_o[l], 8, D)
            s_woutp = self.load_w(wout, None, self.w_out[l], 8, D)
            ml2 = {}
            for who, nm in ((0, "lat"), (1, "ctx")):
                ml2[nm] = self.load_mod_tile(es, l, 2, who, f"G_{nm}")
            oT = [self.sb(es, f"oT{i}", [128, 8, 512], BF16) for i in range(2)]
            g1 = [self.sb(es, f"g1{i}", [128, 8, 512], BF16) for i in range(2)]
            mc = [self.sb(es, f"mc{i}", [128, 8, 512], BF16) for i in range(2)]
            s_oT, s_g1, s_mc = P.slots_n(2), P.slots_n(2), P.slots_n(2)
            mg = [self.sb(es, f"mg{i}", [128, 8, 512], BF16) for i in range(2)]
            s_mg = P.slots_n(2)
            tmp = [self.sb(es, f"tm{i}", [128, 512], F32) for i in range(2)]
            s_tmp = P.slots_n(2)
            pa = [self.ps(es, f"pa{i}", [128, 512]) for i in range(2)]
            s_pa = P.slots_n(2)
            po = [self.ps(es, f"pox{i}", [128, 1024]) for i in range(2)]
            s_po = P.slots_n(2)
            xt = [self.sb(es, f"xt{i}", [128, D], F32) for i in range(2)]
            s_xt = P.slots_n(2)
            xo = [self.sb(es, f"xo{i}", [128, D], F32) for i in range(2)]
            s_xo = P.slots_n(2)
            gc = 0
            tcd = {"tc": 0}
            pend_w = []
            for gi, (r0, n, kind) in enumerate(GROUPS):
                if kind not in kinds:
                    continue
                b = gc % 2
                gc += 1
                G, sG = ml2["ctx" if kind == "ctx" else "lat"]
                P.dma("sync", oT[b][:, :, 0:n], self.OT[:, :, r0:r0 + n].rearrange("j p r -> p j r"), s_oT[b], True)
                P.dma("sync", g1[b][:, :, 0:n], self.GT[0:8, :, r0:r0 + n].rearrange("j p r -> p j r"), s_g1[b], True)
                P.dma("sync", mc[b][:, :, 0:n], self.MC[:, :, r0:r0 + n].rearrange("j p r -> p j r"), s_mc[b], True)
                for m in range(8):
                    i = m % 2

                    def mm(e, i=i, m=m, b=b):
                        for k in range(8):
                            r = e.matmul(pa[i][:, 0:n], wao[:, k, m * 128:(m + 1) * 128], oT[b][:, k, 0:n], start=(k == 0), stop=(k == 7))
                        return r
                    P.op("tensor", mm, reads=(s_waop[m // 4], s_oT[b]), writes=(s_pa[i],))
                    P.op("vector", lambda e, i=i, m=m, b=b: e.tensor_tensor(out=tmp[i][:, 0:n], in0=pa[i][:, 0:n], in1=g1[b][:, m, 0:n], op=ALU.mult),
                         reads=(s_pa[i], s_g1[b]), writes=(s_tmp[i],))
                    P.op("gpsimd", lambda e, i=i, m=m, b=b: e.tensor_tensor(out=mg[b][:, m, 0:n], in0=tmp[i][:, 0:n], in1=mc[b][:, m, 0:n], op=ALU.add),
                         reads=(s_tmp[i], s_mc[b]), writes=(s_mg[b],))
                def wout_part(r0=r0, n=n, b=b, G=G, sG=sG):
                    for t in range(n // 128):
                        i = tcd["tc"] % 2
                        tcd["tc"] += 1
                        P.dma("sync", xt[i][:], xsrc[r0 + t * 128:r0 + (t + 1) * 128, :], s_xt[i], True)

                        def mm(e, i=i, t=t, b=b):
                            for half in range(2):
                                for k in range(8):
                                    r = e.matmul(po[i][:, half * 512:(half + 1) * 512], mg[b][:, k, t * 128:(t + 1) * 128],
                                                 wout[:, k, half * 512:(half + 1) * 512], start=(k == 0), stop=(k == 7))
                            return r
                        P.op("tensor", mm, reads=(s_woutp[0], s_woutp[1], s_mg[b]), writes=(s_po[i],))
                        P.op("vector", lambda e, i=i: e.tensor_tensor(out=xo[i][:], in0=po[i][:], in1=G[:], op=ALU.mult),
                             reads=(s_po[i], sG), writes=(s_xo[i],))
                        P.op("gpsimd", lambda e, i=i: e.tensor_tensor(out=xo[i][:], in0=xo[i][:], in1=xt[i][:], op=ALU.add),
                             reads=(s_xt[i], s_xo[i]), writes=(s_xo[i],))
                        P.dma("sync", xdst[r0 + t * 128:r0 + (t + 1) * 128, :], xo[i][:], s_xo[i], False)
                if pend_w:
                    pend_w.pop(0)()
                pend_w.append(wout_part)
            while pend_w:
                pend_w.pop(0)()
            P.flush()

    def phase_ffn(self, l, groups, xsrc, xdst_fn, experts, router):
        P = self.P
        R = sum(n for _, n, _ in groups)
        NT = R // 128
        with ExitStack() as es:
            pl = self.ps(es, "pl", [128, NT, NE]) if router is not None else None
            es_tp = ExitStack()
            whos = tuple(nm for nm in ("lat", "ctx") if any((k == "ctx") == (nm == "ctx") for _, _, k in groups))
            st = self.setup_norm(es, l, 1, whos=whos, ntp=2, es_tp=es_tp)
            ml5 = {}
            for who, nm in ((0, "lat"), (1, "ctx")):
                if nm in whos:
                    ml5[nm] = self.load_mod_tile(es, l, 5, who, f"G_{nm}")
            hT = self.sb(es, "h2T", [128, 8, R], BF16)
            s_hT = P.slot("h2T")
            accs = self.sb(es, "accs", [128, NT, D], F32)
            s_acc = P.slots_n(NT, "acc")
            col = 0
            for (r0, n, kind) in groups:
                self.emit_hT(st, xsrc, r0, n, kind, hT, s_hT, col0=col)
                col += n
            gates = None
            if router is not None:
                wr = self.sb(es, "wr", [128, 8, NE], BF16)
                s_wr = P.slot("wr")
                P.dma("gpsimd", wr[:], router.rearrange("(k p) n -> p k n", p=128), s_wr, True)
                gates = self.sb(es, "gates", [128, NT, NE], F32)
                s_gates = P.slot("gates")
                lg = self.sb(es, "lg", [128, NT, NE], F32)
                mx = self.sb(es, "mx", [128, NT, 8], F32)
                sm = self.sb(es, "smx", [128, NT, 2], F32)
                s_pl = P.slot("pl")
                s_lg = P.slot("lg")
                for t in range(NT):
                    def mm(e, t=t):
                        for k in range(8):
                            r = e.matmul(pl[:, t, :], hT[:, k, t * 128:(t + 1) * 128], wr[:, k, :], start=(k == 0), stop=(k == 7))
                        return r
                    P.op("tensor", mm, reads=(s_hT, s_wr), writes=(s_pl,))
                P.op("vector", lambda e: e.tensor_copy(out=lg[:], in_=pl[:]), reads=(s_pl,), writes=(s_lg,))
                for t in range(NT):
                    P.op("vector", lambda e, t=t: e.max(out=mx[:, t, :], in_=lg[:, t, :]), reads=(s_lg,), writes=(s_lg,))
                P.op("vector", lambda e: e.tensor_tensor(out=gates[:], in0=lg[:], in1=mx[:, :, 1:2].to_broadcast([128, NT, NE]), op=ALU.is_ge),
                     reads=(s_lg,), writes=(s_gates,))
                P.op("vector", lambda e: e.tensor_tensor(out=lg[:], in0=lg[:], in1=mx[:, :, 0:1].to_broadcast([128, NT, NE]), op=ALU.subtract),
                     reads=(s_lg,), writes=(s_lg,))
                P.op("scalar", lambda e: e.activation(out=lg[:], in_=lg[:], func=AF.Exp), reads=(s_lg,), writes=(s_lg,))
                P.op("vector", lambda e: e.tensor_tensor(out=gates[:], in0=gates[:], in1=lg[:], op=ALU.mult), reads=(s_lg, s_gates), writes=(s_gates,))
                P.op("vector", lambda e: e.tensor_reduce(out=sm[:, :, 0], in_=gates[:], axis=AX.X, op=ALU.add), reads=(s_gates,), writes=(s_lg,))
                P.op("vector", lambda e: e.reciprocal(out=sm[:, :, 1], in_=sm[:, :, 0]), reads=(s_lg,), writes=(s_lg,))
                P.op("vector", lambda e: e.tensor_tensor(out=gates[:], in0=gates[:], in1=sm[:, :, 1:2].to_broadcast([128, NT, NE]), op=ALU.mult),
                     reads=(s_lg, s_gates), writes=(s_gates,))
            es_tp.close()
            NPG = 4 if router is None else 3
            wg = [self.sb(es, f"wg{i}", [128, 8, 512], BF16) for i in range(2)]
            wu = [self.sb(es, f"wu{i}", [128, 8, 512], BF16) for i in range(2)]
            wd = [self.sb(es, f"wd{i}", [128, 4, D], BF16) for i in range(2)]
            s_wg, s_wu, s_wd = P.slots_n(2), P.slots_n(2), P.slots_n(2)
            pgu = [self.ps(es, f"pgu{i}", [128, 512]) for i in range(NPG)]
            s_pgu = P.slots_n(NPG)
            pd = [self.ps(es, f"pd{i}", [128, 1024]) for i in range(2)]
            s_pd = P.slots_n(2)
            sgl = [self.sb(es, f"sgl{i}", [128, 512], F32) for i in range(2)]
            s_sgl = P.slots_n(2)
            act = [self.sb(es, f"act{i}", [128, 4, 512], BF16) for i in range(2)]
            s_act = P.slots_n(2)
            sets = []
            for ei, (wga, wua, wda, F) in enumerate(experts):
                nch = F // 128
                for c0 in range(0, nch, 4):
                    sets.append((ei, c0, min(4, nch - c0)))
            c_gu = 0
            c_act = 0
            c_pd = 0
            first = [True] * NT
            rch = [(c, min(512, R - c)) for c in range(0, R, 512)]

            def load_set(si):
                ei, c0, nc_ = sets[si]
                wga, wua, wda, F = experts[ei]
                b = si % 2
                P.dma("gpsimd", wg[b][:, :, 0:nc_ * 128], wga.rearrange("(k p) n -> p k n", p=128)[:, :, c0 * 128:(c0 + nc_) * 128], s_wg[b], True)
                P.dma("gpsimd", wu[b][:, :, 0:nc_ * 128], wua.rearrange("(k p) n -> p k n", p=128)[:, :, c0 * 128:(c0 + nc_) * 128], s_wu[b], True)
                P.dma("gpsimd", wd[b][:, 0:nc_, :], wda[c0 * 128:(c0 + nc_) * 128, :].rearrange("(k p) n -> p k n", p=128), s_wd[b], True)
            load_set(0)
            cpd = {"c": 0}
            pend_d = []
            for si, (ei, c0, nc_) in enumerate(sets):
                b = si % 2
                while pend_d:
                    pend_d.pop(0)()
                if si + 1 < len(sets):
                    load_set(si + 1)
                for (rc0, rn) in rch:
                    ab = c_act % 2
                    c_act += 1
                    for j in range(nc_):
                        ig = c_gu % NPG
                        iu = (c_gu + 1) % NPG
                        c_gu += 2

                        def mmg(e, j=j, ig=ig, b=b):
                            for k in range(8):
                                r = e.matmul(pgu[ig][:, 0:rn], wg[b][:, k, j * 128:(j + 1) * 128], hT[:, k, rc0:rc0 + rn], start=(k == 0), stop=(k == 7))
                            return r

                        def mmu(e, j=j, iu=iu, b=b):
                            for k in range(8):
                                r = e.matmul(pgu[iu][:, 0:rn], wu[b][:, k, j * 128:(j + 1) * 128], hT[:, k, rc0:rc0 + rn], start=(k == 0), stop=(k == 7))
                            return r
                        P.op("tensor", mmg, reads=(s_wg[b], s_hT), writes=(s_pgu[ig],))
                        P.op("tensor", mmu, reads=(s_wu[b], s_hT), writes=(s_pgu[iu],))
                        k2 = j % 2
                        P.op("scalar", lambda e, ig=ig, k2=k2: e.activation(out=sgl[k2][:, 0:rn], in_=pgu[ig][:, 0:rn], func=AF.Silu),
                             reads=(s_pgu[ig],), writes=(s_sgl[k2],))
                        P.op("vector", lambda e, iu=iu, k2=k2, j=j, ab=ab: e.tensor_tensor(
                            out=act[ab][:, j, 0:rn], in0=pgu[iu][:, 0:rn], in1=sgl[k2][:, 0:rn], op=ALU.mult),
                            reads=(s_pgu[iu], s_sgl[k2]), writes=(s_act[ab],))
                    def down_part(rc0=rc0, rn=rn, ab=ab, b=b, ei=ei, nc_=nc_):
                        for tt in range(rn // 128):
                            t = rc0 // 128 + tt
                            ip = cpd["c"] % 2
                            cpd["c"] += 1

                            def mmd(e, tt=tt, ip=ip, ab=ab, b=b):
                                for half in range(2):
                                    for j in range(nc_):
                                        r = e.matmul(pd[ip][:, half * 512:(half + 1) * 512], act[ab][:, j, tt * 128:(tt + 1) * 128],
                                                     wd[b][:, j, half * 512:(half + 1) * 512], start=(j == 0), stop=(j == nc_ - 1))
                                return r
                            P.op("tensor", mmd, reads=(s_wd[b], s_act[ab]), writes=(s_pd[ip],))
                            if gates is not None:
                                gsc = gates[:, t, ei:ei + 1]
                                rd = (s_pd[ip], s_gates)
                                if first[t]:
                                    P.op("vector", lambda e, t=t, ip=ip, gsc=gsc: e.tensor_scalar(
                                        out=accs[:, t, :], in0=pd[ip][:], scalar1=gsc, scalar2=None, op0=ALU.mult), reads=rd, writes=(s_acc[t],))
                                else:
                                    P.op("vector", lambda e, t=t, ip=ip, gsc=gsc: e.scalar_tensor_tensor(
                                        out=accs[:, t, :], in0=pd[ip][:], scalar=gsc, in1=accs[:, t, :], op0=ALU.mult, op1=ALU.add),
                                        reads=rd + (s_acc[t],), writes=(s_acc[t],))
                            else:
                                if first[t]:
                                    P.op("vector", lambda e, t=t, ip=ip: e.tensor_copy(out=accs[:, t, :], in_=pd[ip][:]), reads=(s_pd[ip],), writes=(s_acc[t],))
                                else:
                                    P.op("vector", lambda e, t=t, ip=ip: e.tensor_tensor(out=accs[:, t, :], in0=pd[ip][:], in1=accs[:, t, :], op=ALU.add),
                                         reads=(s_pd[ip], s_acc[t]), writes=(s_acc[t],))
                            first[t] = False
                    if pend_d:
                        pend_d.pop(0)()
                    pend_d.append(down_part)
            while pend_d:
                pend_d.pop(0)()
            xt = st["x"]
            s_xt = st["s_x"]
            t = 0
            for (r0, n, kind) in groups:
                G, sG = ml5["ctx" if kind == "ctx" else "lat"]
                for tt in range(n // 128):
                    i = t % 2
                    P.dma("sync", xt[i][:], xsrc[r0 + tt * 128:r0 + (tt + 1) * 128, :], s_xt[i], True)
                    P.op("gpsimd", lambda e, t=t: e.tensor_tensor(out=accs[:, t, :], in0=accs[:, t, :], in1=G[:], op=ALU.mult),
                         reads=(s_acc[t], sG), writes=(s_acc[t],))
                    P.op("vector", lambda e, t=t, i=i: e.tensor_tensor(out=accs[:, t, :], in0=accs[:, t, :], in1=xt[i][:], op=ALU.add),
                         reads=(s_acc[t], s_xt[i]), writes=(s_acc[t],))
                    P.dma("sync", xdst_fn(r0 + tt * 128), accs[:, t, :], s_acc[t], False)
                    t += 1
            P.flush()

    def build(self):
        upto = self.upto
        stage = [0]

        def done():
            stage[0] += 1
            return upto is not None and stage[0] >= upto
        self.phase_const()
        ALLK = ("ctx", "own", "oth")
        self.phase_mod((0, 1))
        if done(): return
        self.phase_kvq(0, self.x0, ALLK)
        if done(): return
        self.phase_glu(0, self.x0, set(range(9)), ALLK, do_flush=False)
        self.P.drain("sync")
        self.P.drain("vector")
        self.phase_fix()
        if done(): return
        self.phase_conv(0, ALLK)
        if done(): return
        self.phase_attn(0, ALLK)
        if done(): return
        self.phase_merge(0, self.x0, self.XS, ALLK)
        if done(): return
        dense = [(self.w_ff_gate[0], self.w_ff_up[0], self.w_ff_down[0], DFF)]
        xs_fn = lambda r: self.XS[r:r + 128, :]
        self.phase_ffn(0, GROUPS[0:3], self.XS, xs_fn, dense, None)
        self.phase_ffn(0, GROUPS[3:6], self.XS, xs_fn, dense, None)
        self.phase_ffn(0, GROUPS[6:9], self.XS, xs_fn, dense, None)
        if done(): return
        self.phase_kvq(1, self.XS, ("own",))
        self.phase_glu(1, self.XS, {1, 2, 3, 4, 5, 8}, ("own",), do_flush=False)
        self.P.drain("sync")
        self.P.drain("vector")
        self.phase_fix()
        self.phase_conv(1, ("own",))
        if done(): return
        self.phase_attn(1, ("own",))
        self.phase_merge(1, self.XS, self.XS, ("own",))
        if done(): return
        experts = [(self.w_exp_gate[0, e], self.w_exp_up[0, e], self.w_exp_down[0, e], DFE) for e in range(NE)]
        out_fn = lambda r: self.out[r - CTX:r - CTX + 128, :]
        self.phase_ffn(1, GROUPS[1:5], self.XS, out_fn, experts, self.w_router[0])


def build_nc(upto=None):
    nc = bass.Bass("TRN2", target_bir_lowering=False)
    b = Builder(nc, upto)
    for nm in ("kvq", "glu", "conv", "attn", "merge", "ffn", "mod"):
        if nm in SKIP:
            setattr(b, "phase_" + nm, lambda *a, **k: None)
    b.build()
    return nc


def rope_tables():
    S, GW, RA = 4096, 64, 32
    t = np.arange(S)
    row = (t // GW).astype(np.float32)
    col = (t % GW).astype(np.float32)
    inv = (10000.0 ** (-np.arange(0, RA, 2, dtype=np.float32) / RA)).astype(np.float32)
    ar = row[:, None] * inv
    ac = col[:, None] * inv
    ang = np.concatenate([ar, ar, ac, ac], axis=-1).astype(np.float32)
    cos = np.cos(ang).astype(np.float32)
    sin = np.sin(ang).astype(np.float32)
    ss = sin.copy()
    ss4 = ss.reshape(S, 2, 2, 16)
    ss4[:, :, 0, :] *= -1.0
    return cos, ss4.reshape(S, 64)


def make_in_maps(inputs):
    f = lambda a: np.ascontiguousarray(np.asarray(a, dtype=np.float32))
    x = f(inputs["x"]); c = f(inputs["c"]); ctx = f(inputs["ctx"]); c_ctx = f(inputs["c_ctx"])
    cos, ss = rope_tables()
    shared = {}
    for k in ("w_mod", "b_mod", "g_mix", "w_in", "q_norm_g", "k_norm_g", "lambda_q1", "lambda_k1", "lambda_q2",
              "lambda_k2", "subln_g", "w_attn_o", "w_conv_o", "w_out", "g_ffn", "w_ff_gate", "w_ff_up", "w_ff_down",
              "w_router", "w_exp_gate", "w_exp_up", "w_exp_down"):
        shared[k] = f(inputs[k])
    dw = f(inputs["dw_weight"])
    shared["dwT"] = np.ascontiguousarray(dw.reshape(2, 31, 8, 128).transpose(0, 3, 2, 1))
    cv = np.stack([f(inputs["dw_bias"]), f(inputs["conv_ln_g"]), f(inputs["conv_ln_b"])], axis=1)
    shared["cvp"] = np.ascontiguousarray(cv.reshape(2, 3, 8, 128).transpose(0, 3, 1, 2))
    shared["identf"] = np.eye(128, dtype=np.float32)
    maps = []
    for core in range(8):
        b, h = core // 2, core % 2
        own = slice(h * HALF, (h + 1) * HALF)
        oth = slice((1 - h) * HALF, (2 - h) * HALF)
        m = dict(shared)
        m["x0"] = np.ascontiguousarray(np.concatenate([ctx[b], x[b, own], x[b, oth]], axis=0))
        cvec = np.stack([c[b].reshape(8, 128).T, c_ctx.reshape(8, 128).T], axis=2).reshape(128, 16)
        m["cvec"] = np.ascontiguousarray(cvec)
        rp = np.zeros((RALL, 2, 64), np.float32)
        rp[:CTX, 0, :] = 1.0
        rp[CTX:CTX + HALF, 0, :] = cos[own]; rp[CTX:CTX + HALF, 1, :] = ss[own]
        rp[CTX + HALF:, 0, :] = cos[oth]; rp[CTX + HALF:, 1, :] = ss[oth]
        m["rope"] = rp
        mk = np.zeros((128, 2), np.float32)
        mk[:, 0] = float(h); mk[:, 1] = float(1 - h)
        m["mask"] = mk
        maps.append(m)
    return maps


def kernel(**inputs):
    maps = make_in_maps(inputs)
    nc = build_nc()
    res = run_bass_kernel_spmd(nc, maps, core_ids=list(range(8)))
    out = np.zeros((4, 4096, D), np.float32)
    for core in range(8):
        b, h = core // 2, core % 2
        out[b, h * HALF:(h + 1) * HALF] = res.results[core]["out"]
    return out
```

```python
import math
import types
from contextlib import ExitStack
import numpy as np
import concourse.bass as bass
import concourse.mybir as mybir
from concourse.bass_utils import run_bass_kernel_spmd

F32 = mybir.dt.float32
BF16 = mybir.dt.bfloat16
AF = mybir.ActivationFunctionType
ALU = mybir.AluOpType
AX = mybir.AxisListType

D = 1024
CTX = 256
HALF = 2048
RALL = CTX + 2 * HALF
NH = 8
INW = 7168
DFF = 2816
DFE = 3584
NE = 8
EPS = 1e-6
UT_W = 4448
DEBUG = False
SKIP = set()


def ucol(r):
    if r < CTX:
        return 16 + r
    if r < CTX + HALF:
        return 288 + 16 + (r - CTX)
    return 2368 + 16 + (r - CTX - HALF)


GROUPS = [(0, 256, "ctx")] + [(CTX + 512 * i, 512, "own") for i in range(4)] + \
         [(CTX + HALF + 512 * i, 512, "oth") for i in range(4)]


def freeze(fn):
    if fn.__closure__ is None:
        return fn
    cells = []
    for c in fn.__closure__:
        try:
            cells.append(types.CellType(c.cell_contents))
        except ValueError:
            cells.append(c)
    return types.FunctionType(fn.__code__, fn.__globals__, fn.__name__, fn.__defaults__, tuple(cells))


class Sem:
    def __init__(self, h):
        self.h = h
        self.val = 0


class Slot:
    def __init__(self, name):
        self.name = name
        self.w = {}
        self.r = {}
        self.ld = None
        self.st = None


class Prog:
    CE = ("tensor", "vector", "scalar", "gpsimd")
    ENGS = ("tensor", "vector", "scalar", "gpsimd", "sync")

    def __init__(self, nc, n_hw=40, n_sw=10):
        self.nc = nc
        self.psets = [{e: Sem(nc.alloc_semaphore(name=f"pg{i}_{e}")) for e in self.CE} for i in range(2)]
        self.cur = 1
        self.pools = {"sync": [Sem(nc.alloc_semaphore(name=f"dh_{i}")) for i in range(n_hw)],
                      "gpsimd": [Sem(nc.alloc_semaphore(name=f"ds_{i}")) for i in range(n_sw)]}
        self.rr = {"sync": 0, "gpsimd": 0}
        self.reset()

    def reset(self):
        self.q = {e: [] for e in self.ENGS}
        self.waited = {e: {} for e in self.ENGS}
        self.cur = 1 - self.cur
        self.psem = self.psets[self.cur]
        for sm in self.psem.values():
            sm.val = 0
        self.slots = []

    def drain(self, eng="sync"):
        for pool in self.pools.values():
            for sem in pool:
                if sem.val > 0:
                    self._wait(eng, (sem, sem.val))

    def slot(self, name="s"):
        sl = Slot(name)
        self.slots.append(sl)
        return sl

    def slots_n(self, n, name="s"):
        return [self.slot(f"{name}{i}") for i in range(n)]

    def _wait(self, eng, tk):
        sem, val = tk
        if eng == "tensor" and sem is self.psem["tensor"]:
            return
        if self.waited[eng].get(id(sem), 0) >= val:
            return
        self.waited[eng][id(sem)] = val
        self.q[eng].append(lambda e, sem=sem, val=val: e.wait_ge(sem.h, val))

    def _deps(self, eng, reads, writes):
        for sl in reads:
            for tk in sl.w.values():
                self._wait(eng, tk)
        for sl in writes:
            for tk in sl.w.values():
                self._wait(eng, tk)
            for tk in sl.r.values():
                self._wait(eng, tk)

    def _commit(self, tk, reads, writes):
        key = id(tk[0])
        for sl in reads:
            sl.r[key] = tk
        for sl in writes:
            if sl.r:
                sl.w = {}
                sl.r = {}
            sl.w[key] = tk

    def op(self, eng, fn, reads=(), writes=()):
        self._deps(eng, reads, writes)
        fn = freeze(fn)
        sem = self.psem[eng]
        sem.val += 1
        tk = (sem, sem.val)
        self.q[eng].append(lambda e, fn=fn, sem=sem: fn(e).then_inc(sem.h, 1))
        self._commit(tk, reads, writes)
        return tk

    def dma(self, eng, out, in_, slot, load):
        if load:
            self._deps(eng, (), (slot,))
        else:
            self._deps(eng, (slot,), ())
        pool = self.pools[eng]
        sem = pool[self.rr[eng] % len(pool)]
        self.rr[eng] += 1
        if sem.val > 0:
            self._wait(eng, (sem, sem.val))
        sem.val += 16
        tk = (sem, sem.val)
        self.q[eng].append(
            lambda e, out=out, in_=in_, sem=sem: e.dma_start(out=out, in_=in_).then_inc(sem.h, 16))
        if load:
            self._commit(tk, (), (slot,))
        else:
            self._commit(tk, (slot,), ())
        return tk

    def flush(self):
        for pool in self.pools.values():
            for sem in pool:
                if sem.val > 0:
                    self._wait("sync", (sem, sem.val))
        nc = self.nc
        others = list(self.psets[1 - self.cur].values())
        self.q["vector"].insert(0, lambda e: [e.sem_clear(sm.h) for sm in others])
        with nc.Block() as block:
            for eng in self.ENGS:
                items = self.q[eng]

                def body(e, items=items):
                    for it in items:
                        it(e)
                getattr(block, eng)(body)
        self.reset()


class Builder:
    def __init__(self, nc, upto=None):
        self.nc = nc
        self.upto = upto
        self.P = Prog(nc)
        dt_in = lambda name, shape, dt=F32: nc.dram_tensor(name, list(shape), dt, kind="ExternalInput").ap()
        kind_s = "ExternalOutput"
        dt_sc = lambda name, shape, dt: nc.dram_tensor(name, list(shape), dt, kind=kind_s).ap()
        self.x0 = dt_in("x0", [RALL, D])
        self.cvec = dt_in("cvec", [128, 16])
        self.rope = dt_in("rope", [RALL, 2, 64])
        self.mask = dt_in("mask", [128, 2])
        self.identf = dt_in("identf", [128, 128])
        self.w_mod = dt_in("w_mod", [2, D, 6 * D])
        self.b_mod = dt_in("b_mod", [2, 6 * D])
        self.g_mix = dt_in("g_mix", [2, D])
        self.w_in = dt_in("w_in", [2, D, INW])
        self.q_norm_g = dt_in("q_norm_g", [2, 64])
        self.k_norm_g = dt_in("k_norm_g", [2, 64])
        self.lams = [dt_in(n, [2, 64]) for n in ("lambda_q1", "lambda_k1", "lambda_q2", "lambda_k2")]
        self.subln_g = dt_in("subln_g", [2, 128])
        self.w_attn_o = dt_in("w_attn_o", [2, D, D])
        self.dwT = dt_in("dwT", [2, 128, 8, 31])
        self.cvp = dt_in("cvp", [2, 128, 3, 8])
        self.w_conv_o = dt_in("w_conv_o", [2, D, D])
        self.w_out = dt_in("w_out", [2, D, D])
        self.g_ffn = dt_in("g_ffn", [2, D])
        self.w_ff_gate = dt_in("w_ff_gate", [1, D, DFF])
        self.w_ff_up = dt_in("w_ff_up", [1, D, DFF])
        self.w_ff_down = dt_in("w_ff_down", [1, DFF, D])
        self.w_router = dt_in("w_router", [1, D, NE])
        self.w_exp_gate = dt_in("w_exp_gate", [1, NE, D, DFE])
        self.w_exp_up = dt_in("w_exp_up", [1, NE, D, DFE])
        self.w_exp_down = dt_in("w_exp_down", [1, NE, DFE, D])
        self.out = nc.dram_tensor("out", [HALF, D], F32, kind="ExternalOutput").ap()
        self.XS = dt_sc("XS", [RALL, D], F32)
        self.MODV = dt_sc("MODV", [2, 2, 6 * D], F32)
        self.KT = dt_sc("KT", [NH, 128, RALL], BF16)
        self.QT = dt_sc("QT", [NH, 128, RALL], BF16)
        self.V = dt_sc("V", [RALL, D], BF16)
        self.UT = dt_sc("UT", [8, 128, UT_W], BF16)
        self.GT = dt_sc("GT", [16, 128, RALL], BF16)
        self.MC = dt_sc("MC", [8, 128, RALL], BF16)
        self.OT = dt_sc("OT", [NH, 128, RALL], BF16)
        self.ident = nc.alloc_sbuf_tensor("ident", [128, 128], BF16)
        self.onesf = nc.alloc_sbuf_tensor("onesf", [128, 128], F32)
        self.nhalf = nc.alloc_sbuf_tensor("nhalf", [128, 64], F32)
        self.maskt = nc.alloc_sbuf_tensor("maskt", [128, 2], F32)

    def sb(self, es, name, shape, dt):
        self.uid = getattr(self, "uid", 0) + 1
        return es.enter_context(self.nc.sbuf_tensor(f"{name}_{self.uid}", list(shape), dt))

    def ps(self, es, name, shape, dt=F32):
        self.uid = getattr(self, "uid", 0) + 1
        return es.enter_context(self.nc.psum_tensor(f"{name}_{self.uid}", list(shape), dt))

    def rsqrt(self, src_ap, dst_ap, tmp_ap, n_inv, slots_r, slots_w, tmp_slot, width, mode="act"):
        P = self.P
        P.op("vector", lambda e: e.tensor_scalar(out=tmp_ap, in0=src_ap, scalar1=n_inv, scalar2=EPS,
                                                 op0=ALU.mult, op1=ALU.add),
             reads=slots_r, writes=(tmp_slot,))
        if mode == "pool":
            nh = self.nhalf[:, 0:width]
            P.op("gpsimd", lambda e: e.tensor_tensor(out=dst_ap, in0=tmp_ap, in1=nh, op=ALU.pow),
                 reads=(tmp_slot,), writes=slots_w)
        else:
            P.op("scalar", lambda e: e.activation(out=tmp_ap, in_=tmp_ap, func=AF.Sqrt), reads=(tmp_slot,), writes=(tmp_slot,))
            P.op("vector", lambda e: e.reciprocal(out=dst_ap, in_=tmp_ap), reads=(tmp_slot,), writes=slots_w)

    def load_mod_tile(self, es, l, v, who, name, gbc=None, gslot=None):
        P = self.P
        t = self.sb(es, name, [128, D], F32)
        sl = P.slot(name)
        src = self.MODV[l, who, v * D:(v + 1) * D].partition_broadcast(128)
        P.dma("sync", t[:], src, sl, True)
        if gbc is not None:
            P.op("vector", lambda e: e.scalar_tensor_tensor(out=t[:], in0=t[:], scalar=1.0, in1=gbc[:],
                                                            op0=ALU.add, op1=ALU.mult),
                 reads=(gslot,), writes=(sl,))
        return t, sl

    def load_w(self, dst_tile, dst_slot, src_ap, kchunks, ncols, piece=512, order=None):
        P = self.P
        src = src_ap.rearrange("(k p) n -> p k n", p=128)
        npieces = (ncols + piece - 1) // piece
        slots = [dst_slot] * npieces if dst_slot is not None else P.slots_n(npieces, "wp")
        for pi in (order if order is not None else range(npieces)):
            c0 = pi * piece
            c1 = min(ncols, c0 + piece)
            P.dma("gpsimd", dst_tile[:, 0:kchunks, c0:c1], src[:, :, c0:c1], slots[pi], True)
        return slots

    def phase_const(self):
        P = self.P
        with ExitStack() as es:
            sl = P.slot("c")
            sl2 = P.slot("c2")
            P.dma("gpsimd", self.ident[:], self.identf[:, :], sl2, True)
            P.dma("sync", self.maskt[:], self.mask[:, :], sl, True)
            P.op("vector", lambda e: e.memset(self.onesf[:], 1.0), writes=(sl,))
            P.op("vector", lambda e: e.memset(self.nhalf[:], -0.5), writes=(sl,))
            P.flush()

    def phase_mod(self, layers=(0, 1)):
        P = self.P
        with ExitStack() as es:
            cv = self.sb(es, "cv", [128, 16], F32)
            sg = self.sb(es, "sgm", [128, 16], F32)
            sv = self.sb(es, "sv", [128, 16], BF16)
            bm = [self.sb(es, f"bm{l}", [2, 6 * D], F32) for l in layers]
            mv = [self.sb(es, f"mv{l}", [2, 6 * D], F32) for l in layers]
            wm = [self.sb(es, f"wm{i}", [128, 8, 512], BF16) for i in range(3)]
            pm = [self.ps(es, f"pm{i}", [128, 512]) for i in range(2)]
            s_cv, s_sv = P.slot(), P.slot()
            s_bm, s_mv = P.slots_n(len(layers)), P.slots_n(len(layers))
            s_wm = P.slots_n(3)
            s_pm = P.slots_n(2)
            P.dma("sync", cv[:], self.cvec[:, :], s_cv, True)
            P.op("scalar", lambda e: e.activation(out=sg[:], in_=cv[:], func=AF.Sigmoid), reads=(s_cv,), writes=(s_sv,))
            P.op("vector", lambda e: e.tensor_tensor(out=sv[:], in0=cv[:], in1=sg[:], op=ALU.mult),
                 reads=(s_cv, s_sv), writes=(s_sv,))
            cnt = 0
            for li, l in enumerate(layers):
                P.dma("sync", bm[li][:], self.b_mod[l].partition_broadcast(2), s_bm[li], True)
                wsrc = self.w_mod[l].rearrange("(k p) n -> p k n", p=128)
                for cg in range(12):
                    i = cnt % 3
                    ip = cnt % 2
                    cnt += 1
                    P.dma("gpsimd", wm[i][:], wsrc[:, :, cg * 512:(cg + 1) * 512], s_wm[i], True)

                    def mm(e, i=i, ip=ip):
                        for k in range(8):
                            r = e.matmul(pm[ip][0:2, :], sv[:, 2 * k:2 * k + 2], wm[i][:, k, :], start=(k == 0), stop=(k == 7))
                        return r
                    P.op("tensor", mm, reads=(s_sv, s_wm[i]), writes=(s_pm[ip],))
                    P.op("vector", lambda e, ip=ip, cg=cg, li=li: e.tensor_tensor(
                        out=mv[li][0:2, cg * 512:(cg + 1) * 512], in0=pm[ip][0:2, :], in1=bm[li][0:2, cg * 512:(cg + 1) * 512],
                        op=ALU.add), reads=(s_pm[ip], s_bm[li]), writes=(s_mv[li],))
                P.dma("sync", self.MODV[l], mv[li][:], s_mv[li], False)
            P.flush()

    def setup_norm(self, es, l, which, whos=("lat", "ctx"), ntp=2, es_tp=None):
        P = self.P
        gsrc = (self.g_mix if which == 0 else self.g_ffn)[l]
        gbc = self.sb(es, "gbc", [128, D], F32)
        s_g = P.slot("gbc")
        P.dma("sync", gbc[:], gsrc.partition_broadcast(128), s_g, True)
        vb = 0 if which == 0 else 3
        res = {}
        for who, nm in ((0, "lat"), (1, "ctx")):
            if nm not in whos:
                continue
            B, sB = self.load_mod_tile(es, l, vb, who, f"B_{nm}")
            A, sA = self.load_mod_tile(es, l, vb + 1, who, f"A_{nm}", gbc, s_g)
            res[nm] = (A, sA, B, sB)
        st = dict(mod=res)
        st["x"] = [self.sb(es, f"xr{i}", [128, D], F32) for i in range(2)]
        st["s_x"] = P.slots_n(2, "x")
        st["junk"] = self.sb(es, "junk", [128, D], BF16)
        st["s_junk"] = P.slot("junk")
        st["ss"] = [self.sb(es, f"ss{i}", [128, 4], F32) for i in range(2)]
        st["s_ss"] = P.slots_n(2, "ss")
        st["hf"] = self.sb(es, "hf", [128, D], F32)
        st["s_hf"] = P.slot("hf")
        st["h"] = [self.sb(es, f"h{i}", [128, D], BF16) for i in range(2)]
        st["s_h"] = P.slots_n(2, "h")
        st["ntp"] = ntp
        st["tp"] = [self.ps(es_tp if es_tp is not None else es, f"tp{i}", [128, 8, 128], BF16) for i in range(ntp)]
        st["s_tp"] = P.slots_n(ntp, "tp")
        st["cnt"] = 0
        st["tpc"] = 0
        return st

    def emit_hA(self, st, xsrc, r0, t, kind):
        P = self.P
        A, sA, B, sB = st["mod"]["ctx" if kind == "ctx" else "lat"]
        i = st["cnt"] % 2
        st["cnt"] += 1
        x, sx = st["x"][i], st["s_x"][i]
        ss, sss = st["ss"][i], st["s_ss"][i]
        h, sh = st["h"][i], st["s_h"][i]
        P.dma("sync", x[:], xsrc[r0 + t * 128:r0 + (t + 1) * 128, :], sx, True)
        P.op("scalar", lambda e: e.activation(out=st["junk"][:], in_=x[:], func=AF.Square, accum_out=ss[:, 0:1]),
             reads=(sx,), writes=(st["s_junk"], sss))
        self.rsqrt(ss[:, 0:1], ss[:, 2:3], ss[:, 1:2], 1.0 / D, (sss,), (sss,), sss, 1)
        P.op("vector", lambda e: e.scalar_tensor_tensor(
            out=st["hf"][:], in0=x[:], scalar=ss[:, 2:3], in1=A[:], op0=ALU.mult, op1=ALU.mult),
            reads=(sx, sss, sA), writes=(st["s_hf"],))
        P.op("gpsimd", lambda e: e.tensor_tensor(out=h[:], in0=st["hf"][:], in1=B[:], op=ALU.add),
             reads=(st["s_hf"], sB), writes=(sh,))
        return h, sh

    def emit_hT(self, st, xsrc, r0, n, kind, hT, s_hT, col0=0, tiles=None):
        for t in (range(n // 128) if tiles is None else tiles):
            h, sh = self.emit_hA(st, xsrc, r0, t, kind)
            self.transpose8(st, h, sh, hT, s_hT, col0 + t * 128)

    def h_prefetcher(self, st, xsrc, grp, hT, s_hT):
        state = {"t": 0, "pend": None}
        nT = grp[1] // 128

        def step():
            if state["pend"] is not None:
                h, sh, t = state["pend"]
                self.transpose8(st, h, sh, hT, s_hT, t * 128)
                state["pend"] = None
            if state["t"] < nT:
                t = state["t"]
                state["t"] += 1
                h, sh = self.emit_hA(st, xsrc, grp[0], t, grp[2])
                state["pend"] = (h, sh, t)
            return state["pend"] is not None or state["t"] < nT

        def finish():
            while step():
                pass
        return step, finish

    def transpose8(self, st, src, s_src, dst, s_dst, c0, eng="scalar"):
        P = self.P
        j = st["tpc"] % st["ntp"]
        st["tpc"] += 1
        tp, stp = st["tp"][j], st["s_tp"][j]

        def tr(e):
            for k in range(8):
                r = e.transpose(tp[:, k, :], src[:, k * 128:(k + 1) * 128], self.ident[:])
            return r
        P.op("tensor", tr, reads=(s_src,), writes=(stp,))
        if eng == "scalar":
            P.op("scalar", lambda e: e.copy(out=dst[:, 0:8, c0:c0 + 128], in_=tp[:]), reads=(stp,), writes=(s_dst,))
        else:
            P.op("vector", lambda e: e.tensor_copy(out=dst[:, 0:8, c0:c0 + 128], in_=tp[:]), reads=(stp,), writes=(s_dst,))

    def phase_kvq(self, l, xsrc, q_kinds):
        P = self.P
        with ExitStack() as es:
            st = self.setup_norm(es, l, 0)
            w = self.sb(es, "w", [128, 8, 3072], BF16)
            s_wp = self.load_w(w, None, self.w_in[l][:, 0:3072], 8, 3072, order=[2, 3, 4, 5, 0, 1])
            hT = [self.sb(es, f"hT{i}", [128, 8, 512], BF16) for i in range(2)]
            s_hT = P.slots_n(2, "hT")
            NTOK = 3
            tok = [self.ps(es, f"tok{i}", [128, 1024]) for i in range(NTOK)]
            s_tok = P.slots_n(NTOK, "tok")
            gq = self.sb(es, "gq", [128, 2, 64], F32)
            gsw = self.sb(es, "gsw", [128, 2, 64], F32)
            s_g = P.slot("g")
            P.dma("sync", gq[:, 0, :], self.q_norm_g[l].partition_broadcast(128), s_g, True)
            P.dma("sync", gq[:, 1, :], self.k_norm_g[l].partition_broadcast(128), s_g, True)
            g4 = gq[:].rearrange("p a (b h e) -> p (a b) h e", b=2, h=2)
            gs4 = gsw[:].rearrange("p a (b h e) -> p (a b) h e", b=2, h=2)
            P.op("vector", lambda e: e.tensor_copy(out=gs4[:, :, 0, :], in_=g4[:, :, 1, :]), reads=(s_g,), writes=(s_g,))
            P.op("vector", lambda e: e.tensor_copy(out=gs4[:, :, 1, :], in_=g4[:, :, 0, :]), reads=(s_g,), writes=(s_g,))
            rp = [self.sb(es, f"rp{i}", [128, 2, 64], F32) for i in range(2)]
            s_rp = P.slots_n(2, "rp")
            cg = [self.sb(es, f"cg{i}", [128, 2, 2, 64], F32) for i in range(2)]
            s_cg = P.slots_n(2, "cg")
            sq = [self.sb(es, f"sq{i}", [128, D], F32) for i in range(2)]
            kn = [self.sb(es, f"kn{i}", [128, D], F32) for i in range(2)]
            raw = [self.sb(es, f"raw{i}", [128, D], F32) for i in range(2)]
            s_raw = P.slots_n(2, "raw")
            t2 = [self.sb(es, f"t2{i}", [128, D], F32) for i in range(2)]
            kr = [self.sb(es, f"kr{i}", [128, D], BF16) for i in range(2)]
            s16 = [self.sb(es, f"s16{i}", [128, 3, 16], F32) for i in range(2)]
            s_sq, s_kn, s_t2, s_kr, s_s16 = (P.slots_n(2, "sq"), P.slots_n(2, "kn"), P.slots_n(2, "t2"),
                                             P.slots_n(2, "kr"), P.slots_n(2, "s16"))
            stage = [self.sb(es, f"stg{i}", [128, 8, 512], BF16) for i in range(4)]
            s_stage = P.slots_n(4, "stg")
            vst = [self.sb(es, f"vst{i}", [128, D], BF16) for i in range(2)]
            s_vst = P.slots_n(2, "vst")
            cnt = dict(tok=0, v=0, rp=0)

            def proj_tok(hTt, shT, t, col0):
                i = cnt["tok"] % NTOK
                cnt["tok"] += 1

                def mm(e):
                    for half in range(2):
                        for k in range(8):
                            r = e.matmul(tok[i][:, half * 512:(half + 1) * 512], hTt[:, k, t * 128:(t + 1) * 128],
                                         w[:, k, col0 + half * 512:col0 + (half + 1) * 512], start=(k == 0), stop=(k == 7))
                    return r
                P.op("tensor", mm, reads=(shT, s_wp[col0 // 512], s_wp[col0 // 512 + 1]), writes=(s_tok[i],))
                return tok[i], s_tok[i]

            def chain(pt, spt, b, cgt, scg, stg, sstg, t):
                s3, ss3 = s16[b], s_s16[b]
                g3 = lambda ap: ap.rearrange("p (g e) -> p g e", e=64)
                stages = []
                def st0():
                    P.op("scalar", lambda e: e.activation(out=sq[b][:], in_=pt[:], func=AF.Square), reads=(spt,), writes=(s_sq[b],))
                    P.op("scalar", lambda e: e.copy(out=raw[b][:], in_=pt[:]), reads=(spt,), writes=(s_raw[b],))
                stages.append(st0)
                stages.append(lambda: P.op("vector", lambda e: e.tensor_reduce(out=s3[:, 0, :], in_=g3(sq[b][:]), axis=AX.X, op=ALU.add),
                                           reads=(s_sq[b],), writes=(ss3,)))
                stages.append(lambda: self.rsqrt(s3[:, 0, :], s3[:, 2, :], s3[:, 1, :], 1.0 / 64, (ss3,), (ss3,), ss3, 16))
                stages.append(lambda: P.op("vector", lambda e: e.tensor_tensor(
                    out=g3(kn[b][:]), in0=g3(raw[b][:]), in1=s3[:, 2, :].unsqueeze(2).to_broadcast([128, 16, 64]), op=ALU.mult),
                    reads=(s_raw[b], ss3), writes=(s_kn[b],)))
                stages.append(lambda: P.op("gpsimd", lambda e: e.tensor_tensor(
                    out=g3(sq[b][:]), in0=g3(kn[b][:]), in1=cgt[:, b, 0, :].unsqueeze(1).to_broadcast([128, 16, 64]), op=ALU.mult),
                    reads=(s_kn[b], scg), writes=(s_sq[b],)))
                kn5 = kn[b][:].rearrange("p (g h e) -> p g h e", h=2, e=16)
                t25 = t2[b][:].rearrange("p (g h e) -> p g h e", h=2, e=16)
                sg5 = cgt[:, b, 1, :].rearrange("p (b h e) -> p b h e", h=2, e=16)

                def st_t2():
                    for hh in range(2):
                        P.op("vector", lambda e, hh=hh: e.tensor_tensor(
                            out=t25[:, :, hh, :].rearrange("p (g b) e -> p g b e", b=2),
                            in0=kn5[:, :, 1 - hh, :].rearrange("p (g b) e -> p g b e", b=2),
                            in1=sg5[:, :, hh, :].unsqueeze(1).to_broadcast([128, 16, 2, 16]), op=ALU.mult),
                            reads=(s_kn[b], scg), writes=(s_t2[b],))
                stages.append(st_t2)
                stages.append(lambda: P.op("gpsimd", lambda e: e.tensor_tensor(out=kr[b][:], in0=sq[b][:], in1=t2[b][:], op=ALU.add),
                                           reads=(s_sq[b], s_t2[b]), writes=(s_kr[b],)))
                stages.append(lambda: self.transpose8(st, kr[b], s_kr[b], stg, sstg, t * 128, eng="scalar"))
                return stages

            pending = []
            self.emit_hT(st, xsrc, GROUPS[0][0], GROUPS[0][1], GROUPS[0][2], hT[0], s_hT[0])
            for gi, (r0, n, kind) in enumerate(GROUPS):
                hTt, shT = hT[gi % 2], s_hT[gi % 2]
                nxt = GROUPS[gi + 1] if gi + 1 < len(GROUPS) else None
                need_q = kind in q_kinds
                T = n // 128
                pf_step, pf_finish = (None, None)
                if nxt is not None:
                    pf_step, pf_finish = self.h_prefetcher(st, xsrc, nxt, hT[(gi + 1) % 2], s_hT[(gi + 1) % 2])
                for t in range(T):
                    if pf_step is not None:
                        pf_step()
                    i = cnt["rp"] % 2
                    cnt["rp"] += 1
                    P.dma("sync", rp[i][:], self.rope[r0 + t * 128:r0 + (t + 1) * 128, :, :], s_rp[i], True)
                    for which in range(2):
                        if which == 0 and not need_q:
                            continue
                        P.op("gpsimd", lambda e, i=i, which=which: e.tensor_tensor(
                            out=cg[i][:, which, 0, :], in0=rp[i][:, 0, :], in1=gq[:, which, :], op=ALU.mult),
                            reads=(s_rp[i], s_g), writes=(s_cg[i],))
                        P.op("gpsimd", lambda e, i=i, which=which: e.tensor_tensor(
                            out=cg[i][:, which, 1, :], in0=rp[i][:, 1, :], in1=gsw[:, which, :], op=ALU.mult),
                            reads=(s_rp[i], s_g), writes=(s_cg[i],))
                    pk, spk = proj_tok(hTt, shT, t, 1024)
                    pv, spv = proj_tok(hTt, shT, t, 2048)
                    if need_q:
                        pq, spq = proj_tok(hTt, shT, t, 0)
                    for f in pending:
                        f()
                    pending = []
                    vi = cnt["v"] % 2
                    cnt["v"] += 1
                    P.op("scalar", lambda e, pv=pv, vi=vi: e.copy(out=vst[vi][:], in_=pv[:]), reads=(spv,), writes=(s_vst[vi],))
                    P.dma("sync", self.V[r0 + t * 128:r0 + (t + 1) * 128, :], vst[vi][:], s_vst[vi], False)
                    sk = (gi % 2) * 2 + 1
                    sq_ = (gi % 2) * 2
                    chains = [chain(pk, spk, 1, cg[i], s_cg[i], stage[sk], s_stage[sk], t)]
                    if need_q:
                        chains.append(chain(pq, spq, 0, cg[i], s_cg[i], stage[sq_], s_stage[sq_], t))
                    nst = len(chains[0])
                    for si in range(nst - 1):
                        for ch in chains:
                            ch[si]()
                    for ch in chains:
                        pending.append(ch[nst - 1])
                    if t == T - 1 and pf_finish is not None:
                        pf_finish()
                    if t == T - 1:
                        def stores(r0=r0, n=n, sk=sk, sq_=sq_, need_q=need_q):
                            P.dma("sync", self.KT[:, :, r0:r0 + n].rearrange("h p r -> p h r"), stage[sk][:, :, 0:n], s_stage[sk], False)
                            if need_q:
                                P.dma("sync", self.QT[:, :, r0:r0 + n].rearrange("h p r -> p h r"), stage[sq_][:, :, 0:n], s_stage[sq_], False)
                        pending.append(stores)
            for f in pending:
                f()
            P.flush()

    def phase_glu(self, l, xsrc, glu_groups, gate_kinds, do_flush=True):
        P = self.P
        with ExitStack() as es:
            st = self.setup_norm(es, l, 0)
            w = self.sb(es, "w", [128, 8, 4096], BF16)
            s_wp = self.load_w(w, None, self.w_in[l][:, 3072:INW], 8, 4096, order=[0, 2, 1, 3, 4, 5, 6, 7])
            hT = [self.sb(es, f"hT{i}", [128, 8, 512], BF16) for i in range(2)]
            s_hT = P.slots_n(2, "hT")
            fm = [self.ps(es, f"fm{i}", [128, 512]) for i in range(4)]
            s_fm = P.slots_n(4, "fm")
            sgt = [self.sb(es, f"sgt{i}", [128, 512], F32) for i in range(2)]
            s_sgt = P.slots_n(2, "sgt")
            stage = [self.sb(es, f"stg{i}", [128, 8, 512], BF16) for i in range(3)]
            s_stage = P.slots_n(3, "stg")
            cnt = dict(fm=0, sg=0, stage=0, g=0)

            def proj_fm(hTt, shT, col0, n):
                i = cnt["fm"] % 4
                cnt["fm"] += 1

                def mm(e):
                    for k in range(8):
                        r = e.matmul(fm[i][:, 0:n], w[:, k, col0:col0 + 128], hTt[:, k, 0:n], start=(k == 0), stop=(k == 7))
                    return r
                P.op("tensor", mm, reads=(shT, s_wp[col0 // 512]), writes=(s_fm[i],))
                return fm[i], s_fm[i]

            work = [(gi, g) for gi, g in enumerate(GROUPS) if (gi in glu_groups or g[2] in gate_kinds)]
            pre = {"step": None, "finish": None}

            def prefetch():
                if pre["step"] is not None:
                    pre["step"]()
            g0 = work[0][1]
            self.emit_hT(st, xsrc, g0[0], g0[1], g0[2], hT[0], s_hT[0])
            for wi, (gi, (r0, n, kind)) in enumerate(work):
                need_glu = gi in glu_groups
                need_gate = kind in gate_kinds
                hTt, shT = hT[wi % 2], s_hT[wi % 2]
                if pre["finish"] is not None:
                    pre["finish"]()
                pre["step"] = pre["finish"] = None
                if wi + 1 < len(work):
                    b = (wi + 1) % 2
                    pre["step"], pre["finish"] = self.h_prefetcher(st, xsrc, work[wi + 1][1], hT[b], s_hT[b])
                if need_glu:
                    si = cnt["stage"] % 3
                    cnt["stage"] += 1
                    for j in range(8):
                        if j % 2 == 0:
                            prefetch()
                        pa, spa = proj_fm(hTt, shT, j * 128, n)
                        pg, spg = proj_fm(hTt, shT, 1024 + j * 128, n)
                        k = cnt["sg"] % 2
                        cnt["sg"] += 1
                        P.op("scalar", lambda e, pg=pg, k=k: e.activation(out=sgt[k][:, 0:n], in_=pg[:, 0:n], func=AF.Sigmoid),
                             reads=(spg,), writes=(s_sgt[k],))
                        P.op("vector", lambda e, pa=pa, k=k, j=j, si=si: e.tensor_tensor(
                            out=stage[si][:, j, 0:n], in0=pa[:, 0:n], in1=sgt[k][:, 0:n], op=ALU.mult),
                            reads=(spa, s_sgt[k]), writes=(s_stage[si],))
                    c0 = ucol(r0)
                    P.dma("sync", self.UT[:, :, c0:c0 + n].rearrange("j p r -> p j r"), stage[si][:, :, 0:n], s_stage[si], False)
                if need_gate:
                    for half in range(2):
                        si = cnt["stage"] % 3
                        cnt["stage"] += 1
                        for j in range(8):
                            if j % 2 == 1:
                                prefetch()
                            pg, spg = proj_fm(hTt, shT, 2048 + (half * 8 + j) * 128, n)
                            P.op("scalar", lambda e, pg=pg, j=j, si=si: e.activation(
                                out=stage[si][:, j, 0:n], in_=pg[:, 0:n], func=AF.Sigmoid),
                                reads=(spg,), writes=(s_stage[si],))
                        P.dma("sync", self.GT[half * 8:(half + 1) * 8, :, r0:r0 + n].rearrange("j p r -> p j r"),
                              stage[si][:, :, 0:n], s_stage[si], False)
            if do_flush:
                P.flush()

    def phase_fix(self):
        P = self.P
        if "fix" in SKIP:
            return
        with ExitStack() as es:
            e_in = self.sb(es, "ein", [128, 8, 4, 16], BF16)
            e_out = self.sb(es, "eout", [128, 8, 6, 16], BF16)
            s_in, s_out = P.slot(), P.slot()
            own0, oth0 = 288, 2368
            srcs = [oth0 + 16 + HALF - 16, oth0 + 16, own0 + 16 + HALF - 16, own0 + 16]
            for i, c in enumerate(srcs):
                P.dma("sync", e_in[:, :, i, :], self.UT[:, :, c:c + 16].rearrange("j p r -> p j r"), s_in, True)
            mL, mR = self.maskt[:, 0:1], self.maskt[:, 1:2]
            plan = [(0, mL), (1, mR), (2, mR), (3, mL)]
            dsts = [own0, own0 + 16 + HALF, oth0, oth0 + 16 + HALF, 0, 16 + CTX]
            for i, (srci, m) in enumerate(plan):
                P.op("vector", lambda e, i=i, srci=srci, m=m: e.tensor_scalar(
                    out=e_out[:, :, i, :], in0=e_in[:, :, srci, :], scalar1=m, scalar2=None, op0=ALU.mult),
                    reads=(s_in,), writes=(s_out,))
            P.op("vector", lambda e: e.memset(e_out[:, :, 4:6, :], 0.0), writes=(s_out,))
            for i, c in enumerate(dsts):
                P.dma("sync", self.UT[:, :, c:c + 16].rearrange("j p r -> p j r"), e_out[:, :, i, :], s_out, False)
            P.flush()

    def phase_conv(self, l, kinds):
        P = self.P
        with ExitStack() as es:
            dw = self.sb(es, "dw", [128, 8, 31], F32)
            cvp = self.sb(es, "cvp", [128, 3, 8], F32)
            idf = self.sb(es, "idf", [128, 128], F32)
            s_c = P.slot("c")
            P.dma("sync", dw[:], self.dwT[l], s_c, True)
            P.dma("sync", cvp[:], self.cvp[l], s_c, True)
            P.dma("sync", idf[:], self.identf[:, :], s_c, True)
            diag = self.sb(es, "diag", [128, 8 * 31, 128], BF16)
            s_diag = P.slots_n(8, "diag")
            for j in range(8):
                eng = "vector" if (j % 2 == 0) else "gpsimd"
                P.op(eng, lambda e, j=j: e.tensor_tensor(
                    out=diag[:, j * 31:(j + 1) * 31, :], in0=idf[:].unsqueeze(1).to_broadcast([128, 31, 128]),
                    in1=dw[:, j, :].unsqueeze(2).to_broadcast([128, 31, 128]), op=ALU.mult),
                    reads=(s_c,), writes=(s_diag[j],))
            wco = self.sb(es, "wco", [128, 8, D], BF16)
            s_wco = P.slot("wco")
            self.load_w(wco, s_wco, self.w_conv_o[l], 8, D)
            uw = [self.sb(es, f"uw{i}", [128, 8, 544], BF16) for i in range(2)]
            s_uw = P.slots_n(2, "uw")
            g2 = [self.sb(es, f"g2{i}", [128, 8, 512], BF16) for i in range(2)]
            s_g2 = P.slots_n(2, "g2")
            cT = [self.sb(es, f"cT{i}", [128, 8, 512], F32) for i in range(2)]
            s_cT = P.slots_n(2, "cT")
            sqT = [self.sb(es, f"sqT{i}", [128, 8, 512], BF16) for i in range(2)]
            s_sqT = P.slots_n(2, "sqT")
            onesb = self.sb(es, "onesb", [128, 128], BF16)
            s_ob = P.slot("onesb")
            P.op("vector", lambda e: e.memset(onesb[:], 1.0), writes=(s_ob,))
            aT = self.sb(es, "aT", [128, 8, 512], BF16)
            s_aT = P.slot("aT")
            stt = self.sb(es, "stt", [128, 4, 512], F32)
            s_stt = P.slot("stt")
            tmp = [self.sb(es, f"tmpc{i}", [128, 512], F32) for i in range(2)]
            s_tmp = P.slots_n(2, "tmp")
            mcs = self.sb(es, "mcs", [128, 8, 512], BF16)
            s_mcs = P.slot("mcs")
            pc = [self.ps(es, f"pc{i}", [128, 512]) for i in range(4)]
            s_pc = P.slots_n(4, "pc")
            pst = [self.ps(es, f"pst{i}", [128, 512]) for i in range(2)]
            s_pst = P.slots_n(2, "pst")
            po = [self.ps(es, f"po{i}", [128, 512]) for i in range(2)]
            s_po = P.slots_n(2, "po")
            c_pc = 0
            gcount = 0
            prev = None

            def post_a(b, n, r0):
                def mm1(e):
                    for j in range(8):
                        r = e.matmul(pst[0][:, 0:n], self.onesf[:], cT[b][:, j, 0:n], start=(j == 0), stop=(j == 7))
                    return r

                def mm2(e):
                    for j in range(8):
                        r = e.matmul(pst[1][:, 0:n], onesb[:], sqT[b][:, j, 0:n], start=(j == 0), stop=(j == 7))
                    return r
                P.op("tensor", mm1, reads=(s_cT[b],), writes=(s_pst[0],))
                P.op("tensor", mm2, reads=(s_sqT[b], s_ob), writes=(s_pst[1],))
                P.op("vector", lambda e: e.tensor_scalar(out=stt[:, 0, 0:n], in0=pst[0][:, 0:n], scalar1=1.0 / D, scalar2=None, op0=ALU.mult),
                     reads=(s_pst[0],), writes=(s_stt,))
                P.op("vector", lambda e: e.tensor_tensor(out=stt[:, 2, 0:n], in0=stt[:, 0, 0:n], in1=stt[:, 0, 0:n], op=ALU.mult),
                     reads=(s_stt,), writes=(s_stt,))
                P.op("vector", lambda e: e.scalar_tensor_tensor(out=stt[:, 1, 0:n], in0=pst[1][:, 0:n], scalar=1.0 / D, in1=stt[:, 2, 0:n],
                                                                op0=ALU.mult, op1=ALU.subtract),
                     reads=(s_pst[1], s_stt), writes=(s_stt,))
                P.op("vector", lambda e: e.tensor_scalar(out=stt[:, 2, 0:n], in0=stt[:, 1, 0:n], scalar1=1.0, scalar2=EPS, op0=ALU.mult, op1=ALU.add),
                     reads=(s_stt,), writes=(s_stt,))
                P.op("scalar", lambda e: e.activation(out=stt[:, 3, 0:n], in_=stt[:, 2, 0:n], func=AF.Sqrt), reads=(s_stt,), writes=(s_stt,))
                P.op("vector", lambda e: e.reciprocal(out=stt[:, 1, 0:n], in_=stt[:, 3, 0:n]), reads=(s_stt,), writes=(s_stt,))
                for j in range(8):
                    k = j % 2
                    P.op("vector", lambda e, j=j, k=k: e.tensor_tensor(out=tmp[k][:, 0:n], in0=cT[b][:, j, 0:n], in1=stt[:, 0, 0:n], op=ALU.subtract),
                         reads=(s_cT[b], s_stt), writes=(s_tmp[k],))
                    P.op("gpsimd", lambda e, j=j, k=k: e.tensor_tensor(out=tmp[k][:, 0:n], in0=tmp[k][:, 0:n], in1=stt[:, 1, 0:n], op=ALU.mult),
                         reads=(s_tmp[k], s_stt), writes=(s_tmp[k],))
                    P.op("scalar", lambda e, j=j, k=k: e.activation(out=aT[:, j, 0:n], in_=tmp[k][:, 0:n], func=AF.Silu,
                                                                    bias=cvp[:, 2, j:j + 1], scale=cvp[:, 1, j:j + 1]),
                         reads=(s_tmp[k], s_c), writes=(s_aT,))

            def post_b(b, n, r0):
                for m in range(8):
                    i = m % 2

                    def mmo(e, i=i, m=m):
                        for k in range(8):
                            r = e.matmul(po[i][:, 0:n], wco[:, k, m * 128:(m + 1) * 128], aT[:, k, 0:n], start=(k == 0), stop=(k == 7))
                        return r
                    P.op("tensor", mmo, reads=(s_wco, s_aT), writes=(s_po[i],))
                    P.op("vector", lambda e, i=i, m=m: e.tensor_tensor(out=mcs[:, m, 0:n], in0=po[i][:, 0:n], in1=g2[b][:, m, 0:n], op=ALU.mult),
                         reads=(s_po[i], s_g2[b]), writes=(s_mcs,))
                P.dma("sync", self.MC[:, :, r0:r0 + n].rearrange("j p r -> p j r"), mcs[:, :, 0:n], s_mcs, False)

            for gi, (r0, n, kind) in enumerate(GROUPS):
                if kind not in kinds:
                    continue
                b = gcount % 2
                gcount += 1
                c0 = ucol(r0)
                P.dma("sync", uw[b][:, :, 0:n + 30], self.UT[:, :, c0 - 15:c0 + n + 15].rearrange("j p r -> p j r"), s_uw[b], True)
                P.dma("sync", g2[b][:, :, 0:n], self.GT[8:16, :, r0:r0 + n].rearrange("j p r -> p j r"), s_g2[b], True)
                if prev is not None:
                    post_a(*prev)
                for j in range(8):
                    i = c_pc % 4
                    c_pc += 1

                    def mm(e, i=i, j=j, b=b):
                        for k in range(31):
                            r = e.matmul(pc[i][:, 0:n], diag[:, j * 31 + k, :], uw[b][:, j, k:k + n], start=(k == 0), stop=(k == 30))
                        return r
                    P.op("tensor", mm, reads=(s_diag[j], s_uw[b]), writes=(s_pc[i],))
                    P.op("scalar", lambda e, i=i, j=j: e.activation(out=cT[b][:, j, 0:n], in_=pc[i][:, 0:n], func=AF.Identity,
                                                                    bias=cvp[:, 0, j:j + 1], scale=1.0),
                         reads=(s_pc[i], s_c), writes=(s_cT[b],))
                    P.op("gpsimd", lambda e, j=j: e.tensor_tensor(out=sqT[b][:, j, 0:n], in0=cT[b][:, j, 0:n], in1=cT[b][:, j, 0:n], op=ALU.mult),
                         reads=(s_cT[b],), writes=(s_sqT[b],))
                if prev is not None:
                    post_b(*prev)
                prev = (b, n, r0)
            post_a(*prev)
            post_b(*prev)
            P.flush()

    def phase_attn(self, l, kinds):
        P = self.P
        lam_init = 0.8 - 0.6 * math.exp(-0.3 * l)
        with ExitStack() as es:
            lv = self.sb(es, "lv", [128, 4, 64], F32)
            lsm = self.sb(es, "lsm", [128, 8], F32)
            s_l = P.slot("lam")
            for i in range(4):
                P.dma("sync", lv[:, i, :], self.lams[i][l].partition_broadcast(128), s_l, True)
            P.op("vector", lambda e: e.tensor_tensor(out=lv[:, 0, :], in0=lv[:, 0, :], in1=lv[:, 1, :], op=ALU.mult), reads=(s_l,), writes=(s_l,))
            P.op("vector", lambda e: e.tensor_tensor(out=lv[:, 2, :], in0=lv[:, 2, :], in1=lv[:, 3, :], op=ALU.mult), reads=(s_l,), writes=(s_l,))
            P.op("vector", lambda e: e.tensor_reduce(out=lsm[:, 0:1], in_=lv[:, 0, :], axis=AX.X, op=ALU.add), reads=(s_l,), writes=(s_l,))
            P.op("vector", lambda e: e.tensor_reduce(out=lsm[:, 1:2], in_=lv[:, 2, :], axis=AX.X, op=ALU.add), reads=(s_l,), writes=(s_l,))
            P.op("scalar", lambda e: e.activation(out=lsm[:, 2:4], in_=lsm[:, 0:2], func=AF.Exp), reads=(s_l,), writes=(s_l,))
            P.op("vector", lambda e: e.tensor_tensor(out=lsm[:, 4:5], in0=lsm[:, 3:4], in1=lsm[:, 2:3], op=ALU.subtract), reads=(s_l,), writes=(s_l,))
            P.op("vector", lambda e: e.tensor_scalar(out=lsm[:, 5:6], in0=lsm[:, 4:5], scalar1=-lam_init, scalar2=None, op0=ALU.add), reads=(s_l,), writes=(s_l,))
            nlam = lsm[:, 5:6]
            sgb = self.sb(es, "sgb", [128, 128], F32)
            P.dma("sync", sgb[:], self.subln_g[l].partition_broadcast(128), s_l, True)
            P.op("vector", lambda e: e.tensor_scalar(out=sgb[:], in0=sgb[:], scalar1=(1.0 - lam_init), scalar2=None, op0=ALU.mult), reads=(s_l,), writes=(s_l,))
            kt = [self.sb(es, f"kt{i}", [128, RALL], BF16) for i in range(2)]
            s_kt = P.slots_n(2, "kt")
            vx = [self.sb(es, f"vx{i}", [128, 34, 132], BF16) for i in range(2)]
            s_vx = P.slots_n(2, "vx")
            for i in range(2):
                P.op("gpsimd", lambda e, i=i: e.memset(vx[i][:, :, 128:132], 1.0), writes=(s_vx[i],))
            qz = [[self.sb(es, f"qz{c}_{i}", [128, 512], BF16) for i in range(2)] for c in range(2)]
            s_qt = P.slots_n(2, "qt")
            for c in range(2):
                for i in range(2):
                    P.op("gpsimd", lambda e, c=c, i=i: e.memset(qz[c][i][:], 0.0), writes=(s_qt[i],))
            NPT = 6
            pT = [self.sb(es, f"pT{i}", [128, 512], BF16) for i in range(NPT)]
            s_pT = P.slots_n(NPT, "pT")
            sps = [self.ps(es, f"sps{i}", [128, 512]) for i in range(3)]
            s_sps = P.slots_n(3, "sps")
            acc = self.ps(es, "acc", [128, 8, 256])
            s_acc = P.slots_n(2, "acc")
            tpo = self.ps(es, "tpo", [128, 4, 128], BF16)
            s_tpo = P.slot("tpo")
            rr = self.sb(es, "rr", [128, 8], F32)
            o = self.sb(es, "o", [128, 4, 128], F32)
            o2 = self.sb(es, "o2", [128, 4, 128], F32)
            sso = self.sb(es, "sso", [128, 3, 4], F32)
            on = self.sb(es, "on", [128, 4, 128], BF16)
            s_ep = P.slot("ep")
            s_on = P.slot("on")
            ost = [self.sb(es, f"ost{i}", [128, 512], BF16) for i in range(2)]
            s_ost = P.slots_n(2, "ost")
            c_sps = 0
            c_pt = 0
            c_q = 0
            pend_tail = []
            scale = 1.0 / 8.0
            def load_kv(h):
                hb = h % 2
                P.dma("sync", kt[hb][:], self.KT[h], s_kt[hb], True)
                P.dma("sync", vx[hb][:, :, 0:128], self.V[:, h * 128:(h + 1) * 128].rearrange("(c p) d -> p c d", p=128), s_vx[hb], True)

            def load_q(item, qb):
                h, (r0, n, kind) = item
                P.dma("sync", qz[0][qb][0:64, 0:n], self.QT[h, 0:64, r0:r0 + n], s_qt[qb], True)
                P.dma("sync", qz[1][qb][64:128, 0:n], self.QT[h, 64:128, r0:r0 + n], s_qt[qb], True)
            items = [(h, g) for h in range(NH) for g in GROUPS if g[2] in kinds]
            load_kv(0)
            load_q(items[0], 0)
            for ii, (h, (r0, n, kind)) in enumerate(items):
                hb = h % 2
                if True:
                    T = n // 128
                    nkc = 2 if kind == "ctx" else 34
                    qb = ii % 2
                    if ii + 1 < len(items):
                        load_q(items[ii + 1], (ii + 1) % 2)
                    if (ii == 0 or items[ii - 1][0] != h) and h + 1 < NH:
                        load_kv(h + 1)
                    for c in range(2):
                        LAG = 2
                        pend = []
                        for step in range(nkc + LAG):
                            if c == 0 and pend_tail and step == min(10, nkc + LAG - 1):
                                pend_tail.pop(0)()
                            if step < nkc:
                                kc = step
                                si = c_sps % 3
                                c_sps += 1
                                pi = c_pt % NPT
                                c_pt += 1
                                P.op("tensor", lambda e, si=si, kc=kc, c=c, hb=hb, qb=qb: e.matmul(
                                    sps[si][:, 0:n], kt[hb][:, kc * 128:(kc + 1) * 128],
                                    qz[c][qb][:, 0:n], start=True, stop=True),
                                    reads=(s_kt[hb], s_qt[qb]), writes=(s_sps[si],))
                                P.op("scalar", lambda e, si=si, pi=pi: e.activation(
                                    out=pT[pi][:, 0:n], in_=sps[si][:, 0:n], func=AF.Exp, scale=scale),
                                    reads=(s_sps[si],), writes=(s_pT[pi],))
                                pend.append((kc, pi))
                            if step >= LAG:
                                kc, pi = pend.pop(0)

                                def pv(e, kc=kc, pi=pi, c=c, hb=hb):
                                    for t in range(T):
                                        r = e.matmul(acc[:, c * 4 + t, 0:129], pT[pi][:, t * 128:(t + 1) * 128], vx[hb][:, kc, 0:129],
                                                     start=(kc == 0 and t % 2 == 0), stop=(kc == nkc - 1), skip_group_check=True)
                                    return r
                                P.op("tensor", pv, reads=(s_pT[pi], s_vx[hb]), writes=(s_acc[c],))
                    acc4 = acc[:].rearrange("p (c t) w -> p c t w", c=2)
                    P.op("vector", lambda e: e.reciprocal(out=rr[:].rearrange("p (c t) -> p c t", c=2)[:, :, 0:T],
                                                          in_=acc4[:, :, 0:T, 128]), reads=(s_acc[0], s_acc[1]), writes=(s_ep,))
                    P.op("vector", lambda e: e.tensor_scalar(out=rr[:, 4:4 + T], in0=rr[:, 4:4 + T], scalar1=nlam, scalar2=None, op0=ALU.mult),
                         reads=(s_ep, s_l), writes=(s_ep,))
                    P.op("vector", lambda e: e.tensor_tensor(out=o[:, 0:T, :], in0=acc4[:, 0, 0:T, 0:128],
                                                             in1=rr[:, 0:T].unsqueeze(2).to_broadcast([128, T, 128]), op=ALU.mult),
                         reads=(s_acc[0], s_ep), writes=(s_ep,))
                    P.op("vector", lambda e: e.tensor_tensor(out=o2[:, 0:T, :], in0=acc4[:, 1, 0:T, 0:128],
                                                             in1=rr[:, 4:4 + T].unsqueeze(2).to_broadcast([128, T, 128]), op=ALU.mult),
                         reads=(s_acc[1], s_ep), writes=(s_ep,))
                    P.op("gpsimd", lambda e: e.tensor_tensor(out=o[:, 0:T, :], in0=o[:, 0:T, :], in1=o2[:, 0:T, :], op=ALU.add),
                         reads=(s_ep,), writes=(s_ep,))
                    P.op("gpsimd", lambda e: e.tensor_tensor(out=o2[:, 0:T, :], in0=o[:, 0:T, :], in1=o[:, 0:T, :], op=ALU.mult),
                         reads=(s_ep,), writes=(s_ep,))
                    P.op("vector", lambda e: e.tensor_reduce(out=sso[:, 0, 0:T], in_=o2[:, 0:T, :], axis=AX.X, op=ALU.add),
                         reads=(s_ep,), writes=(s_ep,))
                    self.rsqrt(sso[:, 0, 0:T], sso[:, 2, 0:T], sso[:, 1, 0:T], 1.0 / 128, (s_ep,), (s_ep,), s_ep, T, mode="pool")
                    P.op("vector", lambda e: e.tensor_tensor(out=o[:, 0:T, :], in0=o[:, 0:T, :],
                                                             in1=sso[:, 2, 0:T].unsqueeze(2).to_broadcast([128, T, 128]), op=ALU.mult),
                         reads=(s_ep,), writes=(s_ep,))
                    P.op("vector", lambda e: e.tensor_tensor(out=on[:, 0:T, :], in0=o[:, 0:T, :],
                                                             in1=sgb[:].unsqueeze(1).to_broadcast([128, T, 128]), op=ALU.mult),
                         reads=(s_ep, s_l), writes=(s_on,))

                    def tail(T=T, n=n, ob=qb, h=h, r0=r0):
                        def tr(e):
                            for t in range(T):
                                r = e.transpose(tpo[:, t, :], on[:, t, :], self.ident[:])
                            return r
                        P.op("tensor", tr, reads=(s_on,), writes=(s_tpo,))
                        P.op("vector", lambda e: e.tensor_copy(out=ost[ob][:, 0:n], in_=tpo[:].rearrange("p t q -> p (t q)")[:, 0:n]),
                             reads=(s_tpo,), writes=(s_ost[ob],))
                        P.dma("sync", self.OT[h, :, r0:r0 + n], ost[ob][:, 0:n], s_ost[ob], False)
                    pend_tail.append(tail)
            while pend_tail:
                pend_tail.pop(0)()
            P.flush()

    def phase_merge(self, l, xsrc, xdst, kinds):
        P = self.P
        with ExitStack() as es:
            wao = self.sb(es, "wao", [128, 8, D], BF16)
            wout = self.sb(es, "wout", [128, 8, D], BF16)
            s_waop = self.load_w(wao, None, self.w_attn_o[l], 8, D)
            s_woutp = self.load_w(wout, None, self.w_out[l], 8, D)
            ml2 = {}
            for who, nm in ((0, "lat"), (1, "ctx")):
                ml2[nm] = self.load_mod_tile(es, l, 2, who, f"G_{nm}")
            oT = [self.sb(es, f"oT{i}", [128, 8, 512], BF16) for i in range(2)]
            g1 = [self.sb(es, f"g1{i}", [128, 8, 512], BF16) for i in range(2)]
            mc = [self.sb(es, f"mc{i}", [128, 8, 512], BF16) for i in range(2)]
            s_oT, s_g1, s_mc = P.slots_n(2), P.slots_n(2), P.slots_n(2)
            mg = [self.sb(es, f"mg{i}", [128, 8, 512], BF16) for i in range(2)]
            s_mg = P.slots_n(2)
            tmp = [self.sb(es, f"tm{i}", [128, 512], F32) for i in range(2)]
            s_tmp = P.slots_n(2)
            pa = [self.ps(es, f"pa{i}", [128, 512]) for i in range(2)]
            s_pa = P.slots_n(2)
            po = [self.ps(es, f"pox{i}", [128, 1024]) for i in range(2)]
            s_po = P.slots_n(2)
            xt = [self.sb(es, f"xt{i}", [128, D], F32) for i in range(2)]
            s_xt = P.slots_n(2)
            xo = [self.sb(es, f"xo{i}", [128, D], F32) for i in range(2)]
            s_xo = P.slots_n(2)
            gc = 0
            tcd = {"tc": 0}
            pend_w = []
            work = [g for g in GROUPS if g[2] in kinds]

            def loads(wi):
                r0, n, kind = work[wi]
                b = wi % 2
                P.dma("sync", oT[b][:, :, 0:n], self.OT[:, :, r0:r0 + n].rearrange("j p r -> p j r"), s_oT[b], True)
                P.dma("sync", g1[b][:, :, 0:n], self.GT[0:8, :, r0:r0 + n].rearrange("j p r -> p j r"), s_g1[b], True)
                P.dma("sync", mc[b][:, :, 0:n], self.MC[:, :, r0:r0 + n].rearrange("j p r -> p j r"), s_mc[b], True)
            loads(0)
            for wi, (r0, n, kind) in enumerate(work):
                b = gc % 2
                gc += 1
                G, sG = ml2["ctx" if kind == "ctx" else "lat"]
                if wi + 1 < len(work):
                    loads(wi + 1)
                for m in range(8):
                    i = m % 2

                    def mm(e, i=i, m=m, b=b):
                        for k in range(8):
                            r = e.matmul(pa[i][:, 0:n], wao[:, k, m * 128:(m + 1) * 128], oT[b][:, k, 0:n], start=(k == 0), stop=(k == 7))
                        return r
                    P.op("tensor", mm, reads=(s_waop[m // 4], s_oT[b]), writes=(s_pa[i],))
                    P.op("vector", lambda e, i=i, m=m, b=b: e.tensor_tensor(out=tmp[i][:, 0:n], in0=pa[i][:, 0:n], in1=g1[b][:, m, 0:n], op=ALU.mult),
                         reads=(s_pa[i], s_g1[b]), writes=(s_tmp[i],))
                    P.op("gpsimd", lambda e, i=i, m=m, b=b: e.tensor_tensor(out=mg[b][:, m, 0:n], in0=tmp[i][:, 0:n], in1=mc[b][:, m, 0:n], op=ALU.add),
                         reads=(s_tmp[i], s_mc[b]), writes=(s_mg[b],))
                def wout_part(r0=r0, n=n, b=b, G=G, sG=sG):
                    for t in range(n // 128):
                        i = tcd["tc"] % 2
                        tcd["tc"] += 1
                        P.dma("sync", xt[i][:], xsrc[r0 + t * 128:r0 + (t + 1) * 128, :], s_xt[i], True)

                        def mm(e, i=i, t=t, b=b):
                            for half in range(2):
                                for k in range(8):
                                    r = e.matmul(po[i][:, half * 512:(half + 1) * 512], mg[b][:, k, t * 128:(t + 1) * 128],
                                                 wout[:, k, half * 512:(half + 1) * 512], start=(k == 0), stop=(k == 7))
                            return r
                        P.op("tensor", mm, reads=(s_woutp[0], s_woutp[1], s_mg[b]), writes=(s_po[i],))
                        P.op("vector", lambda e, i=i: e.tensor_tensor(out=xo[i][:], in0=po[i][:], in1=G[:], op=ALU.mult),
                             reads=(s_po[i], sG), writes=(s_xo[i],))
                        P.op("gpsimd", lambda e, i=i: e.tensor_tensor(out=xo[i][:], in0=xo[i][:], in1=xt[i][:], op=ALU.add),
                             reads=(s_xt[i], s_xo[i]), writes=(s_xo[i],))
                        P.dma("sync", xdst[r0 + t * 128:r0 + (t + 1) * 128, :], xo[i][:], s_xo[i], False)
                if pend_w:
                    pend_w.pop(0)()
                pend_w.append(wout_part)
            while pend_w:
                pend_w.pop(0)()
            P.flush()

    def phase_ffn(self, l, groups, xsrc, xdst_fn, experts, router):
        P = self.P
        R = sum(n for _, n, _ in groups)
        NT = R // 128
        with ExitStack() as es:
            pl = self.ps(es, "pl", [128, NT, NE]) if router is not None else None
            es_tp = ExitStack()
            whos = tuple(nm for nm in ("lat", "ctx") if any((k == "ctx") == (nm == "ctx") for _, _, k in groups))
            st = self.setup_norm(es, l, 1, whos=whos, ntp=2, es_tp=es_tp)
            ml5 = {}
            for who, nm in ((0, "lat"), (1, "ctx")):
                if nm in whos:
                    ml5[nm] = self.load_mod_tile(es, l, 5, who, f"G_{nm}")
            hT = self.sb(es, "h2T", [128, 8, R], BF16)
            s_hT = P.slot("h2T")
            accs = self.sb(es, "accs", [128, NT, D], F32)
            s_acc = P.slots_n(NT, "acc")
            col = 0
            for (r0, n, kind) in groups:
                self.emit_hT(st, xsrc, r0, n, kind, hT, s_hT, col0=col)
                col += n
            gates = None
            if router is not None:
                wr = self.sb(es, "wr", [128, 8, NE], BF16)
                s_wr = P.slot("wr")
                P.dma("gpsimd", wr[:], router.rearrange("(k p) n -> p k n", p=128), s_wr, True)
                gates = self.sb(es, "gates", [128, NT, NE], F32)
                s_gates = P.slot("gates")
                lg = self.sb(es, "lg", [128, NT, NE], F32)
                mx = self.sb(es, "mx", [128, NT, 8], F32)
                sm = self.sb(es, "smx", [128, NT, 2], F32)
                s_pl = P.slot("pl")
                s_lg = P.slot("lg")
                for t in range(NT):
                    def mm(e, t=t):
                        for k in range(8):
                            r = e.matmul(pl[:, t, :], hT[:, k, t * 128:(t + 1) * 128], wr[:, k, :], start=(k == 0), stop=(k == 7))
                        return r
                    P.op("tensor", mm, reads=(s_hT, s_wr), writes=(s_pl,))
                P.op("vector", lambda e: e.tensor_copy(out=lg[:], in_=pl[:]), reads=(s_pl,), writes=(s_lg,))
                for t in range(NT):
                    P.op("vector", lambda e, t=t: e.max(out=mx[:, t, :], in_=lg[:, t, :]), reads=(s_lg,), writes=(s_lg,))
                P.op("vector", lambda e: e.tensor_tensor(out=gates[:], in0=lg[:], in1=mx[:, :, 1:2].to_broadcast([128, NT, NE]), op=ALU.is_ge),
                     reads=(s_lg,), writes=(s_gates,))
                P.op("vector", lambda e: e.tensor_tensor(out=lg[:], in0=lg[:], in1=mx[:, :, 0:1].to_broadcast([128, NT, NE]), op=ALU.subtract),
                     reads=(s_lg,), writes=(s_lg,))
                P.op("scalar", lambda e: e.activation(out=lg[:], in_=lg[:], func=AF.Exp), reads=(s_lg,), writes=(s_lg,))
                P.op("vector", lambda e: e.tensor_tensor(out=gates[:], in0=gates[:], in1=lg[:], op=ALU.mult), reads=(s_lg, s_gates), writes=(s_gates,))
                P.op("vector", lambda e: e.tensor_reduce(out=sm[:, :, 0], in_=gates[:], axis=AX.X, op=ALU.add), reads=(s_gates,), writes=(s_lg,))
                P.op("vector", lambda e: e.reciprocal(out=sm[:, :, 1], in_=sm[:, :, 0]), reads=(s_lg,), writes=(s_lg,))
                P.op("vector", lambda e: e.tensor_tensor(out=gates[:], in0=gates[:], in1=sm[:, :, 1:2].to_broadcast([128, NT, NE]), op=ALU.mult),
                     reads=(s_lg, s_gates), writes=(s_gates,))
            es_tp.close()
            NPG = 4 if router is None else 3
            wg = [self.sb(es, f"wg{i}", [128, 8, 512], BF16) for i in range(2)]
            wu = [self.sb(es, f"wu{i}", [128, 8, 512], BF16) for i in range(2)]
            wd = [self.sb(es, f"wd{i}", [128, 4, D], BF16) for i in range(2)]
            s_wg, s_wu, s_wd = P.slots_n(2), P.slots_n(2), P.slots_n(2)
            pgu = [self.ps(es, f"pgu{i}", [128, 512]) for i in range(NPG)]
            s_pgu = P.slots_n(NPG)
            pd = [self.ps(es, f"pd{i}", [128, 1024]) for i in range(2)]
            s_pd = P.slots_n(2)
            sgl = [self.sb(es, f"sgl{i}", [128, 512], F32) for i in range(2)]
            s_sgl = P.slots_n(2)
            act = [self.sb(es, f"act{i}", [128, 4, 512], BF16) for i in range(2)]
            s_act = P.slots_n(2)
            sets = []
            for ei, (wga, wua, wda, F) in enumerate(experts):
                nch = F // 128
                for c0 in range(0, nch, 4):
                    sets.append((ei, c0, min(4, nch - c0)))
            c_gu = 0
            c_act = 0
            c_pd = 0
            first = [True] * NT
            rch = [(c, min(512, R - c)) for c in range(0, R, 512)]

            def load_set(si):
                ei, c0, nc_ = sets[si]
                wga, wua, wda, F = experts[ei]
                b = si % 2
                P.dma("gpsimd", wg[b][:, :, 0:nc_ * 128], wga.rearrange("(k p) n -> p k n", p=128)[:, :, c0 * 128:(c0 + nc_) * 128], s_wg[b], True)
                P.dma("gpsimd", wu[b][:, :, 0:nc_ * 128], wua.rearrange("(k p) n -> p k n", p=128)[:, :, c0 * 128:(c0 + nc_) * 128], s_wu[b], True)
                P.dma("gpsimd", wd[b][:, 0:nc_, :], wda[c0 * 128:(c0 + nc_) * 128, :].rearrange("(k p) n -> p k n", p=128), s_wd[b], True)
            load_set(0)
            cpd = {"c": 0}
            pend_d = []
            for si, (ei, c0, nc_) in enumerate(sets):
                b = si % 2
                while pend_d:
                    pend_d.pop(0)()
                if si + 1 < len(sets):
                    load_set(si + 1)
                for (rc0, rn) in rch:
                    ab = c_act % 2
                    c_act += 1
                    for j in range(nc_):
                        ig = c_gu % NPG
                        iu = (c_gu + 1) % NPG
                        c_gu += 2

                        def mmg(e, j=j, ig=ig, b=b):
                            for k in range(8):
                                r = e.matmul(pgu[ig][:, 0:rn], wg[b][:, k, j * 128:(j + 1) * 128], hT[:, k, rc0:rc0 + rn], start=(k == 0), stop=(k == 7))
                            return r

                        def mmu(e, j=j, iu=iu, b=b):
                            for k in range(8):
                                r = e.matmul(pgu[iu][:, 0:rn], wu[b][:, k, j * 128:(j + 1) * 128], hT[:, k, rc0:rc0 + rn], start=(k == 0), stop=(k == 7))
                            return r
                        P.op("tensor", mmg, reads=(s_wg[b], s_hT), writes=(s_pgu[ig],))
                        P.op("tensor", mmu, reads=(s_wu[b], s_hT), writes=(s_pgu[iu],))
                        k2 = j % 2
                        P.op("scalar", lambda e, ig=ig, k2=k2: e.activation(out=sgl[k2][:, 0:rn], in_=pgu[ig][:, 0:rn], func=AF.Silu),
                             reads=(s_pgu[ig],), writes=(s_sgl[k2],))
                        P.op("vector", lambda e, iu=iu, k2=k2, j=j, ab=ab: e.tensor_tensor(
                            out=act[ab][:, j, 0:rn], in0=pgu[iu][:, 0:rn], in1=sgl[k2][:, 0:rn], op=ALU.mult),
                            reads=(s_pgu[iu], s_sgl[k2]), writes=(s_act[ab],))
                    def down_part(rc0=rc0, rn=rn, ab=ab, b=b, ei=ei, nc_=nc_):
                        for tt in range(rn // 128):
                            t = rc0 // 128 + tt
                            ip = cpd["c"] % 2
                            cpd["c"] += 1

                            def mmd(e, tt=tt, ip=ip, ab=ab, b=b):
                                for half in range(2):
                                    for j in range(nc_):
                                        r = e.matmul(pd[ip][:, half * 512:(half + 1) * 512], act[ab][:, j, tt * 128:(tt + 1) * 128],
                                                     wd[b][:, j, half * 512:(half + 1) * 512], start=(j == 0), stop=(j == nc_ - 1))
                                return r
                            P.op("tensor", mmd, reads=(s_wd[b], s_act[ab]), writes=(s_pd[ip],))
                            if gates is not None:
                                gsc = gates[:, t, ei:ei + 1]
                                rd = (s_pd[ip], s_gates)
                                if first[t]:
                                    P.op("vector", lambda e, t=t, ip=ip, gsc=gsc: e.tensor_scalar(
                                        out=accs[:, t, :], in0=pd[ip][:], scalar1=gsc, scalar2=None, op0=ALU.mult), reads=rd, writes=(s_acc[t],))
                                else:
                                    P.op("vector", lambda e, t=t, ip=ip, gsc=gsc: e.scalar_tensor_tensor(
                                        out=accs[:, t, :], in0=pd[ip][:], scalar=gsc, in1=accs[:, t, :], op0=ALU.mult, op1=ALU.add),
                                        reads=rd + (s_acc[t],), writes=(s_acc[t],))
                            else:
                                if first[t]:
                                    P.op("vector", lambda e, t=t, ip=ip: e.tensor_copy(out=accs[:, t, :], in_=pd[ip][:]), reads=(s_pd[ip],), writes=(s_acc[t],))
                                else:
                                    P.op("vector", lambda e, t=t, ip=ip: e.tensor_tensor(out=accs[:, t, :], in0=pd[ip][:], in1=accs[:, t, :], op=ALU.add),
                                         reads=(s_pd[ip], s_acc[t]), writes=(s_acc[t],))
                            first[t] = False
                    if pend_d:
                        pend_d.pop(0)()
                    pend_d.append(down_part)
            while pend_d:
                pend_d.pop(0)()
            xt = st["x"]
            s_xt = st["s_x"]
            t = 0
            for (r0, n, kind) in groups:
                G, sG = ml5["ctx" if kind == "ctx" else "lat"]
                for tt in range(n // 128):
                    i = t % 2
                    P.dma("sync", xt[i][:], xsrc[r0 + tt * 128:r0 + (tt + 1) * 128, :], s_xt[i], True)
                    P.op("gpsimd", lambda e, t=t: e.tensor_tensor(out=accs[:, t, :], in0=accs[:, t, :], in1=G[:], op=ALU.mult),
                         reads=(s_acc[t], sG), writes=(s_acc[t],))
                    P.op("vector", lambda e, t=t, i=i: e.tensor_tensor(out=accs[:, t, :], in0=accs[:, t, :], in1=xt[i][:], op=ALU.add),
                         reads=(s_acc[t], s_xt[i]), writes=(s_acc[t],))
                    P.dma("sync", xdst_fn(r0 + tt * 128), accs[:, t, :], s_acc[t], False)
                    t += 1
            P.flush()

    def build(self):
        upto = self.upto
        stage = [0]

        def done():
            stage[0] += 1
            return upto is not None and stage[0] >= upto
        self.phase_const()
        ALLK = ("ctx", "own", "oth")
        self.phase_mod((0, 1))
        if done(): return
        self.phase_kvq(0, self.x0, ALLK)
        if done(): return
        self.phase_glu(0, self.x0, set(range(9)), ALLK, do_flush=False)
        self.P.drain("sync")
        self.P.drain("vector")
        self.phase_fix()
        if done(): return
        self.phase_conv(0, ALLK)
        if done(): return
        self.phase_attn(0, ALLK)
        if done(): return
        self.phase_merge(0, self.x0, self.XS, ALLK)
        if done(): return
        dense = [(self.w_ff_gate[0], self.w_ff_up[0], self.w_ff_down[0], DFF)]
        xs_fn = lambda r: self.XS[r:r + 128, :]
        self.phase_ffn(0, GROUPS[0:3], self.XS, xs_fn, dense, None)
        self.phase_ffn(0, GROUPS[3:6], self.XS, xs_fn, dense, None)
        self.phase_ffn(0, GROUPS[6:9], self.XS, xs_fn, dense, None)
        if done(): return
        self.phase_kvq(1, self.XS, ("own",))
        self.phase_glu(1, self.XS, {1, 2, 3, 4, 5, 8}, ("own",), do_flush=False)
        self.P.drain("sync")
        self.P.drain("vector")
        self.phase_fix()
        self.phase_conv(1, ("own",))
        if done(): return
        self.phase_attn(1, ("own",))
        self.phase_merge(1, self.XS, self.XS, ("own",))
        if done(): return
        experts = [(self.w_exp_gate[0, e], self.w_exp_up[0, e], self.w_exp_down[0, e], DFE) for e in range(NE)]
        out_fn = lambda r: self.out[r - CTX:r - CTX + 128, :]
        self.phase_ffn(1, GROUPS[1:5], self.XS, out_fn, experts, self.w_router[0])


def build_nc(upto=None):
    nc = bass.Bass("TRN2", target_bir_lowering=False)
    b = Builder(nc, upto)
    for nm in ("kvq", "glu", "conv", "attn", "merge", "ffn", "mod"):
        if nm in SKIP:
            setattr(b, "phase_" + nm, lambda *a, **k: None)
    b.build()
    return nc


def rope_tables():
    S, GW, RA = 4096, 64, 32
    t = np.arange(S)
    row = (t // GW).astype(np.float32)
    col = (t % GW).astype(np.float32)
    inv = (10000.0 ** (-np.arange(0, RA, 2, dtype=np.float32) / RA)).astype(np.float32)
    ar = row[:, None] * inv
    ac = col[:, None] * inv
    ang = np.concatenate([ar, ar, ac, ac], axis=-1).astype(np.float32)
    cos = np.cos(ang).astype(np.float32)
    sin = np.sin(ang).astype(np.float32)
    ss = sin.copy()
    ss4 = ss.reshape(S, 2, 2, 16)
    ss4[:, :, 0, :] *= -1.0
    return cos, ss4.reshape(S, 64)


def make_in_maps(inputs):
    f = lambda a: np.ascontiguousarray(np.asarray(a, dtype=np.float32))
    x = f(inputs["x"]); c = f(inputs["c"]); ctx = f(inputs["ctx"]); c_ctx = f(inputs["c_ctx"])
    cos, ss = rope_tables()
    shared = {}
    for k in ("w_mod", "b_mod", "g_mix", "w_in", "q_norm_g", "k_norm_g", "lambda_q1", "lambda_k1", "lambda_q2",
              "lambda_k2", "subln_g", "w_attn_o", "w_conv_o", "w_out", "g_ffn", "w_ff_gate", "w_ff_up", "w_ff_down",
              "w_router", "w_exp_gate", "w_exp_up", "w_exp_down"):
        shared[k] = f(inputs[k])
    dw = f(inputs["dw_weight"])
    shared["dwT"] = np.ascontiguousarray(dw.reshape(2, 31, 8, 128).transpose(0, 3, 2, 1))
    cv = np.stack([f(inputs["dw_bias"]), f(inputs["conv_ln_g"]), f(inputs["conv_ln_b"])], axis=1)
    shared["cvp"] = np.ascontiguousarray(cv.reshape(2, 3, 8, 128).transpose(0, 3, 1, 2))
    shared["identf"] = np.eye(128, dtype=np.float32)
    maps = []
    for core in range(8):
        b, h = core // 2, core % 2
        own = slice(h * HALF, (h + 1) * HALF)
        oth = slice((1 - h) * HALF, (2 - h) * HALF)
        m = dict(shared)
        m["x0"] = np.ascontiguousarray(np.concatenate([ctx[b], x[b, own], x[b, oth]], axis=0))
        cvec = np.stack([c[b].reshape(8, 128).T, c_ctx.reshape(8, 128).T], axis=2).reshape(128, 16)
        m["cvec"] = np.ascontiguousarray(cvec)
        rp = np.zeros((RALL, 2, 64), np.float32)
        rp[:CTX, 0, :] = 1.0
        rp[CTX:CTX + HALF, 0, :] = cos[own]; rp[CTX:CTX + HALF, 1, :] = ss[own]
        rp[CTX + HALF:, 0, :] = cos[oth]; rp[CTX + HALF:, 1, :] = ss[oth]
        m["rope"] = rp
        mk = np.zeros((128, 2), np.float32)
        mk[:, 0] = float(h); mk[:, 1] = float(1 - h)
        m["mask"] = mk
        maps.append(m)
    return maps


def kernel(**inputs):
    maps = make_in_maps(inputs)
    nc = build_nc()
    res = run_bass_kernel_spmd(nc, maps, core_ids=list(range(8)))
    out = np.zeros((4, 4096, D), np.float32)
    for core in range(8):
        b, h = core // 2, core % 2
        out[b, h * HALF:(h + 1) * HALF] = res.results[core]["out"]
    return out
```
